# Optimizing a Trainium2 kernel written in Bass

```python
import math
import jax, jax.numpy as jnp
from jax import lax
import numpy as np


D_MODEL = 2048
BATCH = 16
SEQ = 256
DEPTH = 4
DEC_BATCH = 8
DEC_SEQ = 4096
PAST_LEN = 512

GRID_W = 64
CHUNK = 64
N_EVEN = (DEPTH + 1) // 2
N_ODD = DEPTH // 2
H_A = 4
DK_A = 256
DV_A = 512
ROPE_BASE = 10000.0
H_B = 8
DK_B = 128
DV_B = 128
CONV_B = 5
HY_CONV = 3
HY_BANDS = 16
HY_EMB = 1 + 2 * HY_BANDS
HY_FFN = 64
HY_FAST_DECAY = 0.3
HY_SLOW_DECAY = 1.5
HY_TARGET = 1e-2
D_FF = ((8 * D_MODEL // 3 + 255) // 256) * 256
EPS = 1e-6
SIZES_AB = (H_A * DK_A, H_A * DK_A, H_A * DV_A, H_A * DV_A, 2 * H_B * DK_B + H_B * DV_B, H_B * DV_B, 2 * H_B, 2 * H_B)
PROJ_AB = sum(SIZES_AB)
MIX_AB_OUT = H_A * DV_A + H_B * DV_B

kernel_name = "hybrid_retention_gdn_hyena_diffusion_step"

F32 = jnp.float32


def rms_norm(x, w):
    xf = x.astype(F32)
    y = xf * lax.rsqrt(jnp.mean(xf * xf, axis=-1, keepdims=True) + EPS)
    return (y * w.astype(F32)).astype(x.dtype)


def head_layer_norm(y):
    mu = jnp.mean(y, axis=-1, keepdims=True)
    yc = y - mu
    return yc * lax.rsqrt(jnp.mean(yc * yc, axis=-1, keepdims=True) + EPS)


def l2norm(x):
    return x * lax.rsqrt(jnp.sum(x * x, axis=-1, keepdims=True) + EPS)


def short_conv(x, w):
    K = w.shape[0]
    p = K // 2
    L = x.shape[1]
    xp = jnp.pad(x, ((0, 0), (p, p), (0, 0)))
    return sum(xp[:, j:j + L] * w[j] for j in range(K))


def to_chunks(a):
    B, L = a.shape[:2]
    a = a.reshape(B, L // CHUNK, CHUNK, *a.shape[2:])
    return jnp.moveaxis(a, 2, 3)


def from_chunks(a):
    B, n = a.shape[:2]
    a = jnp.moveaxis(a, 3, 2)
    return a.reshape(B, n * CHUNK, *a.shape[3:])


def rope_2d(n_tokens):
    rows = n_tokens // GRID_W
    r = jnp.repeat(jnp.arange(rows, dtype=F32), GRID_W)
    col = (jnp.arange(rows * GRID_W) % GRID_W).astype(F32)
    n_freq = DK_A // 4
    inv = ROPE_BASE ** (-jnp.arange(n_freq, dtype=F32) / n_freq)
    ang = jnp.concatenate([r[:, None] * inv, col[:, None] * inv], axis=-1)
    return jnp.cos(ang), jnp.sin(ang)


def apply_rope(x, cos, sin):
    c = cos[None, :, None, :]
    s = sin[None, :, None, :]
    x1, x2 = x[..., :DK_A // 2], x[..., DK_A // 2:]
    return jnp.concatenate([x1 * c - x2 * s, x1 * s + x2 * c], axis=-1)


def retention_chunked(q, k, v, log_gamma, s0):
    qc, kc, vc = to_chunks(q), to_chunks(k), to_chunks(v)
    idx = jnp.arange(CHUNK, dtype=F32)
    rel = idx[:, None] - idx[None, :]
    dmask = jnp.where(rel >= 0, jnp.exp(jnp.maximum(rel, 0.0) * log_gamma[:, None, None]), 0.0)
    scores = jnp.einsum('bnhtd,bnhsd->bnhts', qc, kc) * dmask
    intra = jnp.einsum('bnhts,bnhse->bnhte', scores, vc)
    q_dec = jnp.exp((idx + 1.0) * log_gamma[:, None])[..., None]
    k_dec = jnp.exp((CHUNK - 1.0 - idx) * log_gamma[:, None])[..., None]
    c_dec = jnp.exp(CHUNK * log_gamma)[:, None, None]

    def step(s, xs):
        q_n, k_n, v_n = xs
        cross = jnp.einsum('bhcd,bhde->bhce', q_n * q_dec, s)
        s = s * c_dec + jnp.einsum('bhcd,bhce->bhde', k_n * k_dec, v_n)
        return s, cross

    xs = (jnp.moveaxis(qc, 1, 0), jnp.moveaxis(kc, 1, 0), jnp.moveaxis(vc, 1, 0))
    s_fin, cross = lax.scan(step, s0, xs)
    return from_chunks(intra + jnp.moveaxis(cross, 0, 1)), s_fin


def gated_delta_chunked(q, k, v, g, beta, s0):
    dk = q.shape[-1]
    qc, kc, vc = to_chunks(q), to_chunks(k), to_chunks(v)
    gc, bc = to_chunks(g), to_chunks(beta)
    G = jnp.cumsum(gc, axis=-1)
    idx = jnp.arange(CHUNK)
    incl = idx[:, None] >= idx[None, :]
    strict = idx[:, None] > idx[None, :]
    decay = jnp.exp(jnp.where(incl, G[..., :, None] - G[..., None, :], -jnp.inf))
    kk = jnp.einsum('bnhtd,bnhsd->bnhts', kc, kc)
    a_mat = jnp.eye(CHUNK, dtype=F32) + jnp.where(strict, bc[..., :, None] * kk * decay, 0.0)
    rhs = jnp.concatenate([(bc * jnp.exp(G))[..., None] * kc, bc[..., None] * vc], axis=-1)
    sol = lax.linalg.triangular_solve(a_mat, rhs, left_side=True, lower=True, unit_diagonal=True)
    w_c, u_c = sol[..., :dk], sol[..., dk:]
    qk = jnp.einsum('bnhtd,bnhsd->bnhts', qc, kc) * decay
    q_dec = qc * jnp.exp(G)[..., None]
    k_dec = kc * jnp.exp(G[..., -1:] - G)[..., None]
    c_dec = jnp.exp(G[..., -1])[..., None, None]

    def step(s, xs):
        w_n, u_n, qk_n, qd_n, kd_n, cd_n = xs
        u = u_n - jnp.einsum('bhcd,bhde->bhce', w_n, s)
        o = jnp.einsum('bhcd,bhde->bhce', qd_n, s) + jnp.einsum('bhts,bhse->bhte', qk_n, u)
        s = s * cd_n + jnp.einsum('bhcd,bhce->bhde', kd_n, u)
        return s, o

    xs = tuple(jnp.moveaxis(a, 1, 0) for a in (w_c, u_c, qk, q_dec, k_dec, c_dec))
    s_fin, o = lax.scan(step, s0, xs)
    return from_chunks(jnp.moveaxis(o, 0, 1)), s_fin


def mixer_ab(h, w_in, w_out, ret_decay, ret_gn_w, gdn_conv_w, gdn_a_log, gdn_dt_bias, gdn_norm_w, ret_s0, gdn_s0, rope):
    B, L, _ = h.shape
    proj = h @ w_in
    split_idx = np.cumsum(SIZES_AB)[:-1].tolist()
    qa, ka, va, ga, qkv_b, zb, ab, bb = jnp.split(proj, split_idx, axis=-1)
    qa = qa.astype(F32).reshape(B, L, H_A, DK_A) * DK_A ** -0.5
    ka = ka.astype(F32).reshape(B, L, H_A, DK_A)
    va = va.astype(F32).reshape(B, L, H_A, DV_A)
    if rope is not None:
        qa = apply_rope(qa, *rope)
        ka = apply_rope(ka, *rope)
    lg = jnp.log1p(-jnp.exp2(ret_decay.astype(F32)))
    s0r = ret_s0.astype(F32)
    ya_f, sa_f = retention_chunked(qa, ka, va, lg[0], s0r[:, 0])
    ya_b, sa_b = retention_chunked(qa[:, ::-1], ka[:, ::-1], va[:, ::-1], lg[1], s0r[:, 1])
    ya = head_layer_norm(ya_f + ya_b[:, ::-1]).reshape(B, L, H_A * DV_A) * ret_gn_w.astype(F32)
    ya = jax.nn.silu(ga.astype(F32)) * ya
    qkv_b = jax.nn.silu(short_conv(qkv_b, gdn_conv_w).astype(F32))
    qb, kb, vb = jnp.split(qkv_b, [H_B * DK_B, 2 * H_B * DK_B], axis=-1)
    qb = l2norm(qb.reshape(B, L, H_B, DK_B)) * DK_B ** -0.5
    kb = l2norm(kb.reshape(B, L, H_B, DK_B))
    vb = vb.reshape(B, L, H_B, DV_B)
    ab = ab.astype(F32).reshape(B, L, 2, H_B)
    bb = bb.astype(F32).reshape(B, L, 2, H_B)
    g = -jnp.exp(gdn_a_log.astype(F32)) * jax.nn.softplus(ab + gdn_dt_bias.astype(F32))
    beta = jax.nn.sigmoid(bb)
    s0g = gdn_s0.astype(F32)
    yb_f, sb_f = gated_delta_chunked(qb, kb, vb, g[:, :, 0], beta[:, :, 0], s0g[:, 0])
    yb_b, sb_b = gated_delta_chunked(qb[:, ::-1], kb[:, ::-1], vb[:, ::-1], g[:, ::-1, 1], beta[:, ::-1, 1], s0g[:, 1])
    yb = rms_norm(yb_f + yb_b[:, ::-1], gdn_norm_w).reshape(B, L, H_B * DV_B) * jax.nn.silu(zb.astype(F32))
    out = jnp.concatenate([ya, yb], axis=-1).astype(h.dtype) @ w_out
    return out, jnp.stack([sa_f, sa_b], axis=1), jnp.stack([sb_f, sb_b], axis=1)


def hyena_filters(L, w1, b1, freq, w2, b2, w3):
    t = jnp.linspace(0.0, 1.0, L, dtype=F32)[:, None]
    w_ang = 2.0 * math.pi * jnp.arange(L, dtype=F32)[:, None] / L
    bands = jnp.linspace(1e-4, HY_BANDS - 1, HY_BANDS, dtype=F32)[None, :]
    z = jnp.concatenate([t, jnp.cos(bands * w_ang), -jnp.sin(bands * w_ang)], axis=-1)
    fr = freq.astype(F32)
    hid = jnp.sin(fr * (z @ w1.astype(F32) + b1.astype(F32)))
    hid = jnp.sin(fr * (hid @ w2.astype(F32) + b2.astype(F32)))
    filt = hid @ w3.astype(F32)
    deltas = jnp.abs(jnp.linspace(math.log(HY_TARGET) / HY_SLOW_DECAY, math.log(HY_TARGET) / HY_FAST_DECAY, D_MODEL, dtype=F32))
    window = jnp.exp(-t * deltas)
    return filt[:, :D_MODEL] * window, filt[:, D_MODEL:] * window


def long_conv_bidir(v, h_f, h_b, bias):
    L, D = h_f.shape
    h_all = jnp.concatenate([h_f, jnp.zeros((1, D), F32), h_b[:0:-1]], axis=0)
    hf = jnp.fft.rfft(h_all, axis=0)
    vf = jnp.fft.rfft(v, n=2 * L, axis=1)
    y = jnp.fft.irfft(vf * hf[None], n=2 * L, axis=1)[:, :L]
    return y + v * bias


def mixer_c(h, w_in, conv_w, w_out, w1, b1, freq, w2, b2, w3, bias):
    L = h.shape[1]
    u = short_conv(h @ w_in, conv_w).astype(F32)
    x0, x1, v = jnp.split(u, 3, axis=-1)
    h_f, h_b = hyena_filters(L, w1, b1, freq, w2, b2, w3)
    y = long_conv_bidir(v * x1, h_f, h_b, bias.astype(F32))
    return (y * x0).astype(h.dtype) @ w_out


def swiglu(h, wg, wu, wd):
    return (jax.nn.silu(h @ wg) * (h @ wu)) @ wd


def trunk(x, cond, ret_s0, gdn_s0, rope, p):
    sc_cond = jax.nn.silu(cond)
    ret_fin, gdn_fin = [], []
    for l in range(DEPTH):
        mod = (sc_cond @ p['w_mod'][l] + p['b_mod'][l])[:, None, :]
        sh1, sc1, g1, sh2, sc2, g2 = jnp.split(mod, 6, axis=-1)
        h = rms_norm(x, p['norm_mix_pre'][l]) * (1.0 + sc1) + sh1
        if l % 2 == 0:
            e = l // 2
            y, s_r, s_g = mixer_ab(h, p['w_in_ab'][e], p['w_out_ab'][e], p['ret_decay'][e], p['ret_gn_w'][e],
                                   p['gdn_conv_w'][e], p['gdn_a_log'][e], p['gdn_dt_bias'][e], p['gdn_norm_w'][e],
                                   ret_s0[:, e], gdn_s0[:, e], rope)
            ret_fin.append(s_r)
            gdn_fin.append(s_g)
        else:
            o = l // 2
            y = mixer_c(h, p['w_in_c'][o], p['hy_conv_w'][o], p['w_out_c'][o], p['hy_w1'][o], p['hy_b1'][o],
                        p['hy_freq'][o], p['hy_w2'][o], p['hy_b2'][o], p['hy_w3'][o], p['hy_bias'][o])
        x = x + g1 * rms_norm(y, p['norm_mix_post'][l])
        h = rms_norm(x, p['norm_ffn_pre'][l]) * (1.0 + sc2) + sh2
        y = swiglu(h, p['w_ffn_gate'][l], p['w_ffn_up'][l], p['w_ffn_down'][l])
        x = x + g2 * rms_norm(y, p['norm_ffn_post'][l])
    return x, jnp.stack(ret_fin, axis=1), jnp.stack(gdn_fin, axis=1)


def setup_inputs(seed: int = 0) -> dict:
    key = jax.random.key(seed)
    ks = iter(jax.random.split(key, 48))

    def nrm(shape, scale):
        return jax.random.normal(next(ks), shape, F32) * scale

    def gain(shape):
        return 1.0 + nrm(shape, 0.05)

    x_prompt = nrm((BATCH, SEQ, D_MODEL), 1.0)
    x_sample = nrm((DEC_BATCH, DEC_SEQ, D_MODEL), 1.0)
    c = nrm((DEC_BATCH, D_MODEL), 1.0)
    state_ret = nrm((DEC_BATCH, N_EVEN, 2, H_A, DK_A, DV_A), 2.0)
    state_gdn = nrm((DEC_BATCH, N_EVEN, 2, H_B, DK_B, DV_B), 0.3)
    c_ctx = nrm((D_MODEL,), 1.0)
    w_mod = nrm((DEPTH, D_MODEL, 6 * D_MODEL), D_MODEL ** -0.5)
    b_mod = nrm((DEPTH, 6 * D_MODEL), 0.02)
    norm_mix_pre = gain((DEPTH, D_MODEL))
    norm_mix_post = gain((DEPTH, D_MODEL))
    norm_ffn_pre = gain((DEPTH, D_MODEL))
    norm_ffn_post = gain((DEPTH, D_MODEL))
    w_ffn_gate = nrm((DEPTH, D_MODEL, D_FF), D_MODEL ** -0.5)
    w_ffn_up = nrm((DEPTH, D_MODEL, D_FF), D_MODEL ** -0.5)
    w_ffn_down = nrm((DEPTH, D_FF, D_MODEL), D_FF ** -0.5)
    w_in_ab = nrm((N_EVEN, D_MODEL, PROJ_AB), D_MODEL ** -0.5)
    w_out_ab = nrm((N_EVEN, MIX_AB_OUT, D_MODEL), MIX_AB_OUT ** -0.5)
    ret_decay = -(5.0 + jnp.arange(H_A, dtype=F32)) + jax.random.uniform(next(ks), (N_EVEN, 2, H_A), F32, -0.3, 0.3)
    ret_gn_w = gain((N_EVEN, H_A * DV_A))
    gdn_conv_w = nrm((N_EVEN, CONV_B, 2 * H_B * DK_B + H_B * DV_B), CONV_B ** -0.5)
    gdn_a_log = jnp.log(jax.random.uniform(next(ks), (N_EVEN, 2, H_B), F32, 1.0, 16.0))
    dt = jnp.exp(jax.random.uniform(next(ks), (N_EVEN, 2, H_B), F32, math.log(1e-3), math.log(0.1)))
    gdn_dt_bias = dt + jnp.log(-jnp.expm1(-dt))
    gdn_norm_w = gain((N_EVEN, DV_B))
    w_in_c = nrm((N_ODD, D_MODEL, 3 * D_MODEL), D_MODEL ** -0.5)
    hy_conv_w = nrm((N_ODD, HY_CONV, 3 * D_MODEL), HY_CONV ** -0.5)
    w_out_c = nrm((N_ODD, D_MODEL, D_MODEL), D_MODEL ** -0.5)
    hy_w1 = nrm((N_ODD, HY_EMB, HY_FFN), HY_EMB ** -0.5)
    hy_b1 = nrm((N_ODD, HY_FFN), 0.02)
    hy_freq = gain((N_ODD, HY_FFN))
    hy_w2 = nrm((N_ODD, HY_FFN, HY_FFN), HY_FFN ** -0.5)
    hy_b2 = nrm((N_ODD, HY_FFN), 0.02)
    hy_w3 = nrm((N_ODD, HY_FFN, 2 * D_MODEL), HY_FFN ** -0.5)
    hy_bias = nrm((N_ODD, D_MODEL), 0.5)
    return {
        'x_prompt': x_prompt, 'x_sample': x_sample, 'c': c, 'state_ret': state_ret, 'state_gdn': state_gdn,
        'c_ctx': c_ctx, 'w_mod': w_mod, 'b_mod': b_mod,
        'norm_mix_pre': norm_mix_pre, 'norm_mix_post': norm_mix_post,
        'norm_ffn_pre': norm_ffn_pre, 'norm_ffn_post': norm_ffn_post,
        'w_ffn_gate': w_ffn_gate, 'w_ffn_up': w_ffn_up, 'w_ffn_down': w_ffn_down,
        'w_in_ab': w_in_ab, 'w_out_ab': w_out_ab, 'ret_decay': ret_decay, 'ret_gn_w': ret_gn_w,
        'gdn_conv_w': gdn_conv_w, 'gdn_a_log': gdn_a_log, 'gdn_dt_bias': gdn_dt_bias, 'gdn_norm_w': gdn_norm_w,
        'w_in_c': w_in_c, 'hy_conv_w': hy_conv_w, 'w_out_c': w_out_c,
        'hy_w1': hy_w1, 'hy_b1': hy_b1, 'hy_freq': hy_freq, 'hy_w2': hy_w2, 'hy_b2': hy_b2,
        'hy_w3': hy_w3, 'hy_bias': hy_bias,
    }


def reference(x_prompt, x_sample, c, state_ret, state_gdn, c_ctx, w_mod, b_mod,
              norm_mix_pre, norm_mix_post, norm_ffn_pre, norm_ffn_post,
              w_ffn_gate, w_ffn_up, w_ffn_down,
              w_in_ab, w_out_ab, ret_decay, ret_gn_w,
              gdn_conv_w, gdn_a_log, gdn_dt_bias, gdn_norm_w,
              w_in_c, hy_conv_w, w_out_c,
              hy_w1, hy_b1, hy_freq, hy_w2, hy_b2, hy_w3, hy_bias):
    p = {
        'w_mod': w_mod, 'b_mod': b_mod,
        'norm_mix_pre': norm_mix_pre, 'norm_mix_post': norm_mix_post,
        'norm_ffn_pre': norm_ffn_pre, 'norm_ffn_post': norm_ffn_post,
        'w_ffn_gate': w_ffn_gate, 'w_ffn_up': w_ffn_up, 'w_ffn_down': w_ffn_down,
        'w_in_ab': w_in_ab, 'w_out_ab': w_out_ab, 'ret_decay': ret_decay, 'ret_gn_w': ret_gn_w,
        'gdn_conv_w': gdn_conv_w, 'gdn_a_log': gdn_a_log, 'gdn_dt_bias': gdn_dt_bias, 'gdn_norm_w': gdn_norm_w,
        'w_in_c': w_in_c, 'hy_conv_w': hy_conv_w, 'w_out_c': w_out_c,
        'hy_w1': hy_w1, 'hy_b1': hy_b1, 'hy_freq': hy_freq, 'hy_w2': hy_w2, 'hy_b2': hy_b2,
        'hy_w3': hy_w3, 'hy_bias': hy_bias,
    }
    nb = x_prompt.shape[0]
    zero_ret = jnp.zeros((nb, N_EVEN, 2, H_A, DK_A, DV_A), F32)
    zero_gdn = jnp.zeros((nb, N_EVEN, 2, H_B, DK_B, DV_B), F32)
    y_prompt, new_state_ret, new_state_gdn = trunk(x_prompt, c_ctx[None, :], zero_ret, zero_gdn, None, p)
    rope = rope_2d(x_sample.shape[1])
    y_sample, _, _ = trunk(x_sample, c, state_ret, state_gdn, rope, p)
    return (y_prompt, y_sample, new_state_ret, new_state_gdn)
```

```python
import math
from contextlib import ExitStack

import ml_dtypes
import numpy as np

import concourse.bass as bass
import concourse.mybir as mybir
from concourse.bass_utils import run_bass_kernel_spmd

F32 = mybir.dt.float32
BF16 = mybir.dt.bfloat16
AF = mybir.ActivationFunctionType
ALU = mybir.AluOpType
NPBF = ml_dtypes.bfloat16


class Cfg:
    D_MODEL = 2048
    BATCH = 16
    SEQ = 256
    DEPTH = 4
    DEC_BATCH = 8
    DEC_SEQ = 4096
    GRID_W = 64
    H_A = 4
    DK_A = 256
    DV_A = 512
    ROPE_BASE = 10000.0
    H_B = 8
    DK_B = 128
    DV_B = 128
    CONV_B = 5
    HY_CONV = 3
    HY_BANDS = 16
    HY_FFN = 64
    HY_FAST_DECAY = 0.3
    HY_SLOW_DECAY = 1.5
    HY_TARGET = 1e-2
    EPS = 1e-6
    N_CORES = 8

    def __init__(self, **kw):
        for k_, v in kw.items():
            setattr(self, k_, v)
        self.D_FF = ((8 * self.D_MODEL // 3 + 255) // 256) * 256
        self.HY_EMB = 1 + 2 * self.HY_BANDS
        self.N_EVEN = (self.DEPTH + 1) // 2
        self.N_ODD = self.DEPTH // 2
        self.SIZES_AB = (self.H_A * self.DK_A, self.H_A * self.DK_A, self.H_A * self.DV_A, self.H_A * self.DV_A,
                         2 * self.H_B * self.DK_B + self.H_B * self.DV_B, self.H_B * self.DV_B,
                         2 * self.H_B, 2 * self.H_B)
        self.PROJ_AB = sum(self.SIZES_AB)
        self.MIX_AB_OUT = self.H_A * self.DV_A + self.H_B * self.DV_B
        self.KC = self.D_MODEL // 128
        self.NPROMPT = self.BATCH // self.N_CORES
        self.T = self.DEC_SEQ + self.NPROMPT * self.SEQ
        self.SEGS = [(0, self.DEC_SEQ, 0)] + [(self.DEC_SEQ + i * self.SEQ, self.SEQ, 1) for i in range(self.NPROMPT)]


class Buf:
    __slots__ = ("t", "w", "r", "x")

    def __init__(self, t, excl=False):
        self.t = t
        self.x = excl
        self.w = []
        self.r = []

    def __getitem__(self, idx):
        return V(self, self.t[idx])


class V:
    __slots__ = ("buf", "ap")

    def __init__(self, buf, ap):
        self.buf = buf
        self.ap = ap

    def __getitem__(self, idx):
        return V(self.buf, self.ap[idx])

    def rearrange(self, *a, **kw):
        return V(self.buf, self.ap.rearrange(*a, **kw))

    def unsqueeze(self, *a):
        return V(self.buf, self.ap.unsqueeze(*a))

    def to_broadcast(self, *a):
        return V(self.buf, self.ap.to_broadcast(*a))

    def bitcast(self, *a):
        return V(self.buf, self.ap.bitcast(*a))


def _ap(x):
    return x.ap if isinstance(x, V) else x


def _bufs(xs):
    return [x.buf for x in xs if isinstance(x, V)]


class K:
    ENG = ("pe", "act", "dve", "pool", "sp")

    def __init__(self, nc):
        self.nc = nc
        self.es = ExitStack()
        self.E = {"pe": nc.tensor, "act": nc.scalar, "dve": nc.vector, "pool": nc.gpsimd, "sp": nc.sync}
        self.sem = {}
        self.cnt = {}
        self.semid = {}
        self.allsems = []
        for e in self.ENG:
            s = self.es.enter_context(nc.semaphore("s_" + e))
            self.sem[e] = s
            self.cnt[e] = 0
            self.allsems.append(s)
        self.dsem = {}
        self.dval = {}
        self.dnext = {}
        for q, n in (("sp", 16), ("pool", 16), ("act", 8)):
            self.dsem[q] = [self.es.enter_context(nc.semaphore(f"d_{q}{i}")) for i in range(n)]
            self.dval[q] = [0] * n
            self.dnext[q] = 0
            self.allsems += self.dsem[q]
        self.val = {id(s): 0 for s in self.allsems}
        self.seen = {e: {} for e in self.ENG}
        self.nuniq = 0
        self.ninst = 0

    def _wait(self, en, ev):
        sem, val = ev
        sid = id(sem)
        sd = self.seen[en]
        if sd.get(sid, 0) >= val:
            return
        sd[sid] = val
        self.E[en].wait_ge(sem, val)

    def _deps(self, en, reads, writes):
        own = id(self.sem[en])
        for b in reads:
            for ev in b.w:
                if en == "pe" and id(ev[0]) == own:
                    continue
                self._wait(en, ev)
            if b.x:
                for ev in b.r:
                    if id(ev[0]) != own:
                        self._wait(en, ev)
        for b in writes:
            for ev in b.w:
                if id(ev[0]) == own:
                    continue
                self._wait(en, ev)
            for ev in b.r:
                if id(ev[0]) == own:
                    continue
                self._wait(en, ev)

    def _mark(self, ev, reads, writes):
        for b in reads:
            sid = id(ev[0])
            b.r = [e for e in b.r if id(e[0]) != sid]
            b.r.append(ev)
        for b in writes:
            b.w = [ev]
            b.r = []

    def do(self, en, fn, reads=(), writes=()):
        rb, wb = _bufs(reads), _bufs(writes)
        self._deps(en, rb, wb)
        ins = fn(self.E[en])
        self.cnt[en] += 1
        ins.then_inc(self.sem[en], 1)
        ev = (self.sem[en], self.cnt[en])
        self.val[id(self.sem[en])] = self.cnt[en]
        self._mark(ev, rb, wb)
        self.ninst += 1
        return ev

    def dma(self, q, out, in_):
        rb, wb = _bufs([in_]), _bufs([out])
        j = self.dnext[q]
        self.dnext[q] = (j + 1) % len(self.dsem[q])
        sem = self.dsem[q][j]
        self._wait(q, (sem, self.dval[q][j]))
        self._deps(q, rb, wb)
        self.E[q].dma_start(out=_ap(out), in_=_ap(in_)).then_inc(sem, 16)
        self.dval[q][j] += 16
        ev = (sem, self.dval[q][j])
        self.val[id(sem)] = self.dval[q][j]
        self._mark(ev, rb, wb)
        self.ninst += 1
        return ev

    def barrier(self, engines=None):
        for en in (engines or self.ENG):
            for s in self.allsems:
                if s is self.sem[en]:
                    continue
                v = self.val[id(s)]
                if v:
                    self._wait(en, (s, v))

    class Scope:
        def __init__(self, k):
            self.k = k
            self.es = ExitStack()

        def sb(self, shape, dt, name=None):
            self.k.nuniq += 1
            t = self.es.enter_context(self.k.nc.sbuf_tensor(name or f"t{self.k.nuniq}", list(shape), dt))
            return Buf(t)

        def ps(self, shape, dt=F32, name=None):
            self.k.nuniq += 1
            t = self.es.enter_context(self.k.nc.psum_tensor(name or f"p{self.k.nuniq}", list(shape), dt))
            return Buf(t, excl=True)

        def __enter__(self):
            return self

        def __exit__(self, *a):
            if a[0] is None:
                self.k.barrier()
            self.es.close()
            return False

    def scope(self):
        return K.Scope(self)

    def mm(self, out, lhsT, rhs, start=True, stop=True):
        return self.do("pe", lambda e: e.matmul(_ap(out), lhsT=_ap(lhsT), rhs=_ap(rhs), start=start, stop=stop),
                       reads=(lhsT, rhs), writes=(out,))

    def tr(self, out, in_, ident):
        return self.do("pe", lambda e: e.transpose(_ap(out), _ap(in_), _ap(ident)), reads=(in_, ident), writes=(out,))

    def act(self, out, in_, func, bias=None, scale=None, en="act"):
        kw = {}
        rd = [in_]
        if bias is not None:
            kw["bias"] = _ap(bias)
            rd.append(bias)
        if scale is not None:
            kw["scale"] = _ap(scale)
            rd.append(scale)
        return self.do("act", lambda e: e.activation(out=_ap(out), in_=_ap(in_), func=func, **kw),
                       reads=rd, writes=(out,))

    def tt(self, out, in0, in1, op, en="dve"):
        return self.do(en, lambda e: e.tensor_tensor(out=_ap(out), in0=_ap(in0), in1=_ap(in1), op=op),
                       reads=(in0, in1), writes=(out,))

    def ts(self, out, in0, s1, s2=None, op0=ALU.mult, op1=None, en="dve"):
        kw = {}
        if op1 is not None:
            kw["op1"] = op1
        return self.do(en, lambda e: e.tensor_scalar(out=_ap(out), in0=_ap(in0), scalar1=_ap(s1), scalar2=_ap(s2),
                                                     op0=op0, **kw),
                       reads=(in0, s1, s2), writes=(out,))

    def stt(self, out, in0, scalar, in1, op0, op1):
        return self.do("dve", lambda e: e.scalar_tensor_tensor(out=_ap(out), in0=_ap(in0), scalar=_ap(scalar),
                                                               in1=_ap(in1), op0=op0, op1=op1),
                       reads=(in0, scalar, in1), writes=(out,))

    def copy(self, out, in_, en="dve"):
        if en == "act":
            return self.do("act", lambda e: e.copy(out=_ap(out), in_=_ap(in_)), reads=(in_,), writes=(out,))
        return self.do(en, lambda e: e.tensor_copy(out=_ap(out), in_=_ap(in_)), reads=(in_,), writes=(out,))

    def recip(self, out, in_):
        return self.do("dve", lambda e: e.reciprocal(out=_ap(out), in_=_ap(in_)), reads=(in_,), writes=(out,))

    def memset(self, out, val, en="dve"):
        return self.do(en, lambda e: e.memset(_ap(out), val), writes=(out,))


class Prog:
    def __init__(self, cfg):
        self.cfg = cfg
        self.nc = bass.Bass("TRN2", target_bir_lowering=False)
        self.k = K(self.nc)
        self.dr = {}

    def din(self, name, shape, dt=F32):
        self.dr[name] = self.nc.dram_tensor(name, list(shape), dt, kind="ExternalInput").ap()
        return self.dr[name]

    def dout(self, name, shape, dt=F32):
        self.dr[name] = self.nc.dram_tensor(name, list(shape), dt, kind="ExternalOutput").ap()
        return self.dr[name]

    def dtmp(self, name, shape, dt=F32):
        kind = "ExternalOutput" if name in getattr(self.cfg, "DEBUG_OUT", ()) else "Internal"
        self.dr[name] = self.nc.dram_tensor(name, list(shape), dt, kind=kind).ap()
        return self.dr[name]

    def load_fm_vec(self, s, dst, src, n):
        k = self.k
        if not hasattr(s, "_fm_tmp"):
            s._fm_tmp = s.sb([128, 128], F32)
            s._fm_ps = s.ps([128, 128], F32)
        tmp, ps = s._fm_tmp, s._fm_ps
        k.dma("sp", tmp[0:n, :], src.rearrange("(c p) -> c p", p=128))
        k.tr(ps[:, 0:n], tmp[0:n, :], self.ident[0:n, 0:n])
        k.copy(dst, ps[:, 0:n])

    def rstd_from_ss(self, s, r, ps, n_feat, width):
        k = self.k
        k.ts(r, ps, 1.0 / n_feat, self.cfg.EPS, op0=ALU.mult, op1=ALU.add)
        k.act(r, r, AF.Sqrt)
        k.recip(r, r)

    def phase_consts(self, S):
        k, cfg = self.k, self.cfg
        self.ident_b = S.sb([128, 128], F32)
        k.dma("sp", self.ident_b[:, :], self.dr["c_ident"])
        self.ident = self.ident_b[:, :]
        self.identh_b = S.sb([128, 128], BF16)
        k.dma("pool", self.identh_b[:, :], self.dr["c_ident"])
        self.identh = self.identh_b[:, :]
        self.ones_b = S.sb([128, 128], BF16)
        k.memset(self.ones_b[:, :], 1.0)
        self.ones = self.ones_b[:, :]

    def phase_mod(self, S):
        k, cfg = self.k, self.cfg
        KC, D = cfg.KC, cfg.D_MODEL
        NL = cfg.DEPTH
        self.modv = S.sb([128, NL * 6 * KC * 2], F32)
        mv = self.modv

        def mvs(l, j, kc, c0=0, c1=2):
            base = ((l * 6 + j) * KC + kc) * 2
            return mv[:, base + c0:base + c1]
        self.mvs = mvs
        with k.scope() as s:
            cond = s.sb([2, D], F32)
            k.dma("sp", cond[:, :], self.dr["cond"])
            k.act(cond[:, :], cond[:, :], AF.Silu)
            scT = s.sb([128, KC, 2], BF16)
            pst = s.ps([128, 512], F32)
            for kc in range(KC):
                k.tr(pst[:, 2 * kc:2 * kc + 2], cond[:, kc * 128:(kc + 1) * 128], self.ident[0:2, 0:2])
            k.copy(scT[:, :, :], pst[:, 0:2 * KC].rearrange("p (c t) -> p c t", t=2))
            nw = s.sb([128, 4, KC], F32)
            raw = s.sb([128, 6 * KC, 2], F32)
            bm = s.sb([128, 6 * KC], F32)
            CB = 512 if 6 * D >= 512 else 6 * D
            wbufs = [s.sb([128, KC, CB], BF16) for _ in range(2)]
            pss = [s.ps([128, 512], F32) for _ in range(2)]
            for l in range(NL):
                for i, nm in enumerate(("norm_mix_pre", "norm_mix_post", "norm_ffn_pre", "norm_ffn_post")):
                    self.load_fm_vec(s, nw[:, i, :], self.dr[nm][l], KC)
                self.load_fm_vec(s, bm[:, :], self.dr["b_mod"][l], 6 * KC)
                wsrc = self.dr["w_mod"][l].rearrange("(c p) n -> p c n", p=128)
                for cb in range(6 * D // CB):
                    wb = wbufs[cb % 2]
                    k.dma("pool", wb[:, :, :], wsrc[:, :, cb * CB:(cb + 1) * CB])
                    ps = pss[cb % 2]
                    nch = CB // 128
                    for m in range(nch):
                        for kc in range(KC):
                            k.mm(ps[:, 2 * m:2 * m + 2], wb[:, kc, m * 128:(m + 1) * 128], scT[:, kc, :],
                                 start=(kc == 0), stop=(kc == KC - 1))
                    ch0 = cb * nch
                    k.tt(raw[:, ch0:ch0 + nch, :], ps[:, 0:2 * nch].rearrange("p (c t) -> p c t", t=2),
                         bm[:, ch0:ch0 + nch].unsqueeze(2).to_broadcast([128, nch, 2]), ALU.add)
                for kc in range(KC):
                    for half, (npre, npost) in enumerate(((0, 1), (2, 3))):
                        sh = raw[:, (3 * half + 0) * KC + kc, :]
                        sc = raw[:, (3 * half + 1) * KC + kc, :]
                        g = raw[:, (3 * half + 2) * KC + kc, :]
                        k.ts(mvs(l, 3 * half + 0, kc), sc, 1.0, nw[:, npre, kc:kc + 1], op0=ALU.add, op1=ALU.mult)
                        k.copy(mvs(l, 3 * half + 1, kc), sh)
                        k.ts(mvs(l, 3 * half + 2, kc), g, nw[:, npost, kc:kc + 1], None, op0=ALU.mult)

    def phase_in(self):
        k, cfg = self.k, self.cfg
        KC, D, T = cfg.KC, cfg.D_MODEL, cfg.T
        xT = self.dr["xT"].rearrange("(c p) t -> p c t", p=128)
        with k.scope() as s:
            xin = [s.sb([128, D], F32) for _ in range(2)]
            xo = [s.sb([128, KC, 512], F32) for _ in range(2)]
            pss = [s.ps([128, 512], F32) for _ in range(4)]
            n = 0
            for t0 in range(0, T, 512):
                ob = xo[(t0 // 512) % 2]
                for tt in range(4):
                    ib = xin[n % 2]
                    k.dma("sp", ib[:, :], self.dr["x_in"][t0 + tt * 128:t0 + (tt + 1) * 128, :])
                    for g in range(KC // 4 if KC >= 4 else 1):
                        ps = pss[n % 4]
                        nn = min(4, KC)
                        for i in range(nn):
                            kc = g * 4 + i
                            k.tr(ps[:, i * 128:(i + 1) * 128], ib[:, kc * 128:(kc + 1) * 128], self.ident)
                        k.copy(ob[:, g * 4:g * 4 + nn, tt * 128:(tt + 1) * 128],
                               ps[:, 0:nn * 128].rearrange("p (c t) -> p c t", t=128),
                               en=("act" if n % 2 else "dve"))
                        n += 1
                k.dma("sp", xT[:, :, t0:t0 + 512], ob[:, :, :])

    def phase_out(self):
        k, cfg = self.k, self.cfg
        KC, D, T = cfg.KC, cfg.D_MODEL, cfg.T
        xT = self.dr["xT"].rearrange("(c p) t -> p c t", p=128)
        with k.scope() as s:
            xi = [s.sb([128, KC, 512], F32) for _ in range(2)]
            yo = [s.sb([128, D], F32) for _ in range(2)]
            pss = [s.ps([128, 512], F32) for _ in range(4)]
            n = 0
            m = 0
            for t0 in range(0, T, 512):
                ib = xi[(t0 // 512) % 2]
                k.dma("sp", ib[:, :, :], xT[:, :, t0:t0 + 512])
                for tt in range(4):
                    ob = yo[m % 2]
                    m += 1
                    for g in range(KC // 4 if KC >= 4 else 1):
                        ps = pss[n % 4]
                        nn = min(4, KC)
                        for i in range(nn):
                            kc = g * 4 + i
                            k.tr(ps[:, i * 128:(i + 1) * 128], ib[:, kc, tt * 128:(tt + 1) * 128], self.ident)
                        k.copy(ob[:, g * 512:g * 512 + nn * 128], ps[:, 0:nn * 128], en=("act" if n % 2 else "dve"))
                        n += 1
                    k.dma("sp", self.dr["y"][t0 + tt * 128:t0 + (tt + 1) * 128, :], ob[:, :])

    def phase_norm(self, l, half):
        k, cfg = self.k, self.cfg
        KC, D, T = cfg.KC, cfg.D_MODEL, cfg.T
        xT = self.dr["xT"].rearrange("(c p) t -> p c t", p=128)
        hT = self.dr["hT"].rearrange("(c p) t -> p c t", p=128)
        with k.scope() as s:
            xb = [s.sb([128, KC, 512], F32) for _ in range(2)]
            sq = [s.sb([128, KC, 512], BF16) for _ in range(2)]
            hb = [s.sb([128, KC, 512], BF16) for _ in range(2)]
            rr = [s.sb([128, 512], F32) for _ in range(2)]
            tm = [s.sb([128, 512], F32) for _ in range(3)]
            pss = [s.ps([128, 512], F32) for _ in range(2)]
            n = 0
            for (st, L, ci) in cfg.SEGS:
                for t0 in range(st, st + L, 512):
                    w = min(512, st + L - t0)
                    i2 = n % 2
                    x_, q_, h_, r_, ps = xb[i2], sq[i2], hb[i2], rr[i2], pss[i2]
                    k.dma("sp", x_[:, :, 0:w], xT[:, :, t0:t0 + w])
                    for kc in range(KC):
                        k.act(q_[:, kc, 0:w], x_[:, kc, 0:w], AF.Square)
                        k.mm(ps[:, 0:w], self.ones, q_[:, kc, 0:w], start=(kc == 0), stop=(kc == KC - 1))
                    self.rstd_from_ss(s, r_[:, 0:w], ps[:, 0:w], D, w)
                    for kc in range(KC):
                        t_ = tm[kc % 3]
                        k.stt(t_[:, 0:w], x_[:, kc, 0:w], self.mvs(l, 3 * half + 0, kc, ci, ci + 1), r_[:, 0:w],
                              ALU.mult, ALU.mult)
                        k.act(h_[:, kc, 0:w], t_[:, 0:w], AF.Identity, bias=self.mvs(l, 3 * half + 1, kc, ci, ci + 1))
                    k.dma("sp", hT[:, :, t0:t0 + w], h_[:, :, 0:w])
                    n += 1

    def phase_post(self, l, half):
        k, cfg = self.k, self.cfg
        KC, D, T = cfg.KC, cfg.D_MODEL, cfg.T
        xT = self.dr["xT"].rearrange("(c p) t -> p c t", p=128)
        yT = self.dr["yT"].rearrange("(c p) t -> p c t", p=128)
        with k.scope() as s:
            xb = [s.sb([128, KC, 512], F32) for _ in range(2)]
            yb = [s.sb([128, KC, 512], F32) for _ in range(2)]
            sq = [s.sb([128, KC, 512], BF16) for _ in range(2)]
            rr = [s.sb([128, 512], F32) for _ in range(2)]
            tm = [s.sb([128, 512], F32) for _ in range(3)]
            pss = [s.ps([128, 512], F32) for _ in range(2)]
            n = 0
            for (st, L, ci) in cfg.SEGS:
                for t0 in range(st, st + L, 512):
                    w = min(512, st + L - t0)
                    i2 = n % 2
                    x_, y_, q_, r_, ps = xb[i2], yb[i2], sq[i2], rr[i2], pss[i2]
                    k.dma("sp", y_[:, :, 0:w], yT[:, :, t0:t0 + w])
                    k.dma("sp", x_[:, :, 0:w], xT[:, :, t0:t0 + w])
                    for kc in range(KC):
                        k.act(q_[:, kc, 0:w], y_[:, kc, 0:w], AF.Square)
                        k.mm(ps[:, 0:w], self.ones, q_[:, kc, 0:w], start=(kc == 0), stop=(kc == KC - 1))
                    self.rstd_from_ss(s, r_[:, 0:w], ps[:, 0:w], D, w)
                    for kc in range(KC):
                        t_ = tm[kc % 3]
                        k.stt(t_[:, 0:w], y_[:, kc, 0:w], self.mvs(l, 3 * half + 2, kc, ci, ci + 1), r_[:, 0:w],
                              ALU.mult, ALU.mult)
                        k.tt(x_[:, kc, 0:w], x_[:, kc, 0:w], t_[:, 0:w], ALU.add, en="pool")
                    k.dma("sp", xT[:, :, t0:t0 + w], x_[:, :, 0:w])
                    n += 1

    def phase_ffn(self, l):
        k, cfg = self.k, self.cfg
        KC, D, T, FF = cfg.KC, cfg.D_MODEL, cfg.T, cfg.D_FF
        JC = FF // 128
        hT = self.dr["hT"].rearrange("(c p) t -> p c t", p=128)
        yT = self.dr["yT"].rearrange("(c p) t -> p c t", p=128)
        wg_src = self.dr["w_ffn_gate"][l].rearrange("(c p) n -> p c n", p=128)
        wu_src = self.dr["w_ffn_up"][l].rearrange("(c p) n -> p c n", p=128)
        wd_src = self.dr["w_ffn_down"][l].rearrange("(c p) n -> p c n", p=128)
        NG = 1024
        CB = 256
        groups = []
        for (st, L, ci) in cfg.SEGS:
            for t0 in range(st, st + L, NG):
                groups.append((t0, min(NG, st + L - t0)))
        merged = []
        for g in groups:
            if merged and merged[-1][1] + g[1] <= NG and merged[-1][0] + merged[-1][1] == g[0]:
                merged[-1] = (merged[-1][0], merged[-1][1] + g[1])
            else:
                merged.append(g)
        for (t0, w) in merged:
            nb = (w + 511) // 512
            with k.scope() as s:
                aT = s.sb([128, JC, w], BF16)
                with k.scope() as s1:
                    h_ = s1.sb([128, KC, w], BF16)
                    k.dma("sp", h_[:, :, :], hT[:, :, t0:t0 + w])
                    wgb = [s1.sb([128, KC, CB], BF16) for _ in range(2)]
                    wub = [s1.sb([128, KC, CB], BF16) for _ in range(2)]
                    sg = [s1.sb([128, 512], F32) for _ in range(2)]
                    pg = [s1.ps([128, 512], F32) for _ in range(2)]
                    pu = [s1.ps([128, 512], F32) for _ in range(2)]
                    n = 0
                    for jb in range(FF // CB):
                        wg, wu = wgb[jb % 2], wub[jb % 2]
                        k.dma("pool", wg[:, :, :], wg_src[:, :, jb * CB:(jb + 1) * CB])
                        k.dma("pool", wu[:, :, :], wu_src[:, :, jb * CB:(jb + 1) * CB])
                        for jj in range(CB // 128):
                            j = jb * (CB // 128) + jj
                            for b in range(nb):
                                c0, c1 = b * 512, min(w, (b + 1) * 512)
                                cw = c1 - c0
                                g_, u_, s_ = pg[n % 2], pu[n % 2], sg[n % 2]
                                n += 1
                                for kc in range(KC):
                                    k.mm(g_[:, 0:cw], wg[:, kc, jj * 128:(jj + 1) * 128], h_[:, kc, c0:c1],
                                         start=(kc == 0), stop=(kc == KC - 1))
                                for kc in range(KC):
                                    k.mm(u_[:, 0:cw], wu[:, kc, jj * 128:(jj + 1) * 128], h_[:, kc, c0:c1],
                                         start=(kc == 0), stop=(kc == KC - 1))
                                k.act(s_[:, 0:cw], g_[:, 0:cw], AF.Silu)
                                k.tt(aT[:, j, c0:c1], s_[:, 0:cw], u_[:, 0:cw], ALU.mult)
                with k.scope() as s2:
                    MB = 2
                    JB = 11 if JC % 11 == 0 else (JC if JC <= 12 else 6)
                    assert JC % JB == 0
                    wdb = [s2.sb([128, JB, MB * 128], BF16) for _ in range(3)]
                    yo = [s2.sb([128, MB, w], F32) for _ in range(2)]
                    pss = [s2.ps([128, 512], F32) for _ in range(8)]
                    nw_ = 0
                    for mg in range(KC // MB):
                        pb = (mg % 2) * 4
                        for jb in range(JC // JB):
                            wd = wdb[nw_ % 3]
                            nw_ += 1
                            k.dma("pool", wd[:, :, :], wd_src[:, jb * JB:(jb + 1) * JB, mg * MB * 128:(mg + 1) * MB * 128])
                            for jj in range(JB):
                                j = jb * JB + jj
                                for mm_ in range(MB):
                                    for b in range(nb):
                                        c0, c1 = b * 512, min(w, (b + 1) * 512)
                                        k.mm(pss[pb + mm_ * 2 + b][:, 0:c1 - c0], wd[:, jj, mm_ * 128:(mm_ + 1) * 128],
                                             aT[:, j, c0:c1], start=(j == 0), stop=(j == JC - 1))
                        yb = yo[mg % 2]
                        for mm_ in range(MB):
                            for b in range(nb):
                                c0, c1 = b * 512, min(w, (b + 1) * 512)
                                k.copy(yb[:, mm_, c0:c1], pss[pb + mm_ * 2 + b][:, 0:c1 - c0],
                                       en=("act" if (mm_ + b) % 2 else "dve"))
                        k.dma("sp", yT[:, mg * MB:(mg + 1) * MB, t0:t0 + w], yb[:, :, :])

    def build(self):
        cfg, k = self.cfg, self.k
        D, T, NL, FF = cfg.D_MODEL, cfg.T, cfg.DEPTH, cfg.D_FF
        self.din("x_in", [T, D])
        self.din("cond", [2, D])
        self.din("c_ident", [128, 128])
        self.din("w_mod", [NL, D, 6 * D])
        self.din("b_mod", [NL, 6 * D])
        for nm in ("norm_mix_pre", "norm_mix_post", "norm_ffn_pre", "norm_ffn_post"):
            self.din(nm, [NL, D])
        self.din("w_ffn_gate", [NL, D, FF])
        self.din("w_ffn_up", [NL, D, FF])
        self.din("w_ffn_down", [NL, FF, D])
        self.declare_mixer_io()
        self.dout("y", [T, D])
        self.dtmp("xT", [D, T])
        self.dtmp("hT", [D, T], BF16)
        self.dtmp("yT", [D, T])
        with k.scope() as S:
            self.phase_consts(S)
            self.phase_mod(S)
            self.phase_in()
            for l in range(NL):
                if getattr(cfg, "MIXERS", True):
                    self.phase_norm(l, 0)
                    if l % 2 == 0:
                        if getattr(cfg, "SKIP_AB", False):
                            continue_ = True
                        else:
                            self.mixer_ab(l // 2)
                            self.phase_post(l, 0)
                    else:
                        self.mixer_c(l // 2)
                        self.phase_post(l, 0)
                self.phase_norm(l, 1)
                self.phase_ffn(l)
                self.phase_post(l, 1)
            self.phase_out()
        k.es.close()
        return self.nc

    def declare_mixer_io(self):
        declare_mixer_c(self)
        declare_mixer_ab(self)

    def mixer_ab(self, e):
        mixer_ab_impl(self, e)

    def mixer_c(self, o):
        mixer_c_impl(self, o)


def host_consts(cfg):
    out = {"c_ident": np.eye(128, dtype=np.float32)}
    out.update(hy_host_consts(cfg))
    out.update(ab_host_consts(cfg))
    return out


def make_in_maps(cfg, inputs):
    consts = host_consts(cfg)
    maps = []
    for core in range(cfg.N_CORES):
        m = dict(consts)
        xs = inputs["x_sample"][core]
        xp = inputs["x_prompt"][core * cfg.NPROMPT:(core + 1) * cfg.NPROMPT].reshape(-1, cfg.D_MODEL)
        m["x_in"] = np.ascontiguousarray(np.concatenate([xs, xp], axis=0))
        m["cond"] = np.ascontiguousarray(np.stack([inputs["c"][core], inputs["c_ctx"]], axis=0))
        for nm in ("w_mod", "b_mod", "norm_mix_pre", "norm_mix_post", "norm_ffn_pre", "norm_ffn_post",
                   "w_ffn_gate", "w_ffn_up", "w_ffn_down") + MIXC_WEIGHTS + MIXAB_WEIGHTS:
            m[nm] = inputs[nm]
        ab_core_inputs(cfg, m, inputs, core)
        maps.append(m)
    return maps


def run(cfg, inputs, trace=False):
    prog = Prog(cfg)
    nc = prog.build()
    maps = make_in_maps(cfg, inputs)
    res = run_bass_kernel_spmd(nc, maps, core_ids=list(range(cfg.N_CORES)))
    ys = np.stack([r["y"][:cfg.DEC_SEQ] for r in res.results], axis=0)
    yp = np.concatenate([r["y"][cfg.DEC_SEQ:].reshape(cfg.NPROMPT, cfg.SEQ, cfg.D_MODEL) for r in res.results], axis=0)
    nsr = np.concatenate([r["nsr"] for r in res.results], axis=0)
    nsg = np.concatenate([r["nsg"] for r in res.results], axis=0)
    return (ys, yp, nsr, nsg), res, prog


_CACHE = {}


def kernel(**inputs):
    cfg = Cfg()
    inputs = {k_: np.asarray(v) for k_, v in inputs.items()}
    outs, res, prog = run(cfg, inputs)
    ys, yp, nsr, nsg = outs
    return (np.ascontiguousarray(yp, dtype=np.float32), np.ascontiguousarray(ys, dtype=np.float32),
            np.ascontiguousarray(nsr, dtype=np.float32), np.ascontiguousarray(nsg, dtype=np.float32))


def token_groups(cfg, NG=1024):
    groups = []
    for (st, L, ci) in cfg.SEGS:
        for t0 in range(st, st + L, NG):
            groups.append((t0, min(NG, st + L - t0)))
    merged = []
    for g in groups:
        if merged and merged[-1][1] + g[1] <= NG and merged[-1][0] + merged[-1][1] == g[0]:
            merged[-1] = (merged[-1][0], merged[-1][1] + g[1])
        else:
            merged.append(g)
    return merged


def proj_fm(P, w_dram, n_in, ncols, in_dram, out_dram, out_dt, groups, col0=0, out_row0=0, CB=256):
    k = P.k
    KCi = n_in // 128
    wsrc = w_dram.rearrange("(c p) n -> p c n", p=128)
    inT = in_dram.rearrange("(c p) t -> p c t", p=128)
    blocks = []
    c = 0
    while c < ncols:
        bw = min(CB, ncols - c)
        blocks.append((c, bw))
        c += bw
    for (t0, w) in groups:
        nb = (w + 511) // 512
        with k.scope() as s:
            h_ = s.sb([128, KCi, w], BF16)
            k.dma("sp", h_[:, :, :], inT[:, :, t0:t0 + w])
            wbs = [s.sb([128, KCi, CB], BF16) for _ in range(2)]
            obs = [s.sb([128, CB // 128, w], out_dt) for _ in range(2)]
            pss = [s.ps([128, 512], F32) for _ in range(4)]
            n = 0
            for bi, (c0, bw) in enumerate(blocks):
                wb, ob = wbs[bi % 2], obs[bi % 2]
                k.dma("pool", wb[:, :, 0:bw], wsrc[:, :, col0 + c0:col0 + c0 + bw])
                nch = (bw + 127) // 128
                for jj in range(nch):
                    m = min(128, bw - jj * 128)
                    for b in range(nb):
                        a0, a1 = b * 512, min(w, (b + 1) * 512)
                        ps = pss[n % 4]
                        for kc in range(KCi):
                            k.mm(ps[0:m, 0:a1 - a0], wb[:, kc, jj * 128:jj * 128 + m], h_[:, kc, a0:a1],
                                 start=(kc == 0), stop=(kc == KCi - 1))
                        k.copy(ob[0:m, jj, a0:a1], ps[0:m, 0:a1 - a0], en=("act" if n % 2 else "dve"))
                        n += 1
                r0 = out_row0 + c0
                if bw % 128 == 0:
                    k.dma("sp", out_dram[r0:r0 + bw, t0:t0 + w].rearrange("(c p) t -> p c t", p=128), ob[:, 0:nch, :])
                else:
                    assert bw < 128
                    k.dma("sp", out_dram[r0:r0 + bw, t0:t0 + w], ob[0:bw, 0, :])


def hy_sizes(L):
    N = 2 * L
    NK = L // 128
    NFC = (L + 1 + 127) // 128
    return N, NK, NFC


def hy_host_consts(cfg):
    out = {}
    for L in sorted({cfg.DEC_SEQ, cfg.SEQ}):
        N, NK, NFC = hy_sizes(L)
        FP = NFC * 128
        n = np.arange(L, dtype=np.float64)
        f = np.arange(FP, dtype=np.float64)
        ang = 2.0 * np.pi * ((n[:, None] * f[None, :]) % N) / N
        valid = (f <= L)[None, :]
        C = np.where(valid, np.cos(ang), 0.0)
        S = np.where(valid, -np.sin(ang), 0.0)
        fw = np.stack([C, S]).reshape(2, NK, 128, NFC, 128).transpose(0, 3, 2, 1, 4)
        out[f"hy_dft_{L}"] = np.ascontiguousarray(fw).astype(NPBF)
        wf = np.where((f == 0) | (f == L), 1.0, 2.0) * (f <= L) / N
        Ci = (wf[:, None] * np.cos(ang.T))
        Si = (-wf[:, None] * np.sin(ang.T))
        nbw = min(512, L)
        iv = np.stack([Ci, Si]).reshape(2, NFC, 128, L // nbw, nbw).transpose(3, 2, 0, 1, 4)
        out[f"hy_idft_{L}"] = np.ascontiguousarray(iv).astype(NPBF)
        t = np.linspace(0.0, 1.0, L, dtype=np.float32).astype(np.float64)
        w_ang = 2.0 * math.pi * np.arange(L, dtype=np.float32).astype(np.float64) / L
        bands = np.linspace(1e-4, cfg.HY_BANDS - 1, cfg.HY_BANDS, dtype=np.float32).astype(np.float64)
        z = np.concatenate([t[:, None], np.cos(bands[None] * w_ang[:, None]), -np.sin(bands[None] * w_ang[:, None])], -1)
        out[f"hy_zT_{L}"] = np.ascontiguousarray(z.T).astype(np.float32)
        out[f"hy_negt_{L}"] = np.ascontiguousarray((-t).reshape(NK, 128).T).astype(np.float32)
    D = cfg.D_MODEL
    deltas = np.abs(np.linspace(math.log(cfg.HY_TARGET) / cfg.HY_SLOW_DECAY, math.log(cfg.HY_TARGET) / cfg.HY_FAST_DECAY,
                                D, dtype=np.float32))
    out["hy_deltas"] = np.ascontiguousarray(np.broadcast_to(deltas[None, :], (128, D))).astype(np.float32)
    return out


def _range_reduce(k, y, m):
    PI = math.pi
    for _ in range(2):
        k.ts(m, y, -PI, 2.0 * PI, op0=ALU.is_lt, op1=ALU.mult)
        k.tt(y, y, m, ALU.add)
        k.ts(m, y, PI, -2.0 * PI, op0=ALU.is_gt, op1=ALU.mult)
        k.tt(y, y, m, ALU.add)


def hy_filters(P, o, L):
    k, cfg = P.k, P.cfg
    D, HF, EMB = cfg.D_MODEL, cfg.HY_FFN, cfg.HY_EMB
    N, NK, NFC = hy_sizes(L)
    BW = min(512, L)
    with k.scope() as s:
        w1 = s.sb([EMB, HF], F32)
        k.dma("sp", w1[:, :], P.dr["hy_w1"][o])
        w2 = s.sb([HF, HF], F32)
        k.dma("sp", w2[:, :], P.dr["hy_w2"][o])
        w3 = s.sb([HF, 2 * D], F32)
        k.dma("sp", w3[:, :], P.dr["hy_w3"][o])
        zT = s.sb([EMB, L], F32)
        k.dma("sp", zT[:, :], P.dr[f"hy_zT_{L}"])
        vec = s.sb([HF, 4], F32)
        k.dma("sp", vec[:, 0:1], P.dr["hy_b1"][o].rearrange("(p o) -> p o", o=1))
        k.dma("sp", vec[:, 1:2], P.dr["hy_freq"][o].rearrange("(p o) -> p o", o=1))
        k.dma("sp", vec[:, 2:3], P.dr["hy_b2"][o].rearrange("(p o) -> p o", o=1))
        fb = s.sb([HF, 2], F32)
        k.tt(fb[:, 0:1], vec[:, 0:1], vec[:, 1:2], ALU.mult)
        k.tt(fb[:, 1:2], vec[:, 2:3], vec[:, 1:2], ALU.mult)
        negt = s.sb([128, NK], F32)
        k.dma("sp", negt[:, :], P.dr[f"hy_negt_{L}"])
        dl = s.sb([128, D], F32)
        k.dma("sp", dl[:, :], P.dr["hy_deltas"])
        bias = s.sb([1, D], F32)
        k.dma("sp", bias[:, :], P.dr["hy_bias"][o].rearrange("(o n) -> o n", o=1))
        h1 = s.sb([HF, L], F32)
        h2 = s.sb([HF, L], F32)
        mk = s.sb([HF, BW], F32)
        ps = s.ps([128, 512], F32)
        for (src, wgt, kdim, dst, fbi) in ((zT, w1, EMB, h1, 0), (h1, w2, HF, h2, 1)):
            for b0 in range(0, L, BW):
                k.mm(ps[0:HF, 0:BW], wgt[0:kdim, :], src[0:kdim, b0:b0 + BW])
                y = dst[:, b0:b0 + BW]
                k.ts(y, ps[0:HF, 0:BW], vec[:, 1:2], fb[:, fbi:fbi + 1], op0=ALU.mult, op1=ALU.add)
                _range_reduce(k, y, mk[:, :])
                k.act(y, y, AF.Sin)
        win = s.sb([128, D], F32)
        hf = s.sb([128, D], F32)
        hb = s.sb([128, D], F32)
        At = [s.sb([128, D], BF16) for _ in range(2)]
        Bt = [s.sb([128, D], BF16) for _ in range(2)]
        pss = [s.ps([128, 512], F32) for _ in range(2)]
        CW = min(512, D)
        n = 0
        for nk in range(NK):
            k.act(win[:, :], dl[:, :], AF.Exp, scale=negt[:, nk:nk + 1])
            for half, dst in ((0, hf), (1, hb)):
                for c0 in range(0, D, CW):
                    p_ = pss[n % 2]
                    n += 1
                    k.mm(p_[:, 0:CW], h2[0:HF, nk * 128:(nk + 1) * 128], w3[0:HF, half * D + c0:half * D + c0 + CW])
                    k.tt(dst[:, c0:c0 + CW], p_[:, 0:CW], win[:, c0:c0 + CW], ALU.mult)
            if nk == 0:
                k.memset(hb[0:1, :], 0.0)
                k.tt(hf[0:1, :], hf[0:1, :], bias[0:1, :], ALU.add)
            a_, b_ = At[nk % 2], Bt[nk % 2]
            k.tt(a_[:, :], hf[:, :], hb[:, :], ALU.add, en="pool")
            k.tt(b_[:, :], hf[:, :], hb[:, :], ALU.subtract, en="pool")
            k.dma("sp", P.dr["hy_A"][nk * 128:(nk + 1) * 128, :], a_[:, :])
            k.dma("sp", P.dr["hy_B"][nk * 128:(nk + 1) * 128, :], b_[:, :])


def hy_fwd(P, L, srcA, srcB, emit):
    k, cfg = P.k, P.cfg
    D = cfg.D_MODEL
    N, NK, NFC = hy_sizes(L)
    CW = min(512, D)
    dft = P.dr[f"hy_dft_{L}"]
    for c0 in range(0, D, CW):
        with k.scope() as s:
            A_ = s.sb([128, NK, CW], BF16)
            k.dma("sp", A_[:, :, :], srcA[:, c0:c0 + CW].rearrange("(k p) c -> p k c", p=128))
            if srcB is srcA:
                B_ = A_
            else:
                B_ = s.sb([128, NK, CW], BF16)
                k.dma("sp", B_[:, :, :], srcB[:, c0:c0 + CW].rearrange("(k p) c -> p k c", p=128))
            dcs = [s.sb([128, NK, 128], BF16) for _ in range(2)]
            dss = [s.sb([128, NK, 128], BF16) for _ in range(2)]
            pre = [s.ps([128, 512], F32) for _ in range(2)]
            pim = [s.ps([128, 512], F32) for _ in range(2)]
            for fc in range(NFC):
                dc, ds = dcs[fc % 2], dss[fc % 2]
                k.dma("sp", dc[:, :, :], dft[0, fc])
                k.dma("sp", ds[:, :, :], dft[1, fc])
                p_re, p_im = pre[fc % 2], pim[fc % 2]
                for nk in range(NK):
                    k.mm(p_re[:, 0:CW], dc[:, nk, :], A_[:, nk, :], start=(nk == 0), stop=(nk == NK - 1))
                for nk in range(NK):
                    k.mm(p_im[:, 0:CW], ds[:, nk, :], B_[:, nk, :], start=(nk == 0), stop=(nk == NK - 1))
                emit(s, fc, c0, CW, p_re, p_im)


def hy_filter_spectrum(P, L):
    k = P.k
    st = {}

    def emit(s, fc, c0, CW, p_re, p_im):
        if "s" not in st or st["s"] is not s:
            st["s"] = s
            st["o"] = [s.sb([128, 2, CW], F32) for _ in range(2)]
        ob = st["o"][fc % 2]
        k.copy(ob[:, 0, :], p_re[:, 0:CW], en="dve")
        k.copy(ob[:, 1, :], p_im[:, 0:CW], en="act")
        k.dma("sp", P.dr["hy_H"][:, fc * 128:(fc + 1) * 128, c0:c0 + CW].rearrange("s p c -> p s c"), ob[:, :, :])
    hy_fwd(P, L, P.dr["hy_A"][0:L, :], P.dr["hy_B"][0:L, :], emit)


def hy_data_spectrum(P, L, tok0):
    k = P.k
    st = {}

    def emit(s, fc, c0, CW, p_re, p_im):
        if "s" not in st or st["s"] is not s:
            st["s"] = s
            st["h"] = [s.sb([128, 2, CW], F32) for _ in range(2)]
            st["t"] = [s.sb([128, 4, CW], F32) for _ in range(2)]
            st["p"] = [s.sb([128, 2, CW], BF16) for _ in range(2)]
        hb, tb, pb = st["h"][fc % 2], st["t"][fc % 2], st["p"][fc % 2]
        k.dma("sp", hb[:, :, :], P.dr["hy_H"][:, fc * 128:(fc + 1) * 128, c0:c0 + CW].rearrange("s p c -> p s c"))
        k.tt(tb[:, 0, :], p_re[:, 0:CW], hb[:, 0, :], ALU.mult)
        k.tt(tb[:, 1, :], p_im[:, 0:CW], hb[:, 1, :], ALU.mult)
        k.tt(tb[:, 2, :], p_re[:, 0:CW], hb[:, 1, :], ALU.mult)
        k.tt(tb[:, 3, :], p_im[:, 0:CW], hb[:, 0, :], ALU.mult)
        k.tt(pb[:, 0, :], tb[:, 0, :], tb[:, 1, :], ALU.subtract, en="pool")
        k.tt(pb[:, 1, :], tb[:, 2, :], tb[:, 3, :], ALU.add, en="pool")
        k.dma("sp", P.dr["hy_P"][:, fc * 128:(fc + 1) * 128, c0:c0 + CW].rearrange("s p c -> p s c"), pb[:, :, :])
    src = P.dr["hy_wtok"][tok0:tok0 + L, :]
    hy_fwd(P, L, src, src, emit)


def hy_inverse(P, L, tok0):
    k, cfg = P.k, P.cfg
    D = cfg.D_MODEL
    N, NK, NFC = hy_sizes(L)
    FK = 2 * NFC
    PZ = 11 if FK % 11 == 0 else FK
    assert FK % PZ == 0 and PZ <= 12
    BWn = min(512, L)
    CW = min(512, D)
    NCH = CW // 128
    idft = P.dr[f"hy_idft_{L}"]
    for c0 in range(0, D, CW):
        with k.scope() as s:
            P_ = s.sb([128, 2, NFC, CW], BF16)
            for cs in range(2):
                k.dma("sp", P_[:, cs, :, :], P.dr["hy_P"][cs, 0:NFC * 128, c0:c0 + CW].rearrange("(c p) n -> p c n", p=128))
            idb = [s.sb([128, PZ, BWn], BF16) for _ in range(3)]
            x0b = [s.sb([128, NCH, BWn], BF16) for _ in range(2)]
            gb = [s.sb([128, NCH, BWn], BF16) for _ in range(2)]
            pss = [s.ps([128, 512], F32) for _ in range(8)]
            ni = 0
            for nb in range(L // BWn):
                pset = pss[(nb % 2) * 4:(nb % 2) * 4 + 4]
                xb, g_ = x0b[nb % 2], gb[nb % 2]
                t0 = tok0 + nb * BWn
                k.dma("sp", xb[:, :, :], P.dr["hy_x0"][c0:c0 + CW, t0:t0 + BWn].rearrange("(c p) t -> p c t", p=128))
                for pz in range(FK // PZ):
                    ib = idb[ni % 3]
                    ni += 1
                    k.dma("sp", ib[:, :, :], idft[nb].rearrange("p s c n -> p (s c) n")[:, pz * PZ:(pz + 1) * PZ, :])
                    for i in range(PZ):
                        fk = pz * PZ + i
                        cs, fc = fk // NFC, fk % NFC
                        for c in range(NCH):
                            k.mm(pset[c][:, 0:BWn], P_[:, cs, fc, c * 128:(c + 1) * 128], ib[:, i, :],
                                 start=(fk == 0), stop=(fk == FK - 1))
                for c in range(NCH):
                    k.tt(g_[:, c, :], pset[c][:, 0:BWn], xb[:, c, :], ALU.mult)
                k.dma("sp", P.dr["hy_gT"][c0:c0 + CW, t0:t0 + BWn].rearrange("(c p) t -> p c t", p=128), g_[:, :, :])


def hy_conv(P, o):
    k, cfg = P.k, P.cfg
    D = cfg.D_MODEL
    CW = min(512, D)
    NCH = CW // 128
    NC3 = 3 * D // 128
    TB = 512
    with k.scope() as s:
        cw = s.sb([128, 3, NC3], F32)
        for j in range(3):
            P.load_fm_vec(s, cw[:, j, :], P.dr["hy_conv_w"][o, j], NC3)
        us = [[s.sb([128, NCH, TB + 2], F32) for _ in range(3)] for _ in range(2)]
        cs_ = [s.sb([128, NCH, TB], F32) for _ in range(3)]
        x0o = [s.sb([128, NCH, TB], BF16) for _ in range(2)]
        wo = [s.sb([128, NCH, TB], BF16) for _ in range(2)]
        wt = [s.sb([128, CW], BF16) for _ in range(2)]
        psb = [s.ps([128, 512], BF16) for _ in range(2)]
        it = 0
        nt = 0
        for (st, L, ci) in cfg.SEGS:
            for t0 in range(st, st + L, TB):
                tw = min(TB, st + L - t0)
                lo = max(st, t0 - 1)
                hi = min(st + L, t0 + tw + 1)
                for c0 in range(0, D, CW):
                    u3 = us[it % 2]
                    for part in range(3):
                        ub = u3[part]
                        if lo == t0:
                            k.memset(ub[:, :, 0:1], 0.0, en="pool")
                        if hi == t0 + tw:
                            k.memset(ub[:, :, tw + 1:tw + 2], 0.0, en="pool")
                        r0 = part * D + c0
                        k.dma("sp", ub[:, :, lo - (t0 - 1):hi - (t0 - 1)],
                              P.dr["hy_uT"][r0:r0 + CW, lo:hi].rearrange("(c p) t -> p c t", p=128))
                        cb = cs_[part]
                        for c in range(NCH):
                            ch = (part * D + c0) // 128 + c
                            k.ts(cb[:, c, 0:tw], ub[:, c, 1:tw + 1], cw[:, 1, ch:ch + 1], None, op0=ALU.mult, en="pool")
                            k.stt(cb[:, c, 0:tw], ub[:, c, 0:tw], cw[:, 0, ch:ch + 1], cb[:, c, 0:tw], ALU.mult, ALU.add)
                            k.stt(cb[:, c, 0:tw], ub[:, c, 2:tw + 2], cw[:, 2, ch:ch + 1], cb[:, c, 0:tw], ALU.mult, ALU.add)
                    xo, w_ = x0o[it % 2], wo[it % 2]
                    k.copy(xo[:, :, 0:tw], cs_[0][:, :, 0:tw], en="act")
                    k.tt(w_[:, :, 0:tw], cs_[2][:, :, 0:tw], cs_[1][:, :, 0:tw], ALU.mult)
                    k.dma("sp", P.dr["hy_x0"][c0:c0 + CW, t0:t0 + tw].rearrange("(c p) t -> p c t", p=128), xo[:, :, 0:tw])
                    for j in range(tw // 128):
                        pb, wt_ = psb[nt % 2], wt[nt % 2]
                        nt += 1
                        for c in range(NCH):
                            k.tr(pb[:, c * 128:(c + 1) * 128], w_[:, c, j * 128:(j + 1) * 128], P.identh)
                        k.copy(wt_[:, 0:CW], pb[:, 0:CW], en=("act" if nt % 2 else "dve"))
                        k.dma("sp", P.dr["hy_wtok"][t0 + j * 128:t0 + (j + 1) * 128, c0:c0 + CW], wt_[:, 0:CW])
                    it += 1


def mixer_c_impl(P, o):
    k, cfg = P.k, P.cfg
    D = cfg.D_MODEL
    groups = token_groups(cfg)
    proj_fm(P, P.dr["w_in_c"][o], D, 3 * D, P.dr["hT"], P.dr["hy_uT"], F32, groups)
    hy_conv(P, o)
    done_L = None
    for (st, L, ci) in cfg.SEGS:
        if L != done_L:
            hy_filters(P, o, L)
            hy_filter_spectrum(P, L)
            done_L = L
        hy_data_spectrum(P, L, st)
        hy_inverse(P, L, st)
    proj_fm(P, P.dr["w_out_c"][o], D, D, P.dr["hy_gT"], P.dr["yT"], F32, groups)


def declare_mixer_c(P):
    cfg = P.cfg
    D, T, NO = cfg.D_MODEL, cfg.T, cfg.N_ODD
    P.din("w_in_c", [NO, D, 3 * D])
    P.din("hy_conv_w", [NO, 3, 3 * D])
    P.din("w_out_c", [NO, D, D])
    P.din("hy_w1", [NO, cfg.HY_EMB, cfg.HY_FFN])
    P.din("hy_b1", [NO, cfg.HY_FFN])
    P.din("hy_freq", [NO, cfg.HY_FFN])
    P.din("hy_w2", [NO, cfg.HY_FFN, cfg.HY_FFN])
    P.din("hy_b2", [NO, cfg.HY_FFN])
    P.din("hy_w3", [NO, cfg.HY_FFN, 2 * D])
    P.din("hy_bias", [NO, D])
    P.din("hy_deltas", [128, D])
    Lmax = max(cfg.DEC_SEQ, cfg.SEQ)
    for L in sorted({cfg.DEC_SEQ, cfg.SEQ}):
        N, NK, NFC = hy_sizes(L)
        P.din(f"hy_dft_{L}", [2, NFC, 128, NK, 128], BF16)
        P.din(f"hy_idft_{L}", [L // min(512, L), 128, 2, NFC, min(512, L)], BF16)
        P.din(f"hy_zT_{L}", [cfg.HY_EMB, L])
        P.din(f"hy_negt_{L}", [128, NK])
    FPmax = hy_sizes(Lmax)[2] * 128
    P.dtmp("hy_uT", [3 * D, T])
    P.dtmp("hy_x0", [D, T], BF16)
    P.dtmp("hy_wtok", [T, D], BF16)
    P.dtmp("hy_gT", [D, T], BF16)
    P.dtmp("hy_A", [Lmax, D], BF16)
    P.dtmp("hy_B", [Lmax, D], BF16)
    P.dtmp("hy_H", [2, FPmax, D])
    P.dtmp("hy_P", [2, FPmax, D], BF16)


MIXC_WEIGHTS = ("w_in_c", "hy_conv_w", "w_out_c", "hy_w1", "hy_b1", "hy_freq", "hy_w2", "hy_b2", "hy_w3", "hy_bias")


MIXAB_WEIGHTS = ("w_in_ab", "w_out_ab", "ret_decay", "ret_gn_w", "gdn_conv_w", "gdn_a_log", "gdn_dt_bias", "gdn_norm_w")
CH = 128
NEG = -30000.0


def ab_host_consts(cfg):
    out = {}
    i = np.arange(CH)
    s_, t_ = i[:, None], i[None, :]
    relf = np.where(t_ >= s_, (t_ - s_).astype(np.float32), 1e9)
    relb = np.where(s_ >= t_, (s_ - t_).astype(np.float32), 1e9)
    out["ab_rel"] = np.stack([relf, relb]).astype(np.float32)
    qd = np.stack([np.broadcast_to((i + 1.0)[None, :], (CH, CH)), np.broadcast_to((CH - i * 1.0)[None, :], (CH, CH))])
    out["ab_qexp"] = np.ascontiguousarray(qd).astype(np.float32)
    out["ab_kexp"] = np.stack([CH - 1.0 - i, i * 1.0], axis=1).astype(np.float32)
    Uf = (s_ <= t_).astype(np.float32)
    Ub = (s_ >= t_).astype(np.float32)
    out["ab_U"] = np.stack([Uf, Ub]).astype(np.float32)
    out["ab_negm"] = np.stack([np.where(s_ <= t_, 0.0, NEG), np.where(s_ >= t_, 0.0, NEG)]).astype(np.float32)
    out["ab_strict"] = np.stack([(s_ < t_), (s_ > t_)]).astype(np.float32)
    lm = np.zeros((2, 7, CH, CH), np.float32)
    for lv in range(7):
        m = 1 << lv
        tt_, ss_ = i[:, None], i[None, :]
        pat = ((tt_ // (2 * m)) == (ss_ // (2 * m))) & ((tt_ % (2 * m)) >= m) & ((ss_ % (2 * m)) < m)
        lm[0, lv] = -pat.astype(np.float32)
        lm[1, lv] = -pat.T.astype(np.float32)
    out["ab_lm"] = lm
    out["ab_lmT"] = np.ascontiguousarray(lm.transpose(0, 1, 3, 2))
    L = cfg.DEC_SEQ
    rows = L // cfg.GRID_W
    r = np.repeat(np.arange(rows, dtype=np.float32), cfg.GRID_W)
    col = (np.arange(rows * cfg.GRID_W) % cfg.GRID_W).astype(np.float32)
    n_freq = cfg.DK_A // 4
    inv = (np.float32(cfg.ROPE_BASE) ** (-np.arange(n_freq, dtype=np.float32) / n_freq)).astype(np.float32)
    ang = np.concatenate([r[:, None] * inv, col[:, None] * inv], axis=-1).astype(np.float32)
    out["ab_rope"] = np.ascontiguousarray(np.stack([np.cos(ang).T, np.sin(ang).T])).astype(np.float32)
    return out


def ab_core_inputs(cfg, m, inputs, core):
    m["state_ret"] = np.ascontiguousarray(inputs["state_ret"][core])
    m["state_gdn"] = np.ascontiguousarray(inputs["state_gdn"][core])


def declare_mixer_ab(P):
    cfg = P.cfg
    D, T, NE = cfg.D_MODEL, cfg.T, cfg.N_EVEN
    HA, DKA, DVA, HB, DKB, DVB = cfg.H_A, cfg.DK_A, cfg.DV_A, cfg.H_B, cfg.DK_B, cfg.DV_B
    P.din("w_in_ab", [NE, D, cfg.PROJ_AB])
    P.din("w_out_ab", [NE, cfg.MIX_AB_OUT, D])
    P.din("ret_decay", [NE, 2, HA])
    P.din("ret_gn_w", [NE, HA * DVA])
    P.din("gdn_conv_w", [NE, cfg.CONV_B, 3 * HB * DKB])
    P.din("gdn_a_log", [NE, 2, HB])
    P.din("gdn_dt_bias", [NE, 2, HB])
    P.din("gdn_norm_w", [NE, DVB])
    P.din("state_ret", [NE, 2, HA, DKA, DVA])
    P.din("state_gdn", [NE, 2, HB, DKB, DVB])
    P.din("ab_rel", [2, CH, CH])
    P.din("ab_qexp", [2, CH, CH])
    P.din("ab_kexp", [CH, 2])
    P.din("ab_U", [2, CH, CH])
    P.din("ab_negm", [2, CH, CH])
    P.din("ab_strict", [2, CH, CH])
    P.din("ab_lm", [2, 7, CH, CH])
    P.din("ab_lmT", [2, 7, CH, CH])
    P.din("ab_rope", [2, 128, cfg.DEC_SEQ])
    P.dout("nsr", [cfg.NPROMPT, NE, 2, HA, DKA, DVA])
    P.dout("nsg", [cfg.NPROMPT, NE, 2, HB, DKB, DVB])
    P.dtmp("ab_pT", [cfg.PROJ_AB, T])
    P.dtmp("ab_rqk", [2 * HA * DKA, T], BF16)
    P.dtmp("ab_rk_tok", [T, HA * DKA], BF16)
    P.dtmp("ab_rv_tok", [T, HA * DVA], BF16)
    P.dtmp("ab_rg_tok", [T, HA * DVA], BF16)
    P.dtmp("ab_gT", [3 * HB * DKB, T], BF16)
    P.dtmp("ab_gtok", [T, 2 * HB * DKB], BF16)
    P.dtmp("ab_gz_tok", [T, HB * DVB], BF16)
    P.dtmp("ab_gbT", [32, T])
    P.dtmp("ab_gb", [T, 32])
    P.dtmp("ab_o1r", [T, HA * DVA])
    P.dtmp("ab_o1g", [T, HB * DVB])
    P.dtmp("ab_catT", [cfg.MIX_AB_OUT, T], BF16)


def fm_to_tm(P, src, row0, nrows, dst, col0, src_dt, dst_dt, func=None):
    k, cfg = P.k, P.cfg
    T = cfg.T
    G = min(4, nrows // 128)
    ident = P.ident if src_dt == F32 else P.identh
    with k.scope() as s:
        ib = [s.sb([128, G, 512], src_dt) for _ in range(2)]
        ob = [s.sb([128, G * 128], dst_dt) for _ in range(3)]
        pss = [s.ps([128, 512], src_dt) for _ in range(3)]
        n = 0
        m = 0
        for t0 in range(0, T, 512):
            tw = min(512, T - t0)
            for r0 in range(0, nrows, G * 128):
                i_ = ib[n % 2]
                n += 1
                k.dma("sp", i_[:, :, 0:tw], src[row0 + r0:row0 + r0 + G * 128, t0:t0 + tw].rearrange("(c p) t -> p c t", p=128))
                for tj in range(tw // 128):
                    ps, o_ = pss[m % 3], ob[m % 3]
                    for g in range(G):
                        k.tr(ps[:, g * 128:(g + 1) * 128], i_[:, g, tj * 128:(tj + 1) * 128], ident)
                    if func is not None:
                        k.act(o_[:, :], ps[:, 0:G * 128], func)
                    else:
                        k.copy(o_[:, :], ps[:, 0:G * 128], en=("act" if m % 2 else "dve"))
                    m += 1
                    k.dma("sp", dst[t0 + tj * 128:t0 + (tj + 1) * 128, col0 + r0:col0 + r0 + G * 128], o_[:, :])


def ab_rope(P):
    k, cfg = P.k, P.cfg
    HA, DKA = cfg.H_A, cfg.DK_A
    qs = DKA ** -0.5
    with k.scope() as s:
        xs = [[s.sb([128, 512], F32) for _ in range(2)] for _ in range(2)]
        cs = [[s.sb([128, 512], F32) for _ in range(2)] for _ in range(2)]
        tm = [s.sb([128, 512], F32) for _ in range(4)]
        ob = [s.sb([128, 2, 512], BF16) for _ in range(2)]
        n = 0
        for si, (st, L, ci) in enumerate(cfg.SEGS):
            for t0 in range(st, st + L, 512):
                tw = min(512, st + L - t0)
                c_, s_ = cs[(t0 // 512) % 2]
                if si == 0:
                    k.dma("sp", c_[:, 0:tw], P.dr["ab_rope"][0, :, t0 - st:t0 - st + tw])
                    k.dma("sp", s_[:, 0:tw], P.dr["ab_rope"][1, :, t0 - st:t0 - st + tw])
                for qk in range(2):
                    sc = qs if qk == 0 else 1.0
                    for h in range(HA):
                        x1, x2 = xs[n % 2]
                        o_ = ob[n % 2]
                        n += 1
                        r0 = qk * HA * DKA + h * DKA
                        k.dma("sp", x1[:, 0:tw], P.dr["ab_pT"][r0:r0 + 128, t0:t0 + tw])
                        k.dma("sp", x2[:, 0:tw], P.dr["ab_pT"][r0 + 128:r0 + 256, t0:t0 + tw])
                        if si == 0:
                            k.tt(tm[0][:, 0:tw], x1[:, 0:tw], c_[:, 0:tw], ALU.mult)
                            k.tt(tm[1][:, 0:tw], x2[:, 0:tw], s_[:, 0:tw], ALU.mult, en="pool")
                            k.tt(tm[2][:, 0:tw], x1[:, 0:tw], s_[:, 0:tw], ALU.mult)
                            k.tt(tm[3][:, 0:tw], x2[:, 0:tw], c_[:, 0:tw], ALU.mult, en="pool")
                            k.stt(o_[:, 0, 0:tw], tm[0][:, 0:tw], sc, tm[1][:, 0:tw], ALU.mult, ALU.subtract) if sc == 1.0 else None
                            if sc != 1.0:
                                k.tt(tm[0][:, 0:tw], tm[0][:, 0:tw], tm[1][:, 0:tw], ALU.subtract)
                                k.ts(o_[:, 0, 0:tw], tm[0][:, 0:tw], sc, None, op0=ALU.mult)
                                k.tt(tm[2][:, 0:tw], tm[2][:, 0:tw], tm[3][:, 0:tw], ALU.add)
                                k.ts(o_[:, 1, 0:tw], tm[2][:, 0:tw], sc, None, op0=ALU.mult)
                            else:
                                k.tt(o_[:, 1, 0:tw], tm[2][:, 0:tw], tm[3][:, 0:tw], ALU.add)
                        else:
                            k.ts(o_[:, 0, 0:tw], x1[:, 0:tw], sc, None, op0=ALU.mult)
                            k.ts(o_[:, 1, 0:tw], x2[:, 0:tw], sc, None, op0=ALU.mult, en="pool")
                        k.dma("sp", P.dr["ab_rqk"][r0:r0 + 256, t0:t0 + tw].rearrange("(c p) t -> p c t", p=128), o_[:, :, 0:tw])


def ab_gdn_conv(P, e):
    k, cfg = P.k, P.cfg
    HB, DKB = cfg.H_B, cfg.DK_B
    NCH = 3 * HB * DKB // 128
    KT = cfg.CONV_B
    PD = KT // 2
    TB = 512
    row_base = 2 * cfg.H_A * cfg.DK_A + 2 * cfg.H_A * cfg.DV_A
    qs = DKB ** -0.5
    with k.scope() as s:
        cw = s.sb([128, KT, NCH], F32)
        for j in range(KT):
            P.load_fm_vec(s, cw[:, j, :], P.dr["gdn_conv_w"][e, j], NCH)
        ub = [s.sb([128, TB + 2 * PD], F32) for _ in range(3)]
        cb = [s.sb([128, TB], F32) for _ in range(2)]
        sq = [s.sb([128, TB], BF16) for _ in range(2)]
        rr = [s.sb([128, TB], F32) for _ in range(2)]
        ob = [s.sb([128, TB], BF16) for _ in range(3)]
        pss = [s.ps([128, 512], F32) for _ in range(2)]
        n = 0
        for (st, L, ci) in cfg.SEGS:
            for t0 in range(st, st + L, TB):
                tw = min(TB, st + L - t0)
                lo = max(st, t0 - PD)
                hi = min(st + L, t0 + tw + PD)
                for ch in range(NCH):
                    u_, c_, q_, r_, o_, ps = ub[n % 3], cb[n % 2], sq[n % 2], rr[n % 2], ob[n % 3], pss[n % 2]
                    n += 1
                    if lo > t0 - PD:
                        k.memset(u_[:, 0:PD], 0.0, en="pool")
                    if hi < t0 + tw + PD:
                        k.memset(u_[:, tw + PD:tw + 2 * PD], 0.0, en="pool")
                    r0 = row_base + ch * 128
                    k.dma("sp", u_[:, lo - (t0 - PD):hi - (t0 - PD)], P.dr["ab_pT"][r0:r0 + 128, lo:hi])
                    k.ts(c_[:, 0:tw], u_[:, 0:tw], cw[:, 0, ch:ch + 1], None, op0=ALU.mult, en="pool")
                    for j in range(1, KT):
                        k.stt(c_[:, 0:tw], u_[:, j:j + tw], cw[:, j, ch:ch + 1], c_[:, 0:tw], ALU.mult, ALU.add)
                    k.act(c_[:, 0:tw], c_[:, 0:tw], AF.Silu)
                    part = ch // (HB * DKB // 128)
                    if part < 2:
                        k.act(q_[:, 0:tw], c_[:, 0:tw], AF.Square)
                        k.mm(ps[:, 0:tw], P.ones, q_[:, 0:tw])
                        k.ts(r_[:, 0:tw], ps[:, 0:tw], cfg.EPS, None, op0=ALU.add)
                        k.act(r_[:, 0:tw], r_[:, 0:tw], AF.Sqrt)
                        k.recip(r_[:, 0:tw], r_[:, 0:tw])
                        if part == 0:
                            k.stt(o_[:, 0:tw], c_[:, 0:tw], qs, r_[:, 0:tw], ALU.mult, ALU.mult)
                        else:
                            k.tt(o_[:, 0:tw], c_[:, 0:tw], r_[:, 0:tw], ALU.mult)
                    else:
                        k.copy(o_[:, 0:tw], c_[:, 0:tw], en="pool")
                    k.dma("sp", P.dr["ab_gT"][ch * 128:(ch + 1) * 128, t0:t0 + tw], o_[:, 0:tw])


def ab_gates(P, e):
    k, cfg = P.k, P.cfg
    T = cfg.T
    r0 = cfg.PROJ_AB - 32
    with k.scope() as s:
        par = s.sb([16, 2], F32)
        k.dma("sp", par[:, 0:1], P.dr["gdn_dt_bias"][e].rearrange("d (h o) -> (d h) o", o=1))
        k.dma("sp", par[:, 1:2], P.dr["gdn_a_log"][e].rearrange("d (h o) -> (d h) o", o=1))
        k.act(par[:, 1:2], par[:, 1:2], AF.Exp)
        k.ts(par[:, 1:2], par[:, 1:2], -1.0, None, op0=ALU.mult)
        xa = [s.sb([16, 512], F32) for _ in range(2)]
        xb = [s.sb([16, 512], F32) for _ in range(2)]
        t1 = [s.sb([16, 512], F32) for _ in range(2)]
        t2 = [s.sb([16, 512], F32) for _ in range(2)]
        for i, t0 in enumerate(range(0, T, 512)):
            tw = min(512, T - t0)
            a_, b_, u_, v_ = xa[i % 2], xb[i % 2], t1[i % 2], t2[i % 2]
            k.dma("sp", a_[:, 0:tw], P.dr["ab_pT"][r0:r0 + 16, t0:t0 + tw])
            k.dma("sp", b_[:, 0:tw], P.dr["ab_pT"][r0 + 16:r0 + 32, t0:t0 + tw])
            k.ts(a_[:, 0:tw], a_[:, 0:tw], par[:, 0:1], None, op0=ALU.add)
            k.ts(u_[:, 0:tw], a_[:, 0:tw], -1.0, None, op0=ALU.mult)
            k.tt(u_[:, 0:tw], u_[:, 0:tw], a_[:, 0:tw], ALU.max)
            k.act(u_[:, 0:tw], u_[:, 0:tw], AF.Exp, scale=-1.0)
            k.act(u_[:, 0:tw], u_[:, 0:tw], AF.Ln, bias=1.0)
            k.ts(v_[:, 0:tw], a_[:, 0:tw], 0.0, None, op0=ALU.max)
            k.tt(v_[:, 0:tw], v_[:, 0:tw], u_[:, 0:tw], ALU.add)
            k.ts(v_[:, 0:tw], v_[:, 0:tw], par[:, 1:2], None, op0=ALU.mult)
            k.act(b_[:, 0:tw], b_[:, 0:tw], AF.Sigmoid)
            k.dma("sp", P.dr["ab_gbT"][0:16, t0:t0 + tw], v_[:, 0:tw])
            k.dma("sp", P.dr["ab_gbT"][16:32, t0:t0 + tw], b_[:, 0:tw])
    with k.scope() as s:
        ib = [s.sb([32, 512], F32) for _ in range(2)]
        ob = [s.sb([128, 32], F32) for _ in range(2)]
        pss = [s.ps([128, 32], F32) for _ in range(2)]
        m = 0
        for i, t0 in enumerate(range(0, T, 512)):
            tw = min(512, T - t0)
            i_ = ib[i % 2]
            k.dma("sp", i_[:, 0:tw], P.dr["ab_gbT"][:, t0:t0 + tw])
            for tj in range(tw // 128):
                ps, o_ = pss[m % 2], ob[m % 2]
                m += 1
                k.tr(ps[:, :], i_[:, tj * 128:(tj + 1) * 128], P.ident[0:32, 0:32])
                k.copy(o_[:, :], ps[:, :])
                k.dma("sp", P.dr["ab_gb"][t0 + tj * 128:t0 + (tj + 1) * 128, :], o_[:, :])


def ab_retention(P, e):
    k, cfg = P.k, P.cfg
    HA, DKA, DVA = cfg.H_A, cfg.DK_A, cfg.DV_A
    NQ = HA * DKA // 128
    DC = DKA // 128
    EV = HA * DVA
    with k.scope() as S:
        lg = S.sb([128, 2 * HA], F32)
        k.dma("sp", lg[:, :], P.dr["ret_decay"][e].rearrange("d h -> (d h)").partition_broadcast(128))
        k.act(lg[:, :], lg[:, :], AF.Exp, scale=math.log(2.0))
        k.ts(lg[:, :], lg[:, :], -1.0, 1.0, op0=ALU.mult, op1=ALU.add)
        k.act(lg[:, :], lg[:, :], AF.Ln)
        rel = S.sb([128, 2, CH], F32)
        k.dma("sp", rel[:, :, :], P.dr["ab_rel"].rearrange("d s t -> s d t"))
        qex = S.sb([128, 2, CH], F32)
        k.dma("sp", qex[:, :, :], P.dr["ab_qexp"].rearrange("d s t -> s d t"))
        kex = S.sb([128, 2], F32)
        k.dma("sp", kex[:, :], P.dr["ab_kexp"])
        mask = S.sb([128, 2 * HA, CH], F32)
        qdec = S.sb([128, 2 * HA, CH], F32)
        kdec = S.sb([128, 2 * HA], F32)
        cdec = S.sb([128, 2 * HA], F32)
        for d in range(2):
            for h in range(HA):
                i = d * HA + h
                k.act(mask[:, i, :], rel[:, d, :], AF.Exp, scale=lg[:, i:i + 1])
                k.act(qdec[:, i, :], qex[:, d, :], AF.Exp, scale=lg[:, i:i + 1])
                k.act(kdec[:, i:i + 1], kex[:, d:d + 1], AF.Exp, scale=lg[:, i:i + 1])
        k.act(cdec[:, :], lg[:, :], AF.Exp, scale=float(CH))
        gnw = S.sb([128, EV], F32)
        k.dma("sp", gnw[:, :], P.dr["ret_gn_w"][e].partition_broadcast(128))
        St = S.sb([128, HA * DC, DVA], F32)
        Sb = S.sb([128, HA * DC, DVA], BF16)
        qT = [S.sb([128, NQ, CH], BF16) for _ in range(2)]
        kT = [S.sb([128, NQ, CH], BF16) for _ in range(2)]
        kt = [S.sb([128, HA * DKA], BF16) for _ in range(2)]
        vt = [S.sb([128, EV], BF16) for _ in range(2)]
        sD = [S.sb([128, CH], BF16) for _ in range(2)]
        qd = [S.sb([128, DC, CH], BF16) for _ in range(2)]
        kd = [S.sb([128, DKA], BF16) for _ in range(2)]
        ot = [S.sb([128, EV], F32) for _ in range(2)]
        o1 = [S.sb([128, EV], F32) for _ in range(2)]
        sg = [S.sb([128, EV], BF16) for _ in range(2)]
        st6 = S.sb([128, HA, 6], F32)
        mv = S.sb([128, HA, 2], F32)
        ya = [S.sb([128, EV], BF16) for _ in range(2)]
        yT = [S.sb([128, 4, CH], BF16) for _ in range(2)]
        p_sc = [S.ps([128, CH], F32) for _ in range(2)]
        p_o = [S.ps([128, 512], F32) for _ in range(2)]
        p_s = [S.ps([128, 512], F32) for _ in range(2)]
        p_t = [S.ps([128, 512], BF16) for _ in range(2)]
        it = 0
        nt = 0
        for si, (st, L, ci) in enumerate(cfg.SEGS):
            NCk = L // CH
            for d in range(2):
                if si == 0:
                    k.dma("sp", St[:, :, :], P.dr["state_ret"][e, d].rearrange("h (c p) v -> p (h c) v", p=128))
                    k.copy(Sb[:, :, :], St[:, :, :], en="pool")
                else:
                    k.memset(St[:, :, :], 0.0, en="pool")
                    k.memset(Sb[:, :, :], 0.0, en="pool")
                order = range(NCk) if d == 0 else range(NCk - 1, -1, -1)
                for cn in order:
                    t0 = st + cn * CH
                    i2 = it % 2
                    it += 1
                    q_, k_, kt_, v_, o_ = qT[i2], kT[i2], kt[i2], vt[i2], ot[i2]
                    k.dma("sp", q_[:, :, :], P.dr["ab_rqk"][0:NQ * 128, t0:t0 + CH].rearrange("(c p) t -> p c t", p=128))
                    k.dma("sp", k_[:, :, :], P.dr["ab_rqk"][NQ * 128:2 * NQ * 128, t0:t0 + CH].rearrange("(c p) t -> p c t", p=128))
                    k.dma("sp", kt_[:, :], P.dr["ab_rk_tok"][t0:t0 + CH, :])
                    k.dma("sp", v_[:, :], P.dr["ab_rv_tok"][t0:t0 + CH, :])
                    if d == 1:
                        k.dma("sp", o1[i2][:, :], P.dr["ab_o1r"][t0:t0 + CH, :])
                        k.dma("sp", sg[i2][:, :], P.dr["ab_rg_tok"][t0:t0 + CH, :])
                    for h in range(HA):
                        i = d * HA + h
                        hh = it * HA + h
                        psc, po, s_, qd_, kd_ = p_sc[hh % 2], p_o[hh % 2], sD[hh % 2], qd[hh % 2], kd[hh % 2]
                        for dc in range(DC):
                            k.mm(psc[:, :], k_[:, h * DC + dc, :], q_[:, h * DC + dc, :], start=(dc == 0), stop=(dc == DC - 1))
                        k.tt(s_[:, :], psc[:, :], mask[:, i, :], ALU.mult)
                        for dc in range(DC):
                            k.tt(qd_[:, dc, :], q_[:, h * DC + dc, :], qdec[:, i, :], ALU.mult, en="pool")
                        k.mm(po[:, 0:DVA], s_[:, :], v_[:, h * DVA:(h + 1) * DVA], start=True, stop=False)
                        for dc in range(DC):
                            k.mm(po[:, 0:DVA], qd_[:, dc, :], Sb[:, h * DC + dc, :], start=False, stop=(dc == DC - 1))
                        k.copy(o_[:, h * DVA:(h + 1) * DVA], po[:, 0:DVA], en="act")
                        k.ts(kd_[:, :], kt_[:, h * DKA:(h + 1) * DKA], kdec[:, i:i + 1], None, op0=ALU.mult, en="pool")
                        for dc in range(DC):
                            pst = p_s[(hh * DC + dc) % 2]
                            k.mm(pst[:, 0:DVA], kd_[:, dc * 128:(dc + 1) * 128], v_[:, h * DVA:(h + 1) * DVA])
                            k.stt(St[:, h * DC + dc, :], St[:, h * DC + dc, :], cdec[:, i:i + 1], pst[:, 0:DVA], ALU.mult, ALU.add)
                            k.copy(Sb[:, h * DC + dc, :], St[:, h * DC + dc, :], en="act")
                    if d == 0:
                        k.dma("sp", P.dr["ab_o1r"][t0:t0 + CH, :], o_[:, :])
                    else:
                        o1_, sg_, ya_ = o1[i2], sg[i2], ya[i2]
                        k.tt(o_[:, :], o_[:, :], o1_[:, :], ALU.add, en="pool")
                        for h in range(HA):
                            k.do("dve", lambda en_, h=h: en_.bn_stats(out=st6[:, h, :].ap, in_=o_[:, h * DVA:(h + 1) * DVA].ap),
                                 reads=(o_[:, :],), writes=(st6[:, :, :],))
                            k.do("dve", lambda en_, h=h: en_.bn_aggr(out=mv[:, h, :].ap, in_=st6[:, h, :].ap),
                                 reads=(st6[:, :, :],), writes=(mv[:, :, :],))
                        k.ts(mv[:, :, 1], mv[:, :, 1], cfg.EPS, None, op0=ALU.add)
                        k.act(mv[:, :, 1], mv[:, :, 1], AF.Sqrt)
                        k.recip(mv[:, :, 1], mv[:, :, 1])
                        for h in range(HA):
                            sl = slice(h * DVA, (h + 1) * DVA)
                            k.ts(o_[:, sl], o_[:, sl], mv[:, h, 0:1], mv[:, h, 1:2], op0=ALU.subtract, op1=ALU.mult)
                        k.tt(o_[:, :], o_[:, :], gnw[:, :], ALU.mult, en="pool")
                        k.tt(ya_[:, :], o_[:, :], sg_[:, :], ALU.mult)
                        for g in range(EV // 512):
                            pt, y_ = p_t[nt % 2], yT[nt % 2]
                            nt += 1
                            for c in range(4):
                                k.tr(pt[:, c * 128:(c + 1) * 128], ya_[:, g * 512 + c * 128:g * 512 + (c + 1) * 128], P.identh)
                            k.copy(y_[:, :, :], pt[:, 0:512].rearrange("p (c t) -> p c t", t=128), en=("act" if nt % 2 else "dve"))
                            k.dma("sp", P.dr["ab_catT"][g * 512:(g + 1) * 512, t0:t0 + CH].rearrange("(c p) t -> p c t", p=128), y_[:, :, :])
                if si > 0:
                    k.dma("sp", P.dr["nsr"][si - 1, e, d].rearrange("h (c p) v -> p (h c) v", p=128), St[:, :, :])


def ab_gdn(P, e):
    import os
    GSTOP = int(os.environ.get('GDN_STOP', '99'))
    GSUB = int(os.environ.get('GDN_SUB', '99'))
    k, cfg = P.k, P.cfg
    HB, DKB, DVB = cfg.H_B, cfg.DK_B, cfg.DV_B
    EV = HB * DVB
    NLEV = 7
    with k.scope() as S:
        U = S.sb([128, 2, CH], F32)
        k.dma("sp", U[:, :, :], P.dr["ab_U"].rearrange("d s t -> s d t"))
        negm = S.sb([128, 2, CH], F32)
        k.dma("sp", negm[:, :, :], P.dr["ab_negm"].rearrange("d s t -> s d t"))
        strict = S.sb([128, 2, CH], F32)
        k.dma("sp", strict[:, :, :], P.dr["ab_strict"].rearrange("d s t -> s d t"))
        gnw = S.sb([128, DVB], F32)
        k.dma("sp", gnw[:, :], P.dr["gdn_norm_w"][e].partition_broadcast(128))
        lm = S.sb([128, 2, 7, CH], F32)
        lmT = S.sb([128, 2, 7, CH], F32)
        for d_ in range(2):
            k.dma("sp", lm[:, d_, :, :], P.dr["ab_lm"][d_].rearrange("l t s -> t l s"))
            k.dma("sp", lmT[:, d_, :, :], P.dr["ab_lmT"][d_].rearrange("l t s -> t l s"))
        Lb, LTb, M1, M2 = ([S.sb([128, CH], BF16) for _ in range(2)] for _ in range(4))
        St = S.sb([128, HB, DVB], F32)
        Sb = S.sb([128, HB, DVB], BF16)
        qT = [S.sb([128, HB, CH], BF16) for _ in range(2)]
        kT = [S.sb([128, HB, CH], BF16) for _ in range(2)]
        kv = [S.sb([128, 2 * EV], BF16) for _ in range(2)]
        gb = [S.sb([128, 32], F32) for _ in range(2)]
        ot = [S.sb([128, EV], F32) for _ in range(2)]
        o1 = [S.sb([128, EV], F32) for _ in range(2)]
        sz = [S.sb([128, EV], BF16) for _ in range(2)]
        yb = [S.sb([128, EV], BF16) for _ in range(2)]
        yT = [S.sb([128, 4, CH], BF16) for _ in range(2)]
        ss = S.sb([128, HB], F32)
        sqt = S.sb([128, EV], F32)

        def two(shape, dt):
            return [S.sb(shape, dt) for _ in range(2)]
        gbc, bbc = two([128, CH], F32), two([128, CH], F32)
        gsm = two([128, CH], F32)
        sc = two([128, 8], F32)
        dec, eGr = two([128, CH], F32), two([128, CH], F32)
        t1 = two([128, CH], F32)
        Qf = two([128, CH], F32)
        Qb, Pb, Xb, Yb = two([128, CH], BF16), two([128, CH], BF16), two([128, CH], BF16), two([128, CH], BF16)
        rv, rk, wT, ub, qk, qd, kd = (two([128, CH], BF16) for _ in range(7))
        uf = two([128, CH], F32)
        pA = [S.ps([128, 512], F32) for _ in range(2)]
        pB = [S.ps([128, 512], F32) for _ in range(2)]
        pC = [S.ps([128, 512], F32) for _ in range(2)]
        pD = [S.ps([128, 512], F32) for _ in range(1)]
        pT = [S.ps([128, 512], BF16) for _ in range(1)]
        identf = P.ident
        ih = 0
        it = 0
        nt = 0
        for si, (st, L, ci) in enumerate(cfg.SEGS):
            NCk = L // CH
            for d in range(2):
                if si == 0:
                    k.dma("sp", St[:, :, :], P.dr["state_gdn"][e, d].rearrange("h p v -> p h v"))
                    k.copy(Sb[:, :, :], St[:, :, :], en="pool")
                else:
                    k.memset(St[:, :, :], 0.0, en="pool")
                    k.memset(Sb[:, :, :], 0.0, en="pool")
                last = CH - 1 if d == 0 else 0
                order = range(NCk) if d == 0 else range(NCk - 1, -1, -1)
                for cn in order:
                    t0 = st + cn * CH
                    i2 = it % 2
                    it += 1
                    q_, k_, kv_, gb_, o_ = qT[i2], kT[i2], kv[i2], gb[i2], ot[i2]
                    k.dma("sp", q_[:, :, :], P.dr["ab_gT"][0:EV, t0:t0 + CH].rearrange("(c p) t -> p c t", p=128))
                    k.dma("sp", k_[:, :, :], P.dr["ab_gT"][EV:2 * EV, t0:t0 + CH].rearrange("(c p) t -> p c t", p=128))
                    k.dma("sp", kv_[:, :], P.dr["ab_gtok"][t0:t0 + CH, :])
                    k.dma("sp", gb_[:, :], P.dr["ab_gb"][t0:t0 + CH, :])
                    if d == 1:
                        k.dma("sp", o1[i2][:, :], P.dr["ab_o1g"][t0:t0 + CH, :])
                        k.dma("sp", sz[i2][:, :], P.dr["ab_gz_tok"][t0:t0 + CH, :])
                    for h in range(HB):
                        j = ih % 2
                        ih += 1
                        gcol = gb_[:, d * HB + h:d * HB + h + 1]
                        bcol = gb_[:, 16 + d * HB + h:16 + d * HB + h + 1]
                        ktok = kv_[:, h * DKB:(h + 1) * DKB]
                        vtok = kv_[:, EV + h * DVB:EV + (h + 1) * DVB]
                        A, B, C, Dp = pA[j], pB[j], pC[j], pD[0]
                        sc_ = sc[j]
                        k.copy(gbc[j][:, :], gcol.to_broadcast([128, CH]), en="pool")
                        k.copy(bbc[j][:, :], bcol.to_broadcast([128, CH]), en="pool")
                        k.tt(gsm[j][:, :], gbc[j][:, :], strict[:, 1 - d, :], ALU.mult, en="pool")
                        k.mm(A[:, 0:CH], gbc[j][:, :], U[:, d, :])
                        k.mm(B[:, 0:CH], gsm[j][:, :], U[:, d, :])
                        k.mm(C[:, 0:1], U[:, d, :], gcol)
                        k.mm(A[:, CH:2 * CH], bbc[j][:, :], identf)
                        k.mm(A[:, 2 * CH:3 * CH], k_[:, h, :], k_[:, h, :])
                        k.mm(A[:, 3 * CH:4 * CH], k_[:, h, :], q_[:, h, :])
                        if GSTOP <= 1:
                            continue
                        k.copy(sc_[:, 0:1], C[:, 0:1])
                        k.act(sc_[:, 1:2], sc_[:, 0:1], AF.Exp)
                        k.act(sc_[:, 2:3], A[:, last:last + 1], AF.Exp)
                        k.act(sc_[:, 3:4], B[:, last:last + 1], AF.Exp)
                        if GSTOP <= 2:
                            continue
                        k.tt(dec[j][:, :], B[:, 0:CH], negm[:, d, :], ALU.add)
                        k.act(dec[j][:, :], dec[j][:, :], AF.Exp)
                        k.act(eGr[j][:, :], A[:, 0:CH], AF.Exp)
                        if GSTOP <= 3:
                            continue
                        k.tt(t1[j][:, :], dec[j][:, :], strict[:, d, :], ALU.mult, en="pool")
                        k.tt(t1[j][:, :], A[:, CH:2 * CH], t1[j][:, :], ALU.mult)
                        k.stt(Qf[j][:, :], A[:, 2 * CH:3 * CH], -1.0, t1[j][:, :], ALU.mult, ALU.mult)
                        if GSUB == 0:
                            continue
                        k.copy(Qb[j][:, :], Qf[j][:, :], en="pool")
                        if GSUB == 1:
                            continue
                        k.tr(Dp[:, 0:CH], Qf[j][:, :], identf)
                        if GSUB == 2:
                            continue
                        k.copy(uf[j][:, :], Dp[:, 0:CH], en="act")
                        k.copy(Xb[j][:, :], identf, en="pool")
                        k.copy(Yb[j][:, :], identf, en="pool")
                        for lev in range(NLEV):
                            lastlev = lev == NLEV - 1
                            k.tt(Lb[j][:, :], uf[j][:, :], lm[:, d, lev, :], ALU.mult, en="pool")
                            k.mm(B[:, 0:CH], Lb[j][:, :], Xb[j][:, :])
                            k.act(M1[j][:, :], B[:, 0:CH], AF.Identity, scale=-1.0)
                            k.mm(B[:, 2 * CH:3 * CH], Yb[j][:, :], M1[j][:, :])
                            if not lastlev:
                                k.tt(LTb[j][:, :], Qf[j][:, :], lmT[:, d, lev, :], ALU.mult, en="pool")
                                k.mm(B[:, CH:2 * CH], LTb[j][:, :], Yb[j][:, :])
                                k.ts(M2[j][:, :], B[:, CH:2 * CH], -1.0, None, op0=ALU.mult)
                                k.mm(B[:, 3 * CH:4 * CH], Xb[j][:, :], M2[j][:, :])
                            k.tt(Xb[j][:, :], B[:, 2 * CH:3 * CH], Xb[j][:, :], ALU.add)
                            if not lastlev:
                                k.tt(Yb[j][:, :], B[:, 3 * CH:4 * CH], Yb[j][:, :], ALU.add)
                        if GSTOP <= 5:
                            continue
                        k.ts(rv[j][:, :], vtok, bcol, None, op0=ALU.mult, en="pool")
                        k.ts(rk[j][:, :], ktok, bcol, sc_[:, 1:2], op0=ALU.mult, op1=ALU.mult, en="pool")
                        k.mm(C[:, CH:2 * CH], Xb[j][:, :], rv[j][:, :])
                        k.mm(C[:, 2 * CH:3 * CH], rk[j][:, :], Xb[j][:, :])
                        k.copy(wT[j][:, :], C[:, 2 * CH:3 * CH], en="act")
                        k.copy(uf[j][:, :], C[:, CH:2 * CH])
                        k.mm(C[:, 3 * CH:4 * CH], wT[j][:, :], Sb[:, h, :])
                        k.tt(ub[j][:, :], uf[j][:, :], C[:, 3 * CH:4 * CH], ALU.subtract)
                        k.tt(qk[j][:, :], A[:, 3 * CH:4 * CH], dec[j][:, :], ALU.mult)
                        k.tt(qd[j][:, :], q_[:, h, :], eGr[j][:, :], ALU.mult, en="pool")
                        k.mm(Dp[:, CH:2 * CH], qd[j][:, :], Sb[:, h, :], start=True, stop=False)
                        k.mm(Dp[:, CH:2 * CH], qk[j][:, :], ub[j][:, :], start=False, stop=True)
                        k.copy(o_[:, h * DVB:(h + 1) * DVB], Dp[:, CH:2 * CH], en="act")
                        k.ts(kd[j][:, :], ktok, sc_[:, 3:4], None, op0=ALU.mult, en="pool")
                        k.mm(Dp[:, 2 * CH:3 * CH], kd[j][:, :], ub[j][:, :])
                        k.stt(St[:, h, :], St[:, h, :], sc_[:, 2:3], Dp[:, 2 * CH:3 * CH], ALU.mult, ALU.add)
                        k.copy(Sb[:, h, :], St[:, h, :], en="act")
                    if d == 0:
                        k.dma("sp", P.dr["ab_o1g"][t0:t0 + CH, :], o_[:, :])
                    else:
                        o1_, sz_, yb_ = o1[i2], sz[i2], yb[i2]
                        k.tt(o_[:, :], o_[:, :], o1_[:, :], ALU.add, en="pool")
                        k.act(sqt[:, :], o_[:, :], AF.Square)
                        k.do("dve", lambda en_: en_.tensor_reduce(out=ss[:, :].ap, in_=sqt[:, :].rearrange("p (h v) -> p h v", v=DVB).ap,
                                                                    axis=mybir.AxisListType.X, op=ALU.add),
                             reads=(sqt[:, :],), writes=(ss[:, :],))
                        k.ts(ss[:, :], ss[:, :], 1.0 / DVB, cfg.EPS, op0=ALU.mult, op1=ALU.add)
                        k.act(ss[:, :], ss[:, :], AF.Sqrt)
                        k.recip(ss[:, :], ss[:, :])
                        for h in range(HB):
                            sl = slice(h * DVB, (h + 1) * DVB)
                            k.stt(o_[:, sl], o_[:, sl], ss[:, h:h + 1], gnw[:, :], ALU.mult, ALU.mult)
                        k.tt(yb_[:, :], o_[:, :], sz_[:, :], ALU.mult)
                        for g in range(EV // 512):
                            pt, y_ = pT[0], yT[nt % 2]
                            nt += 1
                            for c in range(4):
                                k.tr(pt[:, c * 128:(c + 1) * 128], yb_[:, g * 512 + c * 128:g * 512 + (c + 1) * 128], P.identh)
                            k.copy(y_[:, :, :], pt[:, 0:512].rearrange("p (c t) -> p c t", t=128), en=("act" if nt % 2 else "dve"))
                            r0 = cfg.H_A * cfg.DV_A + g * 512
                            k.dma("sp", P.dr["ab_catT"][r0:r0 + 512, t0:t0 + CH].rearrange("(c p) t -> p c t", p=128), y_[:, :, :])
                if si > 0:
                    k.dma("sp", P.dr["nsg"][si - 1, e, d].rearrange("h p v -> p h v"), St[:, :, :])


def mixer_ab_impl(P, e):
    cfg = P.cfg
    D = cfg.D_MODEL
    groups = token_groups(cfg)
    HA, DKA, DVA, HB, DKB, DVB = cfg.H_A, cfg.DK_A, cfg.DV_A, cfg.H_B, cfg.DK_B, cfg.DV_B
    import os
    stop = int(os.environ.get("AB_STOP", "99"))
    steps = [
        lambda: proj_fm(P, P.dr["w_in_ab"][e], D, cfg.PROJ_AB, P.dr["hT"], P.dr["ab_pT"], F32, groups),
        lambda: ab_rope(P),
        lambda: fm_to_tm(P, P.dr["ab_rqk"], HA * DKA, HA * DKA, P.dr["ab_rk_tok"], 0, BF16, BF16),
        lambda: fm_to_tm(P, P.dr["ab_pT"], 2 * HA * DKA, HA * DVA, P.dr["ab_rv_tok"], 0, F32, BF16),
        lambda: fm_to_tm(P, P.dr["ab_pT"], 2 * HA * DKA + HA * DVA, HA * DVA, P.dr["ab_rg_tok"], 0, F32, BF16, func=AF.Silu),
        lambda: ab_gdn_conv(P, e),
        lambda: fm_to_tm(P, P.dr["ab_gT"], HB * DKB, 2 * HB * DKB, P.dr["ab_gtok"], 0, BF16, BF16),
        lambda: fm_to_tm(P, P.dr["ab_pT"], 2 * HA * DKA + 2 * HA * DVA + 3 * HB * DKB, HB * DVB, P.dr["ab_gz_tok"], 0, F32, BF16, func=AF.Silu),
        lambda: ab_gates(P, e),
        lambda: ab_retention(P, e),
        lambda: ab_gdn(P, e),
        lambda: proj_fm(P, P.dr["w_out_ab"][e], cfg.MIX_AB_OUT, D, P.dr["ab_catT"], P.dr["yT"], F32, groups),
    ]
    for i, f in enumerate(steps):
        if i >= stop:
            break
        f()
```

```python
import math
from contextlib import ExitStack

import ml_dtypes
import numpy as np

import concourse.bass as bass
import concourse.mybir as mybir
from concourse.bass_utils import run_bass_kernel_spmd

F32 = mybir.dt.float32
BF16 = mybir.dt.bfloat16
AF = mybir.ActivationFunctionType
ALU = mybir.AluOpType
NPBF = ml_dtypes.bfloat16


class Cfg:
    D_MODEL = 2048
    BATCH = 16
    SEQ = 256
    DEPTH = 4
    DEC_BATCH = 8
    DEC_SEQ = 4096
    GRID_W = 64
    H_A = 4
    DK_A = 256
    DV_A = 512
    ROPE_BASE = 10000.0
    H_B = 8
    DK_B = 128
    DV_B = 128
    CONV_B = 5
    HY_CONV = 3
    HY_BANDS = 16
    HY_FFN = 64
    HY_FAST_DECAY = 0.3
    HY_SLOW_DECAY = 1.5
    HY_TARGET = 1e-2
    EPS = 1e-6
    N_CORES = 8

    def __init__(self, **kw):
        for k_, v in kw.items():
            setattr(self, k_, v)
        self.D_FF = ((8 * self.D_MODEL // 3 + 255) // 256) * 256
        self.HY_EMB = 1 + 2 * self.HY_BANDS
        self.N_EVEN = (self.DEPTH + 1) // 2
        self.N_ODD = self.DEPTH // 2
        self.SIZES_AB = (self.H_A * self.DK_A, self.H_A * self.DK_A, self.H_A * self.DV_A, self.H_A * self.DV_A,
                         2 * self.H_B * self.DK_B + self.H_B * self.DV_B, self.H_B * self.DV_B,
                         2 * self.H_B, 2 * self.H_B)
        self.PROJ_AB = sum(self.SIZES_AB)
        self.MIX_AB_OUT = self.H_A * self.DV_A + self.H_B * self.DV_B
        self.KC = self.D_MODEL // 128
        self.NPROMPT = self.BATCH // self.N_CORES
        self.T = self.DEC_SEQ + self.NPROMPT * self.SEQ
        self.SEGS = [(0, self.DEC_SEQ, 0)] + [(self.DEC_SEQ + i * self.SEQ, self.SEQ, 1) for i in range(self.NPROMPT)]


class Buf:
    __slots__ = ("t", "w", "r", "x")

    def __init__(self, t, excl=False):
        self.t = t
        self.x = excl
        self.w = []
        self.r = []

    def __getitem__(self, idx):
        return V(self, self.t[idx])


class V:
    __slots__ = ("buf", "ap")

    def __init__(self, buf, ap):
        self.buf = buf
        self.ap = ap

    def __getitem__(self, idx):
        return V(self.buf, self.ap[idx])

    def rearrange(self, *a, **kw):
        return V(self.buf, self.ap.rearrange(*a, **kw))

    def unsqueeze(self, *a):
        return V(self.buf, self.ap.unsqueeze(*a))

    def to_broadcast(self, *a):
        return V(self.buf, self.ap.to_broadcast(*a))

    def bitcast(self, *a):
        return V(self.buf, self.ap.bitcast(*a))


def _ap(x):
    return x.ap if isinstance(x, V) else x


def _bufs(xs):
    return [x.buf for x in xs if isinstance(x, V)]


class K:
    ENG = ("pe", "act", "dve", "pool", "sp")

    def __init__(self, nc):
        self.nc = nc
        self.es = ExitStack()
        self.E = {"pe": nc.tensor, "act": nc.scalar, "dve": nc.vector, "pool": nc.gpsimd, "sp": nc.sync}
        self.sem = {}
        self.cnt = {}
        self.semid = {}
        self.allsems = []
        for e in self.ENG:
            s = self.es.enter_context(nc.semaphore("s_" + e))
            self.sem[e] = s
            self.cnt[e] = 0
            self.allsems.append(s)
        self.dsem = {}
        self.dval = {}
        self.dnext = {}
        for q, n in (("sp", 16), ("pool", 16), ("act", 8)):
            self.dsem[q] = [self.es.enter_context(nc.semaphore(f"d_{q}{i}")) for i in range(n)]
            self.dval[q] = [0] * n
            self.dnext[q] = 0
            self.allsems += self.dsem[q]
        self.val = {id(s): 0 for s in self.allsems}
        self.seen = {e: {} for e in self.ENG}
        self.nuniq = 0
        self.ninst = 0

    def _wait(self, en, ev):
        sem, val = ev
        sid = id(sem)
        sd = self.seen[en]
        if sd.get(sid, 0) >= val:
            return
        sd[sid] = val
        self.E[en].wait_ge(sem, val)

    def _deps(self, en, reads, writes):
        own = id(self.sem[en])
        for b in reads:
            for ev in b.w:
                if en == "pe" and id(ev[0]) == own:
                    continue
                self._wait(en, ev)
            if b.x:
                for ev in b.r:
                    if id(ev[0]) != own:
                        self._wait(en, ev)
        for b in writes:
            for ev in b.w:
                if id(ev[0]) == own:
                    continue
                self._wait(en, ev)
            for ev in b.r:
                if id(ev[0]) == own:
                    continue
                self._wait(en, ev)

    def _mark(self, ev, reads, writes):
        for b in reads:
            sid = id(ev[0])
            b.r = [e for e in b.r if id(e[0]) != sid]
            b.r.append(ev)
        for b in writes:
            b.w = [ev]
            b.r = []

    def do(self, en, fn, reads=(), writes=()):
        rb, wb = _bufs(reads), _bufs(writes)
        self._deps(en, rb, wb)
        ins = fn(self.E[en])
        self.cnt[en] += 1
        ins.then_inc(self.sem[en], 1)
        ev = (self.sem[en], self.cnt[en])
        self.val[id(self.sem[en])] = self.cnt[en]
        self._mark(ev, rb, wb)
        self.ninst += 1
        return ev

    def dma(self, q, out, in_):
        rb, wb = _bufs([in_]), _bufs([out])
        j = self.dnext[q]
        self.dnext[q] = (j + 1) % len(self.dsem[q])
        sem = self.dsem[q][j]
        self._wait(q, (sem, self.dval[q][j]))
        self._deps(q, rb, wb)
        self.E[q].dma_start(out=_ap(out), in_=_ap(in_)).then_inc(sem, 16)
        self.dval[q][j] += 16
        ev = (sem, self.dval[q][j])
        self.val[id(sem)] = self.dval[q][j]
        self._mark(ev, rb, wb)
        self.ninst += 1
        return ev

    def barrier(self, engines=None):
        for en in (engines or self.ENG):
            for s in self.allsems:
                if s is self.sem[en]:
                    continue
                v = self.val[id(s)]
                if v:
                    self._wait(en, (s, v))

    class Scope:
        def __init__(self, k):
            self.k = k
            self.es = ExitStack()

        def sb(self, shape, dt, name=None):
            self.k.nuniq += 1
            t = self.es.enter_context(self.k.nc.sbuf_tensor(name or f"t{self.k.nuniq}", list(shape), dt))
            return Buf(t)

        def ps(self, shape, dt=F32, name=None):
            self.k.nuniq += 1
            t = self.es.enter_context(self.k.nc.psum_tensor(name or f"p{self.k.nuniq}", list(shape), dt))
            return Buf(t, excl=True)

        def __enter__(self):
            return self

        def __exit__(self, *a):
            if a[0] is None:
                self.k.barrier()
            self.es.close()
            return False

    def scope(self):
        return K.Scope(self)

    def mm(self, out, lhsT, rhs, start=True, stop=True):
        return self.do("pe", lambda e: e.matmul(_ap(out), lhsT=_ap(lhsT), rhs=_ap(rhs), start=start, stop=stop),
                       reads=(lhsT, rhs), writes=(out,))

    def tr(self, out, in_, ident):
        return self.do("pe", lambda e: e.transpose(_ap(out), _ap(in_), _ap(ident)), reads=(in_, ident), writes=(out,))

    def act(self, out, in_, func, bias=None, scale=None, en="act"):
        kw = {}
        rd = [in_]
        if bias is not None:
            kw["bias"] = _ap(bias)
            rd.append(bias)
        if scale is not None:
            kw["scale"] = _ap(scale)
            rd.append(scale)
        return self.do("act", lambda e: e.activation(out=_ap(out), in_=_ap(in_), func=func, **kw),
                       reads=rd, writes=(out,))

    def tt(self, out, in0, in1, op, en="dve"):
        return self.do(en, lambda e: e.tensor_tensor(out=_ap(out), in0=_ap(in0), in1=_ap(in1), op=op),
                       reads=(in0, in1), writes=(out,))

    def ts(self, out, in0, s1, s2=None, op0=ALU.mult, op1=None, en="dve"):
        kw = {}
        if op1 is not None:
            kw["op1"] = op1
        return self.do(en, lambda e: e.tensor_scalar(out=_ap(out), in0=_ap(in0), scalar1=_ap(s1), scalar2=_ap(s2),
                                                     op0=op0, **kw),
                       reads=(in0, s1, s2), writes=(out,))

    def stt(self, out, in0, scalar, in1, op0, op1):
        return self.do("dve", lambda e: e.scalar_tensor_tensor(out=_ap(out), in0=_ap(in0), scalar=_ap(scalar),
                                                               in1=_ap(in1), op0=op0, op1=op1),
                       reads=(in0, scalar, in1), writes=(out,))

    def copy(self, out, in_, en="dve"):
        if en == "act":
            return self.do("act", lambda e: e.copy(out=_ap(out), in_=_ap(in_)), reads=(in_,), writes=(out,))
        return self.do(en, lambda e: e.tensor_copy(out=_ap(out), in_=_ap(in_)), reads=(in_,), writes=(out,))

    def recip(self, out, in_):
        return self.do("dve", lambda e: e.reciprocal(out=_ap(out), in_=_ap(in_)), reads=(in_,), writes=(out,))

    def memset(self, out, val, en="dve"):
        return self.do(en, lambda e: e.memset(_ap(out), val), writes=(out,))


class Prog:
    def __init__(self, cfg):
        self.cfg = cfg
        self.nc = bass.Bass("TRN2", target_bir_lowering=False)
        self.k = K(self.nc)
        self.dr = {}

    def mark(self, label):
        if not hasattr(self, "marks"):
            self.marks = []
        self.marks.append((label, dict(self.k.cnt)))

    def din(self, name, shape, dt=F32):
        self.dr[name] = self.nc.dram_tensor(name, list(shape), dt, kind="ExternalInput").ap()
        return self.dr[name]

    def dout(self, name, shape, dt=F32):
        self.dr[name] = self.nc.dram_tensor(name, list(shape), dt, kind="ExternalOutput").ap()
        return self.dr[name]

    def dtmp(self, name, shape, dt=F32):
        kind = "ExternalOutput" if name in getattr(self.cfg, "DEBUG_OUT", ()) else "Internal"
        self.dr[name] = self.nc.dram_tensor(name, list(shape), dt, kind=kind).ap()
        return self.dr[name]

    def load_fm_vec(self, s, dst, src, n):
        k = self.k
        if not hasattr(s, "_fm_tmp"):
            s._fm_tmp = s.sb([128, 128], F32)
            s._fm_ps = s.ps([128, 128], F32)
        tmp, ps = s._fm_tmp, s._fm_ps
        k.dma("sp", tmp[0:n, :], src.rearrange("(c p) -> c p", p=128))
        k.tr(ps[:, 0:n], tmp[0:n, :], self.ident[0:n, 0:n])
        k.copy(dst, ps[:, 0:n])

    def rstd_from_ss(self, s, r, ps, n_feat, width):
        k = self.k
        k.ts(r, ps, 1.0 / n_feat, self.cfg.EPS, op0=ALU.mult, op1=ALU.add)
        k.act(r, r, AF.Sqrt)
        k.recip(r, r)

    def phase_consts(self, S):
        k, cfg = self.k, self.cfg
        self.ident_b = S.sb([128, 128], F32)
        k.dma("sp", self.ident_b[:, :], self.dr["c_ident"])
        self.ident = self.ident_b[:, :]
        self.identh_b = S.sb([128, 128], BF16)
        k.dma("pool", self.identh_b[:, :], self.dr["c_ident"])
        self.identh = self.identh_b[:, :]
        self.ones_b = S.sb([128, 128], BF16)
        k.memset(self.ones_b[:, :], 1.0)
        self.ones = self.ones_b[:, :]

    def phase_mod(self, S):
        k, cfg = self.k, self.cfg
        KC, D = cfg.KC, cfg.D_MODEL
        NL = cfg.DEPTH
        self.modv = S.sb([128, NL * 6 * KC * 2], F32)
        mv = self.modv

        def mvs(l, j, kc, c0=0, c1=2):
            base = ((l * 6 + j) * KC + kc) * 2
            return mv[:, base + c0:base + c1]
        self.mvs = mvs
        with k.scope() as s:
            cond = s.sb([2, D], F32)
            k.dma("sp", cond[:, :], self.dr["cond"])
            k.act(cond[:, :], cond[:, :], AF.Silu)
            scT = s.sb([128, KC, 2], BF16)
            pst = s.ps([128, 512], F32)
            for kc in range(KC):
                k.tr(pst[:, 2 * kc:2 * kc + 2], cond[:, kc * 128:(kc + 1) * 128], self.ident[0:2, 0:2])
            k.copy(scT[:, :, :], pst[:, 0:2 * KC].rearrange("p (c t) -> p c t", t=2))
            nw = s.sb([128, 4, KC], F32)
            raw = s.sb([128, 6 * KC, 2], F32)
            bm = s.sb([128, 6 * KC], F32)
            CB = 512 if 6 * D >= 512 else 6 * D
            wbufs = [s.sb([128, KC, CB], BF16) for _ in range(2)]
            pss = [s.ps([128, 512], F32) for _ in range(2)]
            for l in range(NL):
                for i, nm in enumerate(("norm_mix_pre", "norm_mix_post", "norm_ffn_pre", "norm_ffn_post")):
                    self.load_fm_vec(s, nw[:, i, :], self.dr[nm][l], KC)
                self.load_fm_vec(s, bm[:, :], self.dr["b_mod"][l], 6 * KC)
                wsrc = self.dr["w_mod"][l].rearrange("(c p) n -> p c n", p=128)
                for cb in range(6 * D // CB):
                    wb = wbufs[cb % 2]
                    k.dma("pool", wb[:, :, :], wsrc[:, :, cb * CB:(cb + 1) * CB])
                    ps = pss[cb % 2]
                    nch = CB // 128
                    for m in range(nch):
                        for kc in range(KC):
                            k.mm(ps[:, 2 * m:2 * m + 2], wb[:, kc, m * 128:(m + 1) * 128], scT[:, kc, :],
                                 start=(kc == 0), stop=(kc == KC - 1))
                    ch0 = cb * nch
                    k.tt(raw[:, ch0:ch0 + nch, :], ps[:, 0:2 * nch].rearrange("p (c t) -> p c t", t=2),
                         bm[:, ch0:ch0 + nch].unsqueeze(2).to_broadcast([128, nch, 2]), ALU.add)
                for kc in range(KC):
                    for half, (npre, npost) in enumerate(((0, 1), (2, 3))):
                        sh = raw[:, (3 * half + 0) * KC + kc, :]
                        sc = raw[:, (3 * half + 1) * KC + kc, :]
                        g = raw[:, (3 * half + 2) * KC + kc, :]
                        k.ts(mvs(l, 3 * half + 0, kc), sc, 1.0, nw[:, npre, kc:kc + 1], op0=ALU.add, op1=ALU.mult)
                        k.copy(mvs(l, 3 * half + 1, kc), sh)
                        k.ts(mvs(l, 3 * half + 2, kc), g, nw[:, npost, kc:kc + 1], None, op0=ALU.mult)

    def phase_in(self):
        k, cfg = self.k, self.cfg
        KC, D, T = cfg.KC, cfg.D_MODEL, cfg.T
        xT = self.dr["xT"].rearrange("(c p) t -> p c t", p=128)
        with k.scope() as s:
            xin = [s.sb([128, D], F32) for _ in range(2)]
            xo = [s.sb([128, KC, 512], F32) for _ in range(2)]
            pss = [s.ps([128, 512], F32) for _ in range(4)]
            n = 0
            for t0 in range(0, T, 512):
                ob = xo[(t0 // 512) % 2]
                for tt in range(4):
                    ib = xin[n % 2]
                    k.dma("sp", ib[:, :], self.dr["x_in"][t0 + tt * 128:t0 + (tt + 1) * 128, :])
                    for g in range(KC // 4 if KC >= 4 else 1):
                        ps = pss[n % 4]
                        nn = min(4, KC)
                        for i in range(nn):
                            kc = g * 4 + i
                            k.tr(ps[:, i * 128:(i + 1) * 128], ib[:, kc * 128:(kc + 1) * 128], self.ident)
                        k.copy(ob[:, g * 4:g * 4 + nn, tt * 128:(tt + 1) * 128],
                               ps[:, 0:nn * 128].rearrange("p (c t) -> p c t", t=128),
                               en=("act" if n % 2 else "dve"))
                        n += 1
                k.dma("sp", xT[:, :, t0:t0 + 512], ob[:, :, :])

    def phase_out(self):
        k, cfg = self.k, self.cfg
        KC, D, T = cfg.KC, cfg.D_MODEL, cfg.T
        xT = self.dr["xT"].rearrange("(c p) t -> p c t", p=128)
        with k.scope() as s:
            xi = [s.sb([128, KC, 512], F32) for _ in range(2)]
            yo = [s.sb([128, D], F32) for _ in range(2)]
            pss = [s.ps([128, 512], F32) for _ in range(4)]
            n = 0
            m = 0
            for t0 in range(0, T, 512):
                ib = xi[(t0 // 512) % 2]
                k.dma("sp", ib[:, :, :], xT[:, :, t0:t0 + 512])
                for tt in range(4):
                    ob = yo[m % 2]
                    m += 1
                    for g in range(KC // 4 if KC >= 4 else 1):
                        ps = pss[n % 4]
                        nn = min(4, KC)
                        for i in range(nn):
                            kc = g * 4 + i
                            k.tr(ps[:, i * 128:(i + 1) * 128], ib[:, kc, tt * 128:(tt + 1) * 128], self.ident)
                        k.copy(ob[:, g * 512:g * 512 + nn * 128], ps[:, 0:nn * 128], en=("act" if n % 2 else "dve"))
                        n += 1
                    k.dma("sp", self.dr["y"][t0 + tt * 128:t0 + (tt + 1) * 128, :], ob[:, :])

    def phase_norm(self, l, half):
        k, cfg = self.k, self.cfg
        KC, D, T = cfg.KC, cfg.D_MODEL, cfg.T
        xT = self.dr["xT"].rearrange("(c p) t -> p c t", p=128)
        hT = self.dr["hT"].rearrange("(c p) t -> p c t", p=128)
        with k.scope() as s:
            xb = [s.sb([128, KC, 512], F32) for _ in range(2)]
            sq = [s.sb([128, KC, 512], BF16) for _ in range(2)]
            hb = [s.sb([128, KC, 512], BF16) for _ in range(2)]
            rr = [s.sb([128, 512], F32) for _ in range(2)]
            tm = [s.sb([128, 512], F32) for _ in range(3)]
            pss = [s.ps([128, 512], F32) for _ in range(2)]
            items = [(t0, min(512, st + L - t0), ci) for (st, L, ci) in cfg.SEGS for t0 in range(st, st + L, 512)]

            def load(i, it):
                t0, w, ci = it
                k.dma("sp", xb[i % 2][:, :, 0:w], xT[:, :, t0:t0 + w])

            def body(i, it):
                t0, w, ci = it
                i2 = i % 2
                x_, q_, h_, r_, ps = xb[i2], sq[i2], hb[i2], rr[i2], pss[i2]
                k.act(q_[:, :, 0:w], x_[:, :, 0:w], AF.Square)
                for kc in range(KC):
                    k.mm(ps[:, 0:w], self.ones, q_[:, kc, 0:w], start=(kc == 0), stop=(kc == KC - 1))
                self.rstd_from_ss(s, r_[:, 0:w], ps[:, 0:w], D, w)
                for kc in range(KC):
                    t_ = tm[kc % 3]
                    k.stt(t_[:, 0:w], x_[:, kc, 0:w], self.mvs(l, 3 * half + 0, kc, ci, ci + 1), r_[:, 0:w],
                          ALU.mult, ALU.mult)
                    k.act(h_[:, kc, 0:w], t_[:, 0:w], AF.Identity, bias=self.mvs(l, 3 * half + 1, kc, ci, ci + 1))
                k.dma("sp", hT[:, :, t0:t0 + w], h_[:, :, 0:w])
            pipeline(items, load, body)

    def phase_post(self, l, half):
        k, cfg = self.k, self.cfg
        KC, D, T = cfg.KC, cfg.D_MODEL, cfg.T
        xT = self.dr["xT"].rearrange("(c p) t -> p c t", p=128)
        yT = self.dr["yT"].rearrange("(c p) t -> p c t", p=128)
        with k.scope() as s:
            xb = [s.sb([128, KC, 512], F32) for _ in range(2)]
            yb = [s.sb([128, KC, 512], F32) for _ in range(2)]
            sq = [s.sb([128, KC, 512], BF16) for _ in range(2)]
            rr = [s.sb([128, 512], F32) for _ in range(2)]
            tm = [s.sb([128, 512], F32) for _ in range(3)]
            pss = [s.ps([128, 512], F32) for _ in range(2)]
            items = [(t0, min(512, st + L - t0), ci) for (st, L, ci) in cfg.SEGS for t0 in range(st, st + L, 512)]

            def load(i, it):
                t0, w, ci = it
                k.dma("sp", yb[i % 2][:, :, 0:w], yT[:, :, t0:t0 + w])
                k.dma("sp", xb[i % 2][:, :, 0:w], xT[:, :, t0:t0 + w])

            def body(i, it):
                t0, w, ci = it
                i2 = i % 2
                x_, y_, q_, r_, ps = xb[i2], yb[i2], sq[i2], rr[i2], pss[i2]
                k.act(q_[:, :, 0:w], y_[:, :, 0:w], AF.Square)
                for kc in range(KC):
                    k.mm(ps[:, 0:w], self.ones, q_[:, kc, 0:w], start=(kc == 0), stop=(kc == KC - 1))
                self.rstd_from_ss(s, r_[:, 0:w], ps[:, 0:w], D, w)
                for kc in range(KC):
                    t_ = tm[kc % 3]
                    k.stt(t_[:, 0:w], y_[:, kc, 0:w], self.mvs(l, 3 * half + 2, kc, ci, ci + 1), r_[:, 0:w],
                          ALU.mult, ALU.mult)
                    k.tt(x_[:, kc, 0:w], x_[:, kc, 0:w], t_[:, 0:w], ALU.add, en="pool")
                k.dma("sp", xT[:, :, t0:t0 + w], x_[:, :, 0:w])
            pipeline(items, load, body)

    def phase_ffn(self, l):
        k, cfg = self.k, self.cfg
        KC, D, T, FF = cfg.KC, cfg.D_MODEL, cfg.T, cfg.D_FF
        JC = FF // 128
        hT = self.dr["hT"].rearrange("(c p) t -> p c t", p=128)
        yT = self.dr["yT"].rearrange("(c p) t -> p c t", p=128)
        wg_src = self.dr["w_ffn_gate"][l].rearrange("(c p) n -> p c n", p=128)
        wu_src = self.dr["w_ffn_up"][l].rearrange("(c p) n -> p c n", p=128)
        wd_src = self.dr["w_ffn_down"][l].rearrange("(c p) n -> p c n", p=128)
        NG = 1024
        CB = 256
        groups = []
        for (st, L, ci) in cfg.SEGS:
            for t0 in range(st, st + L, NG):
                groups.append((t0, min(NG, st + L - t0)))
        merged = []
        for g in groups:
            if merged and merged[-1][1] + g[1] <= NG and merged[-1][0] + merged[-1][1] == g[0]:
                merged[-1] = (merged[-1][0], merged[-1][1] + g[1])
            else:
                merged.append(g)
        for (t0, w) in merged:
            nb = (w + 511) // 512
            with k.scope() as s:
                aT = s.sb([128, JC, w], BF16)
                with k.scope() as s1:
                    h_ = s1.sb([128, KC, w], BF16)
                    k.dma("sp", h_[:, :, :], hT[:, :, t0:t0 + w])
                    wgb = [s1.sb([128, KC, CB], BF16) for _ in range(2)]
                    wub = [s1.sb([128, KC, CB], BF16) for _ in range(2)]
                    sg = [s1.sb([128, 512], F32) for _ in range(2)]
                    pg = [s1.ps([128, 512], F32) for _ in range(2)]
                    pu = [s1.ps([128, 512], F32) for _ in range(2)]
                    n = 0
                    for jb in range(FF // CB):
                        wg, wu = wgb[jb % 2], wub[jb % 2]
                        k.dma("pool", wg[:, :, :], wg_src[:, :, jb * CB:(jb + 1) * CB])
                        k.dma("pool", wu[:, :, :], wu_src[:, :, jb * CB:(jb + 1) * CB])
                        for jj in range(CB // 128):
                            j = jb * (CB // 128) + jj
                            for b in range(nb):
                                c0, c1 = b * 512, min(w, (b + 1) * 512)
                                cw = c1 - c0
                                g_, u_, s_ = pg[n % 2], pu[n % 2], sg[n % 2]
                                n += 1
                                for kc in range(KC):
                                    k.mm(g_[:, 0:cw], wg[:, kc, jj * 128:(jj + 1) * 128], h_[:, kc, c0:c1],
                                         start=(kc == 0), stop=(kc == KC - 1))
                                for kc in range(KC):
                                    k.mm(u_[:, 0:cw], wu[:, kc, jj * 128:(jj + 1) * 128], h_[:, kc, c0:c1],
                                         start=(kc == 0), stop=(kc == KC - 1))
                                k.act(s_[:, 0:cw], g_[:, 0:cw], AF.Silu)
                                k.tt(aT[:, j, c0:c1], s_[:, 0:cw], u_[:, 0:cw], ALU.mult)
                with k.scope() as s2:
                    MB = 2
                    JB = 11 if JC % 11 == 0 else (JC if JC <= 12 else 6)
                    assert JC % JB == 0
                    wdb = [s2.sb([128, JB, MB * 128], BF16) for _ in range(3)]
                    yo = [s2.sb([128, MB, w], F32) for _ in range(2)]
                    pss = [s2.ps([128, 512], F32) for _ in range(8)]
                    nw_ = 0
                    for mg in range(KC // MB):
                        pb = (mg % 2) * 4
                        for jb in range(JC // JB):
                            wd = wdb[nw_ % 3]
                            nw_ += 1
                            k.dma("pool", wd[:, :, :], wd_src[:, jb * JB:(jb + 1) * JB, mg * MB * 128:(mg + 1) * MB * 128])
                            for jj in range(JB):
                                j = jb * JB + jj
                                for mm_ in range(MB):
                                    for b in range(nb):
                                        c0, c1 = b * 512, min(w, (b + 1) * 512)
                                        k.mm(pss[pb + mm_ * 2 + b][:, 0:c1 - c0], wd[:, jj, mm_ * 128:(mm_ + 1) * 128],
                                             aT[:, j, c0:c1], start=(j == 0), stop=(j == JC - 1))
                        yb = yo[mg % 2]
                        for mm_ in range(MB):
                            for b in range(nb):
                                c0, c1 = b * 512, min(w, (b + 1) * 512)
                                k.copy(yb[:, mm_, c0:c1], pss[pb + mm_ * 2 + b][:, 0:c1 - c0],
                                       en=("act" if (mm_ + b) % 2 else "dve"))
                        k.dma("sp", yT[:, mg * MB:(mg + 1) * MB, t0:t0 + w], yb[:, :, :])

    def build(self):
        cfg, k = self.cfg, self.k
        D, T, NL, FF = cfg.D_MODEL, cfg.T, cfg.DEPTH, cfg.D_FF
        self.din("x_in", [T, D])
        self.din("cond", [2, D])
        self.din("c_ident", [128, 128])
        self.din("w_mod", [NL, D, 6 * D])
        self.din("b_mod", [NL, 6 * D])
        for nm in ("norm_mix_pre", "norm_mix_post", "norm_ffn_pre", "norm_ffn_post"):
            self.din(nm, [NL, D])
        self.din("w_ffn_gate", [NL, D, FF])
        self.din("w_ffn_up", [NL, D, FF])
        self.din("w_ffn_down", [NL, FF, D])
        self.declare_mixer_io()
        self.dout("y", [T, D])
        self.dtmp("xT", [D, T])
        self.dtmp("hT", [D, T], BF16)
        self.dtmp("yT", [D, T])
        with k.scope() as S:
            self.phase_consts(S)
            self.phase_mod(S)
            self.mark("mod")
            self.phase_in()
            self.mark("in")
            for l in range(NL):
                if getattr(cfg, "MIXERS", True):
                    self.phase_norm(l, 0)
                    self.mark(f"L{l}.norm1")
                    if l % 2 == 0:
                        if getattr(cfg, "SKIP_AB", False):
                            continue_ = True
                        else:
                            self.mixer_ab(l // 2)
                            self.phase_post(l, 0)
                            self.mark(f"L{l}.post1")
                    else:
                        self.mixer_c(l // 2)
                        self.phase_post(l, 0)
                        self.mark(f"L{l}.post1")
                self.phase_norm(l, 1)
                self.mark(f"L{l}.norm2")
                self.phase_ffn(l)
                self.mark(f"L{l}.ffn")
                self.phase_post(l, 1)
                self.mark(f"L{l}.post2")
            self.phase_out()
            self.mark("out")
        k.es.close()
        return self.nc

    def declare_mixer_io(self):
        declare_mixer_c(self)
        declare_mixer_ab(self)

    def mixer_ab(self, e):
        mixer_ab_impl(self, e)

    def mixer_c(self, o):
        mixer_c_impl(self, o)


def host_consts(cfg):
    out = {"c_ident": np.eye(128, dtype=np.float32)}
    out.update(hy_host_consts(cfg))
    out.update(ab_host_consts(cfg))
    return out


def make_in_maps(cfg, inputs):
    consts = host_consts(cfg)
    maps = []
    for core in range(cfg.N_CORES):
        m = dict(consts)
        xs = inputs["x_sample"][core]
        xp = inputs["x_prompt"][core * cfg.NPROMPT:(core + 1) * cfg.NPROMPT].reshape(-1, cfg.D_MODEL)
        m["x_in"] = np.ascontiguousarray(np.concatenate([xs, xp], axis=0))
        m["cond"] = np.ascontiguousarray(np.stack([inputs["c"][core], inputs["c_ctx"]], axis=0))
        for nm in ("w_mod", "b_mod", "norm_mix_pre", "norm_mix_post", "norm_ffn_pre", "norm_ffn_post",
                   "w_ffn_gate", "w_ffn_up", "w_ffn_down") + MIXC_WEIGHTS + MIXAB_WEIGHTS:
            m[nm] = inputs[nm]
        ab_core_inputs(cfg, m, inputs, core)
        maps.append(m)
    return maps


def run(cfg, inputs, trace=False):
    prog = Prog(cfg)
    nc = prog.build()
    maps = make_in_maps(cfg, inputs)
    res = run_bass_kernel_spmd(nc, maps, core_ids=list(range(cfg.N_CORES)))
    ys = np.stack([r["y"][:cfg.DEC_SEQ] for r in res.results], axis=0)
    yp = np.concatenate([r["y"][cfg.DEC_SEQ:].reshape(cfg.NPROMPT, cfg.SEQ, cfg.D_MODEL) for r in res.results], axis=0)
    nsr = np.concatenate([r["nsr"] for r in res.results], axis=0)
    nsg = np.concatenate([r["nsg"] for r in res.results], axis=0)
    return (ys, yp, nsr, nsg), res, prog


_CACHE = {}


def kernel(**inputs):
    cfg = Cfg()
    inputs = {k_: np.asarray(v) for k_, v in inputs.items()}
    outs, res, prog = run(cfg, inputs)
    ys, yp, nsr, nsg = outs
    return (np.ascontiguousarray(yp, dtype=np.float32), np.ascontiguousarray(ys, dtype=np.float32),
            np.ascontiguousarray(nsr, dtype=np.float32), np.ascontiguousarray(nsg, dtype=np.float32))


def pipeline(items, load, body):
    if not items:
        return
    load(0, items[0])
    for i, it in enumerate(items):
        if i + 1 < len(items):
            load(i + 1, items[i + 1])
        body(i, it)


def token_groups(cfg, NG=1024):
    groups = []
    for (st, L, ci) in cfg.SEGS:
        for t0 in range(st, st + L, NG):
            groups.append((t0, min(NG, st + L - t0)))
    merged = []
    for g in groups:
        if merged and merged[-1][1] + g[1] <= NG and merged[-1][0] + merged[-1][1] == g[0]:
            merged[-1] = (merged[-1][0], merged[-1][1] + g[1])
        else:
            merged.append(g)
    return merged


def proj_fm(P, w_dram, n_in, ncols, in_dram, out_dram, out_dt, groups, col0=0, out_row0=0, CB=256):
    k = P.k
    KCi = n_in // 128
    wsrc = w_dram.rearrange("(c p) n -> p c n", p=128)
    inT = in_dram.rearrange("(c p) t -> p c t", p=128)
    blocks = []
    c = 0
    while c < ncols:
        bw = min(CB, ncols - c)
        blocks.append((c, bw))
        c += bw
    for (t0, w) in groups:
        nb = (w + 511) // 512
        with k.scope() as s:
            h_ = s.sb([128, KCi, w], BF16)
            k.dma("sp", h_[:, :, :], inT[:, :, t0:t0 + w])
            wbs = [s.sb([128, KCi, CB], BF16) for _ in range(2)]
            obs = [s.sb([128, CB // 128, w], out_dt) for _ in range(2)]
            pss = [s.ps([128, 512], F32) for _ in range(4)]
            n = 0
            for bi, (c0, bw) in enumerate(blocks):
                wb, ob = wbs[bi % 2], obs[bi % 2]
                k.dma("pool", wb[:, :, 0:bw], wsrc[:, :, col0 + c0:col0 + c0 + bw])
                nch = (bw + 127) // 128
                for jj in range(nch):
                    m = min(128, bw - jj * 128)
                    for b in range(nb):
                        a0, a1 = b * 512, min(w, (b + 1) * 512)
                        ps = pss[n % 4]
                        for kc in range(KCi):
                            k.mm(ps[0:m, 0:a1 - a0], wb[:, kc, jj * 128:jj * 128 + m], h_[:, kc, a0:a1],
                                 start=(kc == 0), stop=(kc == KCi - 1))
                        k.copy(ob[0:m, jj, a0:a1], ps[0:m, 0:a1 - a0], en=("act" if n % 2 else "dve"))
                        n += 1
                r0 = out_row0 + c0
                if bw % 128 == 0:
                    k.dma("sp", out_dram[r0:r0 + bw, t0:t0 + w].rearrange("(c p) t -> p c t", p=128), ob[:, 0:nch, :])
                else:
                    assert bw < 128
                    k.dma("sp", out_dram[r0:r0 + bw, t0:t0 + w], ob[0:bw, 0, :])


def hy_sizes(L):
    N = 2 * L
    NK = L // 128
    NFC = (L + 1 + 127) // 128
    return N, NK, NFC


def hy_host_consts(cfg):
    out = {}
    for L in sorted({cfg.DEC_SEQ, cfg.SEQ}):
        N, NK, NFC = hy_sizes(L)
        FP = NFC * 128
        n = np.arange(L, dtype=np.float64)
        f = np.arange(FP, dtype=np.float64)
        ang = 2.0 * np.pi * ((n[:, None] * f[None, :]) % N) / N
        valid = (f <= L)[None, :]
        C = np.where(valid, np.cos(ang), 0.0)
        S = np.where(valid, -np.sin(ang), 0.0)
        fw = np.stack([C, S]).reshape(2, NK, 128, NFC, 128).transpose(0, 3, 2, 1, 4)
        out[f"hy_dft_{L}"] = np.ascontiguousarray(fw).astype(NPBF)
        wf = np.where((f == 0) | (f == L), 1.0, 2.0) * (f <= L) / N
        Ci = (wf[:, None] * np.cos(ang.T))
        Si = (-wf[:, None] * np.sin(ang.T))
        nbw = min(512, L)
        iv = np.stack([Ci, Si]).reshape(2, NFC, 128, L // nbw, nbw).transpose(3, 2, 0, 1, 4)
        out[f"hy_idft_{L}"] = np.ascontiguousarray(iv).astype(NPBF)
        t = np.linspace(0.0, 1.0, L, dtype=np.float32).astype(np.float64)
        w_ang = 2.0 * math.pi * np.arange(L, dtype=np.float32).astype(np.float64) / L
        bands = np.linspace(1e-4, cfg.HY_BANDS - 1, cfg.HY_BANDS, dtype=np.float32).astype(np.float64)
        z = np.concatenate([t[:, None], np.cos(bands[None] * w_ang[:, None]), -np.sin(bands[None] * w_ang[:, None])], -1)
        out[f"hy_zT_{L}"] = np.ascontiguousarray(z.T).astype(np.float32)
        out[f"hy_negt_{L}"] = np.ascontiguousarray((-t).reshape(NK, 128).T).astype(np.float32)
    D = cfg.D_MODEL
    deltas = np.abs(np.linspace(math.log(cfg.HY_TARGET) / cfg.HY_SLOW_DECAY, math.log(cfg.HY_TARGET) / cfg.HY_FAST_DECAY,
                                D, dtype=np.float32))
    out["hy_deltas"] = np.ascontiguousarray(np.broadcast_to(deltas[None, :], (128, D))).astype(np.float32)
    return out


def _range_reduce(k, y, m):
    PI = math.pi
    for _ in range(2):
        k.ts(m, y, -PI, 2.0 * PI, op0=ALU.is_lt, op1=ALU.mult)
        k.tt(y, y, m, ALU.add)
        k.ts(m, y, PI, -2.0 * PI, op0=ALU.is_gt, op1=ALU.mult)
        k.tt(y, y, m, ALU.add)


def hy_filters(P, o, L):
    k, cfg = P.k, P.cfg
    D, HF, EMB = cfg.D_MODEL, cfg.HY_FFN, cfg.HY_EMB
    N, NK, NFC = hy_sizes(L)
    BW = min(512, L)
    with k.scope() as s:
        w1 = s.sb([EMB, HF], F32)
        k.dma("sp", w1[:, :], P.dr["hy_w1"][o])
        w2 = s.sb([HF, HF], F32)
        k.dma("sp", w2[:, :], P.dr["hy_w2"][o])
        w3 = s.sb([HF, 2 * D], F32)
        k.dma("sp", w3[:, :], P.dr["hy_w3"][o])
        zT = s.sb([EMB, L], F32)
        k.dma("sp", zT[:, :], P.dr[f"hy_zT_{L}"])
        vec = s.sb([HF, 4], F32)
        k.dma("sp", vec[:, 0:1], P.dr["hy_b1"][o].rearrange("(p o) -> p o", o=1))
        k.dma("sp", vec[:, 1:2], P.dr["hy_freq"][o].rearrange("(p o) -> p o", o=1))
        k.dma("sp", vec[:, 2:3], P.dr["hy_b2"][o].rearrange("(p o) -> p o", o=1))
        fb = s.sb([HF, 2], F32)
        k.tt(fb[:, 0:1], vec[:, 0:1], vec[:, 1:2], ALU.mult)
        k.tt(fb[:, 1:2], vec[:, 2:3], vec[:, 1:2], ALU.mult)
        negt = s.sb([128, NK], F32)
        k.dma("sp", negt[:, :], P.dr[f"hy_negt_{L}"])
        dl = s.sb([128, D], F32)
        k.dma("sp", dl[:, :], P.dr["hy_deltas"])
        bias = s.sb([1, D], F32)
        k.dma("sp", bias[:, :], P.dr["hy_bias"][o].rearrange("(o n) -> o n", o=1))
        h1 = s.sb([HF, L], F32)
        h2 = s.sb([HF, L], F32)
        mk = s.sb([HF, BW], F32)
        ps = s.ps([128, 512], F32)
        for (src, wgt, kdim, dst, fbi) in ((zT, w1, EMB, h1, 0), (h1, w2, HF, h2, 1)):
            for b0 in range(0, L, BW):
                k.mm(ps[0:HF, 0:BW], wgt[0:kdim, :], src[0:kdim, b0:b0 + BW])
                y = dst[:, b0:b0 + BW]
                k.ts(y, ps[0:HF, 0:BW], vec[:, 1:2], fb[:, fbi:fbi + 1], op0=ALU.mult, op1=ALU.add)
                _range_reduce(k, y, mk[:, :])
                k.act(y, y, AF.Sin)
        win = s.sb([128, D], F32)
        hf = s.sb([128, D], F32)
        hb = s.sb([128, D], F32)
        At = [s.sb([128, D], BF16) for _ in range(2)]
        Bt = [s.sb([128, D], BF16) for _ in range(2)]
        pss = [s.ps([128, 512], F32) for _ in range(2)]
        CW = min(512, D)
        n = 0
        for nk in range(NK):
            k.act(win[:, :], dl[:, :], AF.Exp, scale=negt[:, nk:nk + 1])
            for half, dst in ((0, hf), (1, hb)):
                for c0 in range(0, D, CW):
                    p_ = pss[n % 2]
                    n += 1
                    k.mm(p_[:, 0:CW], h2[0:HF, nk * 128:(nk + 1) * 128], w3[0:HF, half * D + c0:half * D + c0 + CW])
                    k.tt(dst[:, c0:c0 + CW], p_[:, 0:CW], win[:, c0:c0 + CW], ALU.mult)
            if nk == 0:
                k.memset(hb[0:1, :], 0.0)
                k.tt(hf[0:1, :], hf[0:1, :], bias[0:1, :], ALU.add)
            a_, b_ = At[nk % 2], Bt[nk % 2]
            k.tt(a_[:, :], hf[:, :], hb[:, :], ALU.add, en="pool")
            k.tt(b_[:, :], hf[:, :], hb[:, :], ALU.subtract, en="pool")
            k.dma("sp", P.dr["hy_A"][nk * 128:(nk + 1) * 128, :], a_[:, :])
            k.dma("sp", P.dr["hy_B"][nk * 128:(nk + 1) * 128, :], b_[:, :])


def hy_fwd(P, L, srcA, srcB, emit, NH=1):
    k, cfg = P.k, P.cfg
    D = cfg.D_MODEL
    N, NK, NFC = hy_sizes(L)
    HW = min(512, D)
    CW = min(NH * HW, D)
    NH = CW // HW
    dft = P.dr[f"hy_dft_{L}"]
    for c0 in range(0, D, CW):
        with k.scope() as s:
            A_ = s.sb([128, NK, CW], BF16)
            k.dma("sp", A_[:, :, :], srcA[:, c0:c0 + CW].rearrange("(k p) c -> p k c", p=128))
            if srcB is srcA:
                B_ = A_
            else:
                B_ = s.sb([128, NK, CW], BF16)
                k.dma("sp", B_[:, :, :], srcB[:, c0:c0 + CW].rearrange("(k p) c -> p k c", p=128))
            dcs = [s.sb([128, NK, 128], BF16) for _ in range(2)]
            dss = [s.sb([128, NK, 128], BF16) for _ in range(2)]
            pre = [[s.ps([128, 512], F32) for _ in range(NH)] for _ in range(2)]
            pim = [[s.ps([128, 512], F32) for _ in range(NH)] for _ in range(2)]

            def load(i, fc):
                k.dma("sp", dcs[i % 2][:, :, :], dft[0, fc])
                k.dma("sp", dss[i % 2][:, :, :], dft[1, fc])

            def body(i, fc):
                dc, ds = dcs[i % 2], dss[i % 2]
                for hf in range(NH):
                    p_re, p_im = pre[i % 2][hf], pim[i % 2][hf]
                    for nk in range(NK):
                        k.mm(p_re[:, 0:HW], dc[:, nk, :], A_[:, nk, hf * HW:(hf + 1) * HW], start=(nk == 0), stop=(nk == NK - 1))
                    for nk in range(NK):
                        k.mm(p_im[:, 0:HW], ds[:, nk, :], B_[:, nk, hf * HW:(hf + 1) * HW], start=(nk == 0), stop=(nk == NK - 1))
                    emit(s, fc, c0 + hf * HW, HW, p_re, p_im)
            pipeline(list(range(NFC)), load, body)


def hy_filter_spectrum(P, L):
    k = P.k
    st = {"n": 0}

    def emit(s, fc, c0, CW, p_re, p_im):
        if "s" not in st or st["s"] is not s:
            st["s"] = s
            st["o"] = [s.sb([128, 2, CW], F32) for _ in range(2)]
        ob = st["o"][st["n"] % 2]
        st["n"] += 1
        k.copy(ob[:, 0, :], p_re[:, 0:CW], en="dve")
        k.copy(ob[:, 1, :], p_im[:, 0:CW], en="act")
        k.dma("sp", P.dr["hy_H"][:, fc * 128:(fc + 1) * 128, c0:c0 + CW].rearrange("s p c -> p s c"), ob[:, :, :])
    hy_fwd(P, L, P.dr["hy_A"][0:L, :], P.dr["hy_B"][0:L, :], emit)


def hy_data_spectrum(P, L, tok0):
    k = P.k
    st = {"n": 0}

    def emit(s, fc, c0, CW, p_re, p_im):
        if "s" not in st or st["s"] is not s:
            st["s"] = s
            st["h"] = [s.sb([128, 2, CW], F32) for _ in range(2)]
            st["t"] = [s.sb([128, 4, CW], F32) for _ in range(2)]
            st["p"] = [s.sb([128, 2, CW], BF16) for _ in range(2)]
        i2 = st["n"] % 2
        st["n"] += 1
        hb, tb, pb = st["h"][i2], st["t"][i2], st["p"][i2]
        k.dma("sp", hb[:, :, :], P.dr["hy_H"][:, fc * 128:(fc + 1) * 128, c0:c0 + CW].rearrange("s p c -> p s c"))
        k.tt(tb[:, 0, :], p_re[:, 0:CW], hb[:, 0, :], ALU.mult)
        k.tt(tb[:, 1, :], p_im[:, 0:CW], hb[:, 1, :], ALU.mult)
        k.tt(tb[:, 2, :], p_re[:, 0:CW], hb[:, 1, :], ALU.mult)
        k.tt(tb[:, 3, :], p_im[:, 0:CW], hb[:, 0, :], ALU.mult)
        k.tt(pb[:, 0, :], tb[:, 0, :], tb[:, 1, :], ALU.subtract, en="pool")
        k.tt(pb[:, 1, :], tb[:, 2, :], tb[:, 3, :], ALU.add, en="pool")
        k.dma("sp", P.dr["hy_P"][:, fc * 128:(fc + 1) * 128, c0:c0 + CW].rearrange("s p c -> p s c"), pb[:, :, :])
    src = P.dr["hy_wtok"][tok0:tok0 + L, :]
    hy_fwd(P, L, src, src, emit, NH=2)


def hy_inverse(P, L, tok0):
    k, cfg = P.k, P.cfg
    D = cfg.D_MODEL
    N, NK, NFC = hy_sizes(L)
    FK = 2 * NFC
    PZ = 11 if FK % 11 == 0 else FK
    assert FK % PZ == 0 and PZ <= 12
    BWn = min(512, L)
    CW = min(512, D)
    NCH = CW // 128
    idft = P.dr[f"hy_idft_{L}"]
    for c0 in range(0, D, CW):
        with k.scope() as s:
            P_ = s.sb([128, 2, NFC, CW], BF16)
            for cs in range(2):
                k.dma("sp", P_[:, cs, :, :], P.dr["hy_P"][cs, 0:NFC * 128, c0:c0 + CW].rearrange("(c p) n -> p c n", p=128))
            idb = [s.sb([128, PZ, BWn], BF16) for _ in range(3)]
            x0b = [s.sb([128, NCH, BWn], BF16) for _ in range(2)]
            gb = [s.sb([128, NCH, BWn], BF16) for _ in range(2)]
            pss = [s.ps([128, 512], F32) for _ in range(8)]
            ni = 0
            for nb in range(L // BWn):
                pset = pss[(nb % 2) * 4:(nb % 2) * 4 + 4]
                xb, g_ = x0b[nb % 2], gb[nb % 2]
                t0 = tok0 + nb * BWn
                k.dma("sp", xb[:, :, :], P.dr["hy_x0"][c0:c0 + CW, t0:t0 + BWn].rearrange("(c p) t -> p c t", p=128))
                for pz in range(FK // PZ):
                    ib = idb[ni % 3]
                    ni += 1
                    k.dma("sp", ib[:, :, :], idft[nb].rearrange("p s c n -> p (s c) n")[:, pz * PZ:(pz + 1) * PZ, :])
                    for i in range(PZ):
                        fk = pz * PZ + i
                        cs, fc = fk // NFC, fk % NFC
                        for c in range(NCH):
                            k.mm(pset[c][:, 0:BWn], P_[:, cs, fc, c * 128:(c + 1) * 128], ib[:, i, :],
                                 start=(fk == 0), stop=(fk == FK - 1))
                for c in range(NCH):
                    k.tt(g_[:, c, :], pset[c][:, 0:BWn], xb[:, c, :], ALU.mult)
                k.dma("sp", P.dr["hy_gT"][c0:c0 + CW, t0:t0 + BWn].rearrange("(c p) t -> p c t", p=128), g_[:, :, :])


def hy_conv(P, o):
    k, cfg = P.k, P.cfg
    D = cfg.D_MODEL
    CW = min(256, D)
    NCH = CW // 128
    NC3 = 3 * D // 128
    TB = min(1024, max(L for (_, L, _) in cfg.SEGS))
    with k.scope() as s:
        cw = s.sb([128, 3, NC3], F32)
        for j in range(3):
            P.load_fm_vec(s, cw[:, j, :], P.dr["hy_conv_w"][o, j], NC3)
        us = [[s.sb([128, NCH, TB + 2], F32) for _ in range(3)] for _ in range(2)]
        cs_ = [s.sb([128, NCH, TB], F32) for _ in range(3)]
        x0o = [s.sb([128, NCH, TB], BF16) for _ in range(2)]
        wo = [s.sb([128, NCH, TB], BF16) for _ in range(2)]
        wt = [s.sb([128, CW], BF16) for _ in range(3)]
        psb = [s.ps([128, 512], BF16) for _ in range(3)]
        items = []
        for (st, L, ci) in cfg.SEGS:
            for t0 in range(st, st + L, TB):
                tw = min(TB, st + L - t0)
                for c0 in range(0, D, CW):
                    items.append((st, L, t0, tw, c0))
        nt = [0]

        def load(i, it):
            st, L, t0, tw, c0 = it
            lo = max(st, t0 - 1)
            hi = min(st + L, t0 + tw + 1)
            for part in range(3):
                ub = us[i % 2][part]
                if lo == t0:
                    k.memset(ub[:, :, 0:1], 0.0, en="pool")
                if hi == t0 + tw:
                    k.memset(ub[:, :, tw + 1:tw + 2], 0.0, en="pool")
                r0 = part * D + c0
                k.dma("sp", ub[:, :, lo - (t0 - 1):hi - (t0 - 1)],
                      P.dr["hy_uT"][r0:r0 + CW, lo:hi].rearrange("(c p) t -> p c t", p=128))

        def body(i, it):
            st, L, t0, tw, c0 = it
            u3 = us[i % 2]
            for part in range(3):
                ub = u3[part]
                cb = cs_[part]
                for c in range(NCH):
                    ch = (part * D + c0) // 128 + c
                    k.ts(cb[:, c, 0:tw], ub[:, c, 1:tw + 1], cw[:, 1, ch:ch + 1], None, op0=ALU.mult, en="pool")
                    k.stt(cb[:, c, 0:tw], ub[:, c, 0:tw], cw[:, 0, ch:ch + 1], cb[:, c, 0:tw], ALU.mult, ALU.add)
                    k.stt(cb[:, c, 0:tw], ub[:, c, 2:tw + 2], cw[:, 2, ch:ch + 1], cb[:, c, 0:tw], ALU.mult, ALU.add)
            xo, w_ = x0o[i % 2], wo[i % 2]
            k.copy(xo[:, :, 0:tw], cs_[0][:, :, 0:tw], en="act")
            k.tt(w_[:, :, 0:tw], cs_[2][:, :, 0:tw], cs_[1][:, :, 0:tw], ALU.mult, en="pool")
            k.dma("sp", P.dr["hy_x0"][c0:c0 + CW, t0:t0 + tw].rearrange("(c p) t -> p c t", p=128), xo[:, :, 0:tw])
            for j in range(tw // 128):
                n_ = nt[0]
                nt[0] += 1
                pb, wt_ = psb[n_ % 3], wt[n_ % 3]
                for c in range(NCH):
                    k.tr(pb[:, c * 128:(c + 1) * 128], w_[:, c, j * 128:(j + 1) * 128], P.identh)
                k.copy(wt_[:, 0:CW], pb[:, 0:CW], en=("act" if n_ % 2 else "dve"))
                k.dma("sp", P.dr["hy_wtok"][t0 + j * 128:t0 + (j + 1) * 128, c0:c0 + CW], wt_[:, 0:CW])
        pipeline(items, load, body)


def mixer_c_impl(P, o):
    k, cfg = P.k, P.cfg
    D = cfg.D_MODEL
    groups = token_groups(cfg)
    proj_fm(P, P.dr["w_in_c"][o], D, 3 * D, P.dr["hT"], P.dr["hy_uT"], F32, groups)
    P.mark(f"c{o}.inproj")
    hy_conv(P, o)
    P.mark(f"c{o}.conv")
    done_L = None
    for (st, L, ci) in cfg.SEGS:
        if L != done_L:
            hy_filters(P, o, L)
            P.mark(f"c{o}.filters{L}")
            hy_filter_spectrum(P, L)
            P.mark(f"c{o}.fspec{L}")
            done_L = L
        hy_data_spectrum(P, L, st)
        P.mark(f"c{o}.dspec{st}")
        hy_inverse(P, L, st)
        P.mark(f"c{o}.inv{st}")
    proj_fm(P, P.dr["w_out_c"][o], D, D, P.dr["hy_gT"], P.dr["yT"], F32, groups)
    P.mark(f"c{o}.outproj")


def declare_mixer_c(P):
    cfg = P.cfg
    D, T, NO = cfg.D_MODEL, cfg.T, cfg.N_ODD
    P.din("w_in_c", [NO, D, 3 * D])
    P.din("hy_conv_w", [NO, 3, 3 * D])
    P.din("w_out_c", [NO, D, D])
    P.din("hy_w1", [NO, cfg.HY_EMB, cfg.HY_FFN])
    P.din("hy_b1", [NO, cfg.HY_FFN])
    P.din("hy_freq", [NO, cfg.HY_FFN])
    P.din("hy_w2", [NO, cfg.HY_FFN, cfg.HY_FFN])
    P.din("hy_b2", [NO, cfg.HY_FFN])
    P.din("hy_w3", [NO, cfg.HY_FFN, 2 * D])
    P.din("hy_bias", [NO, D])
    P.din("hy_deltas", [128, D])
    Lmax = max(cfg.DEC_SEQ, cfg.SEQ)
    for L in sorted({cfg.DEC_SEQ, cfg.SEQ}):
        N, NK, NFC = hy_sizes(L)
        P.din(f"hy_dft_{L}", [2, NFC, 128, NK, 128], BF16)
        P.din(f"hy_idft_{L}", [L // min(512, L), 128, 2, NFC, min(512, L)], BF16)
        P.din(f"hy_zT_{L}", [cfg.HY_EMB, L])
        P.din(f"hy_negt_{L}", [128, NK])
    FPmax = hy_sizes(Lmax)[2] * 128
    P.dtmp("hy_uT", [3 * D, T])
    P.dtmp("hy_x0", [D, T], BF16)
    P.dtmp("hy_wtok", [T, D], BF16)
    P.dtmp("hy_gT", [D, T], BF16)
    P.dtmp("hy_A", [Lmax, D], BF16)
    P.dtmp("hy_B", [Lmax, D], BF16)
    P.dtmp("hy_H", [2, FPmax, D])
    P.dtmp("hy_P", [2, FPmax, D], BF16)


MIXC_WEIGHTS = ("w_in_c", "hy_conv_w", "w_out_c", "hy_w1", "hy_b1", "hy_freq", "hy_w2", "hy_b2", "hy_w3", "hy_bias")


MIXAB_WEIGHTS = ("w_in_ab", "w_out_ab", "ret_decay", "ret_gn_w", "gdn_conv_w", "gdn_a_log", "gdn_dt_bias", "gdn_norm_w")
CH = 128
NEG = -30000.0


def ab_host_consts(cfg):
    out = {}
    i = np.arange(CH)
    s_, t_ = i[:, None], i[None, :]
    relf = np.where(t_ >= s_, (t_ - s_).astype(np.float32), 1e9)
    relb = np.where(s_ >= t_, (s_ - t_).astype(np.float32), 1e9)
    out["ab_rel"] = np.stack([relf, relb]).astype(np.float32)
    qd = np.stack([np.broadcast_to((i + 1.0)[None, :], (CH, CH)), np.broadcast_to((CH - i * 1.0)[None, :], (CH, CH))])
    out["ab_qexp"] = np.ascontiguousarray(qd).astype(np.float32)
    out["ab_kexp"] = np.stack([CH - 1.0 - i, i * 1.0], axis=1).astype(np.float32)
    Uf = (s_ <= t_).astype(np.float32)
    Ub = (s_ >= t_).astype(np.float32)
    out["ab_U"] = np.stack([Uf, Ub]).astype(np.float32)
    out["ab_negm"] = np.stack([np.where(s_ <= t_, 0.0, NEG), np.where(s_ >= t_, 0.0, NEG)]).astype(np.float32)
    out["ab_strict"] = np.stack([(s_ < t_), (s_ > t_)]).astype(np.float32)
    lm = np.zeros((2, 7, CH, CH), np.float32)
    for lv in range(7):
        m = 1 << lv
        tt_, ss_ = i[:, None], i[None, :]
        pat = ((tt_ // (2 * m)) == (ss_ // (2 * m))) & ((tt_ % (2 * m)) >= m) & ((ss_ % (2 * m)) < m)
        lm[0, lv] = -pat.astype(np.float32)
        lm[1, lv] = -pat.T.astype(np.float32)
    out["ab_lm"] = lm
    out["ab_lmT"] = np.ascontiguousarray(lm.transpose(0, 1, 3, 2))
    L = cfg.DEC_SEQ
    rows = L // cfg.GRID_W
    r = np.repeat(np.arange(rows, dtype=np.float32), cfg.GRID_W)
    col = (np.arange(rows * cfg.GRID_W) % cfg.GRID_W).astype(np.float32)
    n_freq = cfg.DK_A // 4
    inv = (np.float32(cfg.ROPE_BASE) ** (-np.arange(n_freq, dtype=np.float32) / n_freq)).astype(np.float32)
    ang = np.concatenate([r[:, None] * inv, col[:, None] * inv], axis=-1).astype(np.float32)
    out["ab_rope"] = np.ascontiguousarray(np.stack([np.cos(ang).T, np.sin(ang).T])).astype(np.float32)
    return out


def ab_core_inputs(cfg, m, inputs, core):
    m["state_ret"] = np.ascontiguousarray(inputs["state_ret"][core])
    m["state_gdn"] = np.ascontiguousarray(inputs["state_gdn"][core])


def declare_mixer_ab(P):
    cfg = P.cfg
    D, T, NE = cfg.D_MODEL, cfg.T, cfg.N_EVEN
    HA, DKA, DVA, HB, DKB, DVB = cfg.H_A, cfg.DK_A, cfg.DV_A, cfg.H_B, cfg.DK_B, cfg.DV_B
    P.din("w_in_ab", [NE, D, cfg.PROJ_AB])
    P.din("w_out_ab", [NE, cfg.MIX_AB_OUT, D])
    P.din("ret_decay", [NE, 2, HA])
    P.din("ret_gn_w", [NE, HA * DVA])
    P.din("gdn_conv_w", [NE, cfg.CONV_B, 3 * HB * DKB])
    P.din("gdn_a_log", [NE, 2, HB])
    P.din("gdn_dt_bias", [NE, 2, HB])
    P.din("gdn_norm_w", [NE, DVB])
    P.din("state_ret", [NE, 2, HA, DKA, DVA])
    P.din("state_gdn", [NE, 2, HB, DKB, DVB])
    P.din("ab_rel", [2, CH, CH])
    P.din("ab_qexp", [2, CH, CH])
    P.din("ab_kexp", [CH, 2])
    P.din("ab_U", [2, CH, CH])
    P.din("ab_negm", [2, CH, CH])
    P.din("ab_strict", [2, CH, CH])
    P.din("ab_lm", [2, 7, CH, CH])
    P.din("ab_lmT", [2, 7, CH, CH])
    P.din("ab_rope", [2, 128, cfg.DEC_SEQ])
    P.dout("nsr", [cfg.NPROMPT, NE, 2, HA, DKA, DVA])
    P.dout("nsg", [cfg.NPROMPT, NE, 2, HB, DKB, DVB])
    P.dtmp("ab_pT", [cfg.PROJ_AB, T])
    P.dtmp("ab_rqk", [2 * HA * DKA, T], BF16)
    P.dtmp("ab_rk_tok", [T, HA * DKA], BF16)
    P.dtmp("ab_rv_tok", [T, HA * DVA], BF16)
    P.dtmp("ab_rg_tok", [T, HA * DVA], BF16)
    P.dtmp("ab_gT", [3 * HB * DKB, T], BF16)
    P.dtmp("ab_gtok", [T, 2 * HB * DKB], BF16)
    P.dtmp("ab_gz_tok", [T, HB * DVB], BF16)
    P.dtmp("ab_gbT", [32, T])
    P.dtmp("ab_gb", [T, 32])
    P.dtmp("ab_o1r", [T, HA * DVA])
    P.dtmp("ab_o1g", [T, HB * DVB])
    P.dtmp("ab_catT", [cfg.MIX_AB_OUT, T], BF16)


def fm_to_tm(P, src, row0, nrows, dst, col0, src_dt, dst_dt, func=None):
    k, cfg = P.k, P.cfg
    T = cfg.T
    G = min(4, nrows // 128)
    ident = P.ident if src_dt == F32 else P.identh
    with k.scope() as s:
        ib = [s.sb([128, G, 512], src_dt) for _ in range(2)]
        ob = [s.sb([128, G * 128], dst_dt) for _ in range(3)]
        pss = [s.ps([128, 512], src_dt) for _ in range(3)]
        items = [(t0, min(512, T - t0), r0) for t0 in range(0, T, 512) for r0 in range(0, nrows, G * 128)]
        cnt = [0]

        def load(i, it):
            t0, tw, r0 = it
            k.dma("sp", ib[i % 2][:, :, 0:tw], src[row0 + r0:row0 + r0 + G * 128, t0:t0 + tw].rearrange("(c p) t -> p c t", p=128))

        def body(i, it):
            t0, tw, r0 = it
            i_ = ib[i % 2]
            for tj in range(tw // 128):
                m = cnt[0]
                cnt[0] += 1
                ps, o_ = pss[m % 3], ob[m % 3]
                for g in range(G):
                    k.tr(ps[:, g * 128:(g + 1) * 128], i_[:, g, tj * 128:(tj + 1) * 128], ident)
                if func is not None:
                    k.act(o_[:, :], ps[:, 0:G * 128], func)
                else:
                    k.copy(o_[:, :], ps[:, 0:G * 128], en=("act" if m % 2 else "dve"))
                k.dma("sp", dst[t0 + tj * 128:t0 + (tj + 1) * 128, col0 + r0:col0 + r0 + G * 128], o_[:, :])
        pipeline(items, load, body)


def ab_rope(P):
    k, cfg = P.k, P.cfg
    HA, DKA = cfg.H_A, cfg.DK_A
    qs = DKA ** -0.5
    with k.scope() as s:
        xs = [[s.sb([128, 512], F32) for _ in range(2)] for _ in range(2)]
        cs = [[s.sb([128, 512], F32) for _ in range(2)] for _ in range(2)]
        tm = [s.sb([128, 512], F32) for _ in range(4)]
        ob = [s.sb([128, 2, 512], BF16) for _ in range(2)]
        n = 0
        for si, (st, L, ci) in enumerate(cfg.SEGS):
            for t0 in range(st, st + L, 512):
                tw = min(512, st + L - t0)
                c_, s_ = cs[(t0 // 512) % 2]
                if si == 0:
                    k.dma("sp", c_[:, 0:tw], P.dr["ab_rope"][0, :, t0 - st:t0 - st + tw])
                    k.dma("sp", s_[:, 0:tw], P.dr["ab_rope"][1, :, t0 - st:t0 - st + tw])
                for qk in range(2):
                    sc = qs if qk == 0 else 1.0
                    for h in range(HA):
                        x1, x2 = xs[n % 2]
                        o_ = ob[n % 2]
                        n += 1
                        r0 = qk * HA * DKA + h * DKA
                        k.dma("sp", x1[:, 0:tw], P.dr["ab_pT"][r0:r0 + 128, t0:t0 + tw])
                        k.dma("sp", x2[:, 0:tw], P.dr["ab_pT"][r0 + 128:r0 + 256, t0:t0 + tw])
                        if si == 0:
                            k.tt(tm[0][:, 0:tw], x1[:, 0:tw], c_[:, 0:tw], ALU.mult)
                            k.tt(tm[1][:, 0:tw], x2[:, 0:tw], s_[:, 0:tw], ALU.mult, en="pool")
                            k.tt(tm[2][:, 0:tw], x1[:, 0:tw], s_[:, 0:tw], ALU.mult)
                            k.tt(tm[3][:, 0:tw], x2[:, 0:tw], c_[:, 0:tw], ALU.mult, en="pool")
                            k.stt(o_[:, 0, 0:tw], tm[0][:, 0:tw], sc, tm[1][:, 0:tw], ALU.mult, ALU.subtract) if sc == 1.0 else None
                            if sc != 1.0:
                                k.tt(tm[0][:, 0:tw], tm[0][:, 0:tw], tm[1][:, 0:tw], ALU.subtract)
                                k.ts(o_[:, 0, 0:tw], tm[0][:, 0:tw], sc, None, op0=ALU.mult)
                                k.tt(tm[2][:, 0:tw], tm[2][:, 0:tw], tm[3][:, 0:tw], ALU.add)
                                k.ts(o_[:, 1, 0:tw], tm[2][:, 0:tw], sc, None, op0=ALU.mult)
                            else:
                                k.tt(o_[:, 1, 0:tw], tm[2][:, 0:tw], tm[3][:, 0:tw], ALU.add)
                        else:
                            k.ts(o_[:, 0, 0:tw], x1[:, 0:tw], sc, None, op0=ALU.mult)
                            k.ts(o_[:, 1, 0:tw], x2[:, 0:tw], sc, None, op0=ALU.mult, en="pool")
                        k.dma("sp", P.dr["ab_rqk"][r0:r0 + 256, t0:t0 + tw].rearrange("(c p) t -> p c t", p=128), o_[:, :, 0:tw])


def ab_gdn_conv(P, e):
    k, cfg = P.k, P.cfg
    HB, DKB = cfg.H_B, cfg.DK_B
    NCH = 3 * HB * DKB // 128
    KT = cfg.CONV_B
    PD = KT // 2
    TB = min(2048, max(L for (_, L, _) in cfg.SEGS))
    row_base = 2 * cfg.H_A * cfg.DK_A + 2 * cfg.H_A * cfg.DV_A
    qs = DKB ** -0.5
    with k.scope() as s:
        cw = s.sb([128, KT, NCH], F32)
        for j in range(KT):
            P.load_fm_vec(s, cw[:, j, :], P.dr["gdn_conv_w"][e, j], NCH)
        ub = [s.sb([128, TB + 2 * PD], F32) for _ in range(2)]
        cb = [s.sb([128, TB], F32) for _ in range(2)]
        sq = [s.sb([128, TB], BF16) for _ in range(2)]
        rr = [s.sb([128, TB], F32) for _ in range(2)]
        ob = [s.sb([128, TB], BF16) for _ in range(2)]
        pss = [s.ps([128, 512], F32) for _ in range(4)]
        items = []
        for (st, L, ci) in cfg.SEGS:
            for t0 in range(st, st + L, TB):
                tw = min(TB, st + L - t0)
                for ch in range(NCH):
                    items.append((st, L, t0, tw, ch))
        pcnt = [0]

        def load(i, it):
            st, L, t0, tw, ch = it
            u_ = ub[i % 2]
            lo = max(st, t0 - PD)
            hi = min(st + L, t0 + tw + PD)
            if lo > t0 - PD:
                k.memset(u_[:, 0:PD], 0.0, en="pool")
            if hi < t0 + tw + PD:
                k.memset(u_[:, tw + PD:tw + 2 * PD], 0.0, en="pool")
            r0 = row_base + ch * 128
            k.dma("sp", u_[:, lo - (t0 - PD):hi - (t0 - PD)], P.dr["ab_pT"][r0:r0 + 128, lo:hi])

        def body(i, it):
            st, L, t0, tw, ch = it
            u_, c_, q_, r_, o_ = ub[i % 2], cb[i % 2], sq[i % 2], rr[i % 2], ob[i % 2]
            k.ts(c_[:, 0:tw], u_[:, 0:tw], cw[:, 0, ch:ch + 1], None, op0=ALU.mult, en="pool")
            for j in range(1, KT):
                k.stt(c_[:, 0:tw], u_[:, j:j + tw], cw[:, j, ch:ch + 1], c_[:, 0:tw], ALU.mult, ALU.add)
            k.act(c_[:, 0:tw], c_[:, 0:tw], AF.Silu)
            part = ch // (HB * DKB // 128)
            if part < 2:
                k.act(q_[:, 0:tw], c_[:, 0:tw], AF.Square)
                for b0 in range(0, tw, 512):
                    bw = min(512, tw - b0)
                    ps = pss[pcnt[0] % 4]
                    pcnt[0] += 1
                    k.mm(ps[:, 0:bw], P.ones, q_[:, b0:b0 + bw])
                    k.ts(r_[:, b0:b0 + bw], ps[:, 0:bw], cfg.EPS, None, op0=ALU.add)
                k.act(r_[:, 0:tw], r_[:, 0:tw], AF.Sqrt)
                k.recip(r_[:, 0:tw], r_[:, 0:tw])
                if part == 0:
                    k.stt(o_[:, 0:tw], c_[:, 0:tw], qs, r_[:, 0:tw], ALU.mult, ALU.mult)
                else:
                    k.tt(o_[:, 0:tw], c_[:, 0:tw], r_[:, 0:tw], ALU.mult, en="pool")
            else:
                k.copy(o_[:, 0:tw], c_[:, 0:tw], en="pool")
            k.dma("sp", P.dr["ab_gT"][ch * 128:(ch + 1) * 128, t0:t0 + tw], o_[:, 0:tw])
        pipeline(items, load, body)


def ab_gates(P, e):
    k, cfg = P.k, P.cfg
    T = cfg.T
    r0 = cfg.PROJ_AB - 32
    with k.scope() as s:
        par = s.sb([16, 2], F32)
        k.dma("sp", par[:, 0:1], P.dr["gdn_dt_bias"][e].rearrange("d (h o) -> (d h) o", o=1))
        k.dma("sp", par[:, 1:2], P.dr["gdn_a_log"][e].rearrange("d (h o) -> (d h) o", o=1))
        k.act(par[:, 1:2], par[:, 1:2], AF.Exp)
        k.ts(par[:, 1:2], par[:, 1:2], -1.0, None, op0=ALU.mult)
        xa = [s.sb([16, 512], F32) for _ in range(2)]
        xb = [s.sb([16, 512], F32) for _ in range(2)]
        t1 = [s.sb([16, 512], F32) for _ in range(2)]
        t2 = [s.sb([16, 512], F32) for _ in range(2)]
        for i, t0 in enumerate(range(0, T, 512)):
            tw = min(512, T - t0)
            a_, b_, u_, v_ = xa[i % 2], xb[i % 2], t1[i % 2], t2[i % 2]
            k.dma("sp", a_[:, 0:tw], P.dr["ab_pT"][r0:r0 + 16, t0:t0 + tw])
            k.dma("sp", b_[:, 0:tw], P.dr["ab_pT"][r0 + 16:r0 + 32, t0:t0 + tw])
            k.ts(a_[:, 0:tw], a_[:, 0:tw], par[:, 0:1], None, op0=ALU.add)
            k.ts(u_[:, 0:tw], a_[:, 0:tw], -1.0, None, op0=ALU.mult)
            k.tt(u_[:, 0:tw], u_[:, 0:tw], a_[:, 0:tw], ALU.max)
            k.act(u_[:, 0:tw], u_[:, 0:tw], AF.Exp, scale=-1.0)
            k.act(u_[:, 0:tw], u_[:, 0:tw], AF.Ln, bias=1.0)
            k.ts(v_[:, 0:tw], a_[:, 0:tw], 0.0, None, op0=ALU.max)
            k.tt(v_[:, 0:tw], v_[:, 0:tw], u_[:, 0:tw], ALU.add)
            k.ts(v_[:, 0:tw], v_[:, 0:tw], par[:, 1:2], None, op0=ALU.mult)
            k.act(b_[:, 0:tw], b_[:, 0:tw], AF.Sigmoid)
            k.dma("sp", P.dr["ab_gbT"][0:16, t0:t0 + tw], v_[:, 0:tw])
            k.dma("sp", P.dr["ab_gbT"][16:32, t0:t0 + tw], b_[:, 0:tw])
    with k.scope() as s:
        ib = [s.sb([32, 512], F32) for _ in range(2)]
        ob = [s.sb([128, 32], F32) for _ in range(2)]
        pss = [s.ps([128, 32], F32) for _ in range(2)]
        m = 0
        for i, t0 in enumerate(range(0, T, 512)):
            tw = min(512, T - t0)
            i_ = ib[i % 2]
            k.dma("sp", i_[:, 0:tw], P.dr["ab_gbT"][:, t0:t0 + tw])
            for tj in range(tw // 128):
                ps, o_ = pss[m % 2], ob[m % 2]
                m += 1
                k.tr(ps[:, :], i_[:, tj * 128:(tj + 1) * 128], P.ident[0:32, 0:32])
                k.copy(o_[:, :], ps[:, :])
                k.dma("sp", P.dr["ab_gb"][t0 + tj * 128:t0 + (tj + 1) * 128, :], o_[:, :])


def ab_retention(P, e):
    k, cfg = P.k, P.cfg
    HA, DKA, DVA = cfg.H_A, cfg.DK_A, cfg.DV_A
    NQ = HA * DKA // 128
    DC = DKA // 128
    EV = HA * DVA
    with k.scope() as S:
        lg = S.sb([128, 2 * HA], F32)
        k.dma("sp", lg[:, :], P.dr["ret_decay"][e].rearrange("d h -> (d h)").partition_broadcast(128))
        k.act(lg[:, :], lg[:, :], AF.Exp, scale=math.log(2.0))
        k.ts(lg[:, :], lg[:, :], -1.0, 1.0, op0=ALU.mult, op1=ALU.add)
        k.act(lg[:, :], lg[:, :], AF.Ln)
        rel = S.sb([128, 2, CH], F32)
        k.dma("sp", rel[:, :, :], P.dr["ab_rel"].rearrange("d s t -> s d t"))
        qex = S.sb([128, 2, CH], F32)
        k.dma("sp", qex[:, :, :], P.dr["ab_qexp"].rearrange("d s t -> s d t"))
        kex = S.sb([128, 2], F32)
        k.dma("sp", kex[:, :], P.dr["ab_kexp"])
        mask = S.sb([128, 2 * HA, CH], F32)
        qdec = S.sb([128, 2 * HA, CH], F32)
        kdec = S.sb([128, 2 * HA], F32)
        cdec = S.sb([128, 2 * HA], F32)
        for d in range(2):
            for h in range(HA):
                i = d * HA + h
                k.act(mask[:, i, :], rel[:, d, :], AF.Exp, scale=lg[:, i:i + 1])
                k.act(qdec[:, i, :], qex[:, d, :], AF.Exp, scale=lg[:, i:i + 1])
                k.act(kdec[:, i:i + 1], kex[:, d:d + 1], AF.Exp, scale=lg[:, i:i + 1])
        k.act(cdec[:, :], lg[:, :], AF.Exp, scale=float(CH))
        gnw = S.sb([128, EV], F32)
        k.dma("sp", gnw[:, :], P.dr["ret_gn_w"][e].partition_broadcast(128))
        St = S.sb([128, HA * DC, DVA], F32)
        Sb = S.sb([128, HA * DC, DVA], BF16)
        qT = [S.sb([128, NQ, CH], BF16) for _ in range(2)]
        kT = [S.sb([128, NQ, CH], BF16) for _ in range(2)]
        kt = [S.sb([128, HA * DKA], BF16) for _ in range(2)]
        vt = [S.sb([128, EV], BF16) for _ in range(2)]
        sD = [S.sb([128, CH], BF16) for _ in range(2)]
        qd = [S.sb([128, DC, CH], BF16) for _ in range(2)]
        kd = [S.sb([128, DKA], BF16) for _ in range(2)]
        ot = [S.sb([128, EV], F32) for _ in range(2)]
        o1 = [S.sb([128, EV], F32) for _ in range(2)]
        sg = [S.sb([128, EV], BF16) for _ in range(2)]
        st6 = S.sb([128, HA, 6], F32)
        mv = S.sb([128, HA, 2], F32)
        ya = [S.sb([128, EV], BF16) for _ in range(2)]
        yT = [S.sb([128, 4, CH], BF16) for _ in range(2)]
        p_sc = [S.ps([128, CH], F32) for _ in range(2)]
        p_o = [S.ps([128, 512], F32) for _ in range(2)]
        p_s = [S.ps([128, 512], F32) for _ in range(2)]
        p_t = [S.ps([128, 512], BF16) for _ in range(2)]
        it = 0
        nt = 0
        for si, (st, L, ci) in enumerate(cfg.SEGS):
            NCk = L // CH
            for d in range(2):
                if si == 0:
                    k.dma("sp", St[:, :, :], P.dr["state_ret"][e, d].rearrange("h (c p) v -> p (h c) v", p=128))
                    k.copy(Sb[:, :, :], St[:, :, :], en="pool")
                else:
                    k.memset(St[:, :, :], 0.0, en="pool")
                    k.memset(Sb[:, :, :], 0.0, en="pool")
                order = range(NCk) if d == 0 else range(NCk - 1, -1, -1)
                for cn in order:
                    t0 = st + cn * CH
                    i2 = it % 2
                    it += 1
                    q_, k_, kt_, v_, o_ = qT[i2], kT[i2], kt[i2], vt[i2], ot[i2]
                    k.dma("sp", q_[:, :, :], P.dr["ab_rqk"][0:NQ * 128, t0:t0 + CH].rearrange("(c p) t -> p c t", p=128))
                    k.dma("sp", k_[:, :, :], P.dr["ab_rqk"][NQ * 128:2 * NQ * 128, t0:t0 + CH].rearrange("(c p) t -> p c t", p=128))
                    k.dma("sp", kt_[:, :], P.dr["ab_rk_tok"][t0:t0 + CH, :])
                    k.dma("sp", v_[:, :], P.dr["ab_rv_tok"][t0:t0 + CH, :])
                    if d == 1:
                        k.dma("sp", o1[i2][:, :], P.dr["ab_o1r"][t0:t0 + CH, :])
                        k.dma("sp", sg[i2][:, :], P.dr["ab_rg_tok"][t0:t0 + CH, :])
                    for h in range(HA):
                        i = d * HA + h
                        hh = it * HA + h
                        psc, po, s_, qd_, kd_ = p_sc[hh % 2], p_o[hh % 2], sD[hh % 2], qd[hh % 2], kd[hh % 2]
                        for dc in range(DC):
                            k.mm(psc[:, :], k_[:, h * DC + dc, :], q_[:, h * DC + dc, :], start=(dc == 0), stop=(dc == DC - 1))
                        k.tt(s_[:, :], psc[:, :], mask[:, i, :], ALU.mult)
                        for dc in range(DC):
                            k.tt(qd_[:, dc, :], q_[:, h * DC + dc, :], qdec[:, i, :], ALU.mult, en="pool")
                        k.mm(po[:, 0:DVA], s_[:, :], v_[:, h * DVA:(h + 1) * DVA], start=True, stop=False)
                        for dc in range(DC):
                            k.mm(po[:, 0:DVA], qd_[:, dc, :], Sb[:, h * DC + dc, :], start=False, stop=(dc == DC - 1))
                        k.copy(o_[:, h * DVA:(h + 1) * DVA], po[:, 0:DVA], en="act")
                        k.ts(kd_[:, :], kt_[:, h * DKA:(h + 1) * DKA], kdec[:, i:i + 1], None, op0=ALU.mult, en="pool")
                        for dc in range(DC):
                            pst = p_s[(hh * DC + dc) % 2]
                            k.mm(pst[:, 0:DVA], kd_[:, dc * 128:(dc + 1) * 128], v_[:, h * DVA:(h + 1) * DVA])
                            k.stt(St[:, h * DC + dc, :], St[:, h * DC + dc, :], cdec[:, i:i + 1], pst[:, 0:DVA], ALU.mult, ALU.add)
                            k.copy(Sb[:, h * DC + dc, :], St[:, h * DC + dc, :], en="act")
                    if d == 0:
                        k.dma("sp", P.dr["ab_o1r"][t0:t0 + CH, :], o_[:, :])
                    else:
                        o1_, sg_, ya_ = o1[i2], sg[i2], ya[i2]
                        k.tt(o_[:, :], o_[:, :], o1_[:, :], ALU.add, en="pool")
                        for h in range(HA):
                            k.do("dve", lambda en_, h=h: en_.bn_stats(out=st6[:, h, :].ap, in_=o_[:, h * DVA:(h + 1) * DVA].ap),
                                 reads=(o_[:, :],), writes=(st6[:, :, :],))
                            k.do("dve", lambda en_, h=h: en_.bn_aggr(out=mv[:, h, :].ap, in_=st6[:, h, :].ap),
                                 reads=(st6[:, :, :],), writes=(mv[:, :, :],))
                        k.ts(mv[:, :, 1], mv[:, :, 1], cfg.EPS, None, op0=ALU.add)
                        k.act(mv[:, :, 1], mv[:, :, 1], AF.Sqrt)
                        k.recip(mv[:, :, 1], mv[:, :, 1])
                        for h in range(HA):
                            sl = slice(h * DVA, (h + 1) * DVA)
                            k.ts(o_[:, sl], o_[:, sl], mv[:, h, 0:1], mv[:, h, 1:2], op0=ALU.subtract, op1=ALU.mult)
                        k.tt(o_[:, :], o_[:, :], gnw[:, :], ALU.mult, en="pool")
                        k.tt(ya_[:, :], o_[:, :], sg_[:, :], ALU.mult)
                        for g in range(EV // 512):
                            pt, y_ = p_t[nt % 2], yT[nt % 2]
                            nt += 1
                            for c in range(4):
                                k.tr(pt[:, c * 128:(c + 1) * 128], ya_[:, g * 512 + c * 128:g * 512 + (c + 1) * 128], P.identh)
                            k.copy(y_[:, :, :], pt[:, 0:512].rearrange("p (c t) -> p c t", t=128), en=("act" if nt % 2 else "dve"))
                            k.dma("sp", P.dr["ab_catT"][g * 512:(g + 1) * 512, t0:t0 + CH].rearrange("(c p) t -> p c t", p=128), y_[:, :, :])
                if si > 0:
                    k.dma("sp", P.dr["nsr"][si - 1, e, d].rearrange("h (c p) v -> p (h c) v", p=128), St[:, :, :])


def ab_gdn(P, e):
    k, cfg = P.k, P.cfg
    HB, DKB, DVB = cfg.H_B, cfg.DK_B, cfg.DV_B
    EV = HB * DVB
    NLEV = 7
    G = 4
    NG_ = HB // G
    GW = G * CH
    with k.scope() as S:
        U = S.sb([128, 2, CH], F32)
        k.dma("sp", U[:, :, :], P.dr["ab_U"].rearrange("d s t -> s d t"))
        negm = S.sb([128, 2, CH], F32)
        k.dma("sp", negm[:, :, :], P.dr["ab_negm"].rearrange("d s t -> s d t"))
        strict = S.sb([128, 2, CH], F32)
        k.dma("sp", strict[:, :, :], P.dr["ab_strict"].rearrange("d s t -> s d t"))
        gnw = S.sb([128, DVB], F32)
        k.dma("sp", gnw[:, :], P.dr["gdn_norm_w"][e].partition_broadcast(128))
        lm = S.sb([128, 2, 7, CH], F32)
        lmT = S.sb([128, 2, 7, CH], F32)
        for d_ in range(2):
            k.dma("sp", lm[:, d_, :, :], P.dr["ab_lm"][d_].rearrange("l t s -> t l s"))
            k.dma("sp", lmT[:, d_, :, :], P.dr["ab_lmT"][d_].rearrange("l t s -> t l s"))
        St = S.sb([128, HB, DVB], F32)
        Sb = S.sb([128, HB, DVB], BF16)
        qT = [S.sb([128, HB, CH], BF16) for _ in range(2)]
        kT = [S.sb([128, HB, CH], BF16) for _ in range(2)]
        kv = [S.sb([128, 2 * EV], BF16) for _ in range(2)]
        gb = [S.sb([128, 32], F32) for _ in range(2)]
        ot = [S.sb([128, EV], F32) for _ in range(2)]
        o1 = [S.sb([128, EV], F32) for _ in range(2)]
        sz = [S.sb([128, EV], BF16) for _ in range(2)]
        ybf = [S.sb([128, EV], F32) for _ in range(2)]
        yT = [S.sb([128, 4, CH], BF16) for _ in range(2)]
        ss = S.sb([128, HB], F32)
        sqt = S.sb([128, EV], F32)

        def grp(shape, dt):
            return [S.sb(shape, dt) for _ in range(NG_)]
        W3 = [128, G, CH]
        gbc, bbc, gsm = grp(W3, F32), grp(W3, F32), grp(W3, F32)
        sc = grp([128, 6, G], F32)
        dec, eGr, t1, Qf, uf = grp(W3, F32), grp(W3, F32), grp(W3, F32), grp(W3, F32), grp(W3, F32)
        Wb, Zb, Lb, LTb, M1, M2 = (grp(W3, BF16) for _ in range(6))
        rv, rk, wT, ub, qk, qd, kd = (grp(W3, BF16) for _ in range(7))
        tS = grp(W3, F32)
        bank = [[S.ps([128, 512], F32) for _ in range(4)] for _ in range(NG_)]
        identf = P.ident

        def bc_h(v):
            return v.unsqueeze(1).to_broadcast([128, G, CH])

        def bc_t(v):
            return v.unsqueeze(2).to_broadcast([128, G, CH])

        def v3(b):
            return b[:, 0:GW].rearrange("p (g t) -> p g t", t=CH)
        it = 0
        nt = 0
        for si, (st, L, ci) in enumerate(cfg.SEGS):
            NCk = L // CH
            for d in range(2):
                if si == 0:
                    k.dma("sp", St[:, :, :], P.dr["state_gdn"][e, d].rearrange("h p v -> p h v"))
                    k.copy(Sb[:, :, :], St[:, :, :], en="pool")
                else:
                    k.memset(St[:, :, :], 0.0, en="pool")
                    k.memset(Sb[:, :, :], 0.0, en="pool")
                last = CH - 1 if d == 0 else 0
                order = list(range(NCk)) if d == 0 else list(range(NCk - 1, -1, -1))

                def load(i, cn, d=d, st=st):
                    t0 = st + cn * CH
                    i2 = i % 2
                    k.dma("sp", qT[i2][:, :, :], P.dr["ab_gT"][0:EV, t0:t0 + CH].rearrange("(c p) t -> p c t", p=128))
                    k.dma("sp", kT[i2][:, :, :], P.dr["ab_gT"][EV:2 * EV, t0:t0 + CH].rearrange("(c p) t -> p c t", p=128))
                    k.dma("sp", kv[i2][:, :], P.dr["ab_gtok"][t0:t0 + CH, :])
                    k.dma("sp", gb[i2][:, :], P.dr["ab_gb"][t0:t0 + CH, :])
                    if d == 1:
                        k.dma("sp", o1[i2][:, :], P.dr["ab_o1g"][t0:t0 + CH, :])
                        k.dma("sp", sz[i2][:, :], P.dr["ab_gz_tok"][t0:t0 + CH, :])

                def body(i, cn, d=d, st=st, last=last):
                    nonlocal nt
                    t0 = st + cn * CH
                    i2 = i % 2
                    q_, k_, kv_, gb_, o_ = qT[i2], kT[i2], kv[i2], gb[i2], ot[i2]
                    stages = []
                    for g in range(NG_):
                        h0 = g * G
                        B0, B1, B2, B3 = bank[g]
                        gcols = gb_[:, d * HB + h0:d * HB + h0 + G]
                        bcols = gb_[:, 16 + d * HB + h0:16 + d * HB + h0 + G]
                        ktok3 = kv_[:, h0 * DKB:(h0 + G) * DKB].rearrange("p (g t) -> p g t", t=DKB)
                        vtok3 = kv_[:, EV + h0 * DVB:EV + (h0 + G) * DVB].rearrange("p (g t) -> p g t", t=DVB)
                        sc_ = sc[g]
                        sg = []

                        def s1(g=g, h0=h0, B0=B0, B1=B1, B3=B3, gcols=gcols, bcols=bcols, sc_=sc_):
                            k.copy(gbc[g][:, :, :], bc_t(gcols), en="pool")
                            k.tt(gsm[g][:, :, :], gbc[g][:, :, :], bc_h(strict[:, 1 - d, :]), ALU.mult, en="pool")
                            k.copy(bbc[g][:, :, :], bc_t(bcols), en="pool")
                            for h in range(G):
                                k.mm(B0[:, h * CH:(h + 1) * CH], gbc[g][:, h, :], U[:, d, :])
                            for h in range(G):
                                k.mm(B1[:, h * CH:(h + 1) * CH], gsm[g][:, h, :], U[:, d, :])
                            for h in range(G):
                                k.mm(B3[:, h:h + 1], U[:, d, :], gcols[:, h:h + 1])
                        sg.append(s1)

                        def s2(g=g, B0=B0, B1=B1, B3=B3, sc_=sc_, bcols=bcols):
                            k.copy(sc_[:, 0, :], B3[:, 0:G])
                            k.act(sc_[:, 1, :], sc_[:, 0, :], AF.Exp)
                            k.act(sc_[:, 2, :], v3(B0)[:, :, last], AF.Exp)
                            k.act(sc_[:, 3, :], v3(B1)[:, :, last], AF.Exp)
                            k.tt(sc_[:, 4, :], sc_[:, 1, :], bcols, ALU.mult)
                            k.tt(dec[g][:, :, :], v3(B1), bc_h(negm[:, d, :]), ALU.add)
                            k.act(dec[g][:, :, :], dec[g][:, :, :], AF.Exp)
                            k.act(eGr[g][:, :, :], v3(B0), AF.Exp)
                        sg.append(s2)

                        def s3(g=g, h0=h0, B0=B0, B2=B2, B3=B3):
                            for h in range(G):
                                k.mm(B2[:, h * CH:(h + 1) * CH], bbc[g][:, h, :], identf)
                            for h in range(G):
                                k.mm(B3[:, h * CH:(h + 1) * CH], k_[:, h0 + h, :], k_[:, h0 + h, :])
                            for h in range(G):
                                k.mm(B0[:, h * CH:(h + 1) * CH], k_[:, h0 + h, :], q_[:, h0 + h, :])
                            k.tt(t1[g][:, :, :], dec[g][:, :, :], bc_h(strict[:, d, :]), ALU.mult, en="pool")
                        sg.append(s3)

                        def s4(g=g, B0=B0, B1=B1, B2=B2, B3=B3):
                            k.tt(t1[g][:, :, :], v3(B2), t1[g][:, :, :], ALU.mult)
                            k.stt(Qf[g][:, :, :], v3(B3), -1.0, t1[g][:, :, :], ALU.mult, ALU.mult)
                            k.tt(qk[g][:, :, :], v3(B0), dec[g][:, :, :], ALU.mult)
                            for h in range(G):
                                k.tr(B1[:, h * CH:(h + 1) * CH], Qf[g][:, h, :], identf)
                            k.copy(uf[g][:, :, :], v3(B1), en="act")
                            k.copy(Wb[g][:, :, :], bc_h(identf), en="pool")
                            k.copy(Zb[g][:, :, :], bc_h(identf), en="pool")
                        sg.append(s4)
                        for lev in range(NLEV):
                            lastlev = lev == NLEV - 1

                            def sl(g=g, lev=lev, lastlev=lastlev, B0=B0, B1=B1, B2=B2, B3=B3):
                                k.tt(Lb[g][:, :, :], uf[g][:, :, :], bc_h(lm[:, d, lev, :]), ALU.mult, en="pool")
                                for h in range(G):
                                    k.mm(B2[:, h * CH:(h + 1) * CH], Lb[g][:, h, :], Wb[g][:, h, :])
                                k.act(M1[g][:, :, :], v3(B2), AF.Identity, scale=-1.0)
                                for h in range(G):
                                    k.mm(B0[:, h * CH:(h + 1) * CH], Zb[g][:, h, :], M1[g][:, h, :])
                                if not lastlev:
                                    k.tt(LTb[g][:, :, :], Qf[g][:, :, :], bc_h(lmT[:, d, lev, :]), ALU.mult, en="pool")
                                    for h in range(G):
                                        k.mm(B3[:, h * CH:(h + 1) * CH], LTb[g][:, h, :], Zb[g][:, h, :])
                                    k.ts(M2[g][:, :, :], v3(B3), -1.0, None, op0=ALU.mult)
                                    for h in range(G):
                                        k.mm(B1[:, h * CH:(h + 1) * CH], Wb[g][:, h, :], M2[g][:, h, :])
                                k.tt(Wb[g][:, :, :], v3(B0), Wb[g][:, :, :], ALU.add)
                                if not lastlev:
                                    k.tt(Zb[g][:, :, :], v3(B1), Zb[g][:, :, :], ALU.add)
                            sg.append(sl)

                        def s5(g=g, h0=h0, B0=B0, B1=B1, B2=B2, B3=B3, sc_=sc_, bcols=bcols, ktok3=ktok3, vtok3=vtok3):
                            k.tt(rv[g][:, :, :], vtok3, bc_t(bcols), ALU.mult, en="pool")
                            k.tt(rk[g][:, :, :], ktok3, bc_t(sc_[:, 4, :]), ALU.mult, en="pool")
                            k.tt(kd[g][:, :, :], ktok3, bc_t(sc_[:, 3, :]), ALU.mult, en="pool")
                            k.tt(qd[g][:, :, :], q_[:, h0:h0 + G, :], eGr[g][:, :, :], ALU.mult, en="pool")
                            for h in range(G):
                                k.mm(B2[:, h * CH:(h + 1) * CH], Wb[g][:, h, :], rv[g][:, h, :])
                            for h in range(G):
                                k.mm(B3[:, h * CH:(h + 1) * CH], rk[g][:, h, :], Wb[g][:, h, :])
                            k.copy(wT[g][:, :, :], v3(B3), en="act")
                            k.copy(tS[g][:, :, :], v3(B2))
                            for h in range(G):
                                k.mm(B0[:, h * CH:(h + 1) * CH], wT[g][:, h, :], Sb[:, h0 + h, :])
                            k.tt(ub[g][:, :, :], tS[g][:, :, :], v3(B0), ALU.subtract)
                        sg.append(s5)

                        def s6(g=g, h0=h0, B1=B1, B2=B2, sc_=sc_):
                            for h in range(G):
                                k.mm(B1[:, h * CH:(h + 1) * CH], qd[g][:, h, :], Sb[:, h0 + h, :], start=True, stop=False)
                                k.mm(B1[:, h * CH:(h + 1) * CH], qk[g][:, h, :], ub[g][:, h, :], start=False, stop=True)
                            k.copy(o_[:, h0 * DVB:(h0 + G) * DVB], B1[:, 0:GW], en="act")
                            for h in range(G):
                                k.mm(B2[:, h * CH:(h + 1) * CH], kd[g][:, h, :], ub[g][:, h, :])
                            k.tt(tS[g][:, :, :], St[:, h0:h0 + G, :], bc_t(sc_[:, 2, :]), ALU.mult, en="pool")
                            k.tt(St[:, h0:h0 + G, :], v3(B2), tS[g][:, :, :], ALU.add)
                            k.copy(Sb[:, h0:h0 + G, :], St[:, h0:h0 + G, :], en="act")
                        sg.append(s6)
                        stages.append(sg)
                    for sidx in range(len(stages[0])):
                        for g in range(NG_):
                            stages[g][sidx]()
                    if d == 0:
                        k.dma("sp", P.dr["ab_o1g"][t0:t0 + CH, :], o_[:, :])
                    else:
                        o1_, sz_, yb_ = o1[i2], sz[i2], ybf[i2]
                        k.tt(o_[:, :], o_[:, :], o1_[:, :], ALU.add, en="pool")
                        k.act(sqt[:, :], o_[:, :], AF.Square)
                        k.do("dve", lambda en_: en_.tensor_reduce(out=ss[:, :].ap, in_=sqt[:, :].rearrange("p (h v) -> p h v", v=DVB).ap,
                                                                    axis=mybir.AxisListType.X, op=ALU.add),
                             reads=(sqt[:, :],), writes=(ss[:, :],))
                        k.ts(ss[:, :], ss[:, :], 1.0 / DVB, cfg.EPS, op0=ALU.mult, op1=ALU.add)
                        k.act(ss[:, :], ss[:, :], AF.Sqrt)
                        k.recip(ss[:, :], ss[:, :])
                        o3 = o_[:, :].rearrange("p (h v) -> p h v", v=DVB)
                        k.tt(o3, o3, ss[:, :].unsqueeze(2).to_broadcast([128, HB, DVB]), ALU.mult)
                        k.tt(o3, o3, gnw[:, :].unsqueeze(1).to_broadcast([128, HB, DVB]), ALU.mult, en="pool")
                        k.tt(yb_[:, :], o_[:, :], sz_[:, :], ALU.mult)
                        for gg in range(EV // 512):
                            pt, y_ = bank[gg % NG_][0], yT[nt % 2]
                            nt += 1
                            for c in range(4):
                                k.tr(pt[:, c * 128:(c + 1) * 128], yb_[:, gg * 512 + c * 128:gg * 512 + (c + 1) * 128], identf)
                            k.copy(y_[:, :, :], pt[:, 0:512].rearrange("p (c t) -> p c t", t=128), en=("act" if nt % 2 else "dve"))
                            r0 = cfg.H_A * cfg.DV_A + gg * 512
                            k.dma("sp", P.dr["ab_catT"][r0:r0 + 512, t0:t0 + CH].rearrange("(c p) t -> p c t", p=128), y_[:, :, :])
                pipeline(order, load, body)
                if si > 0:
                    k.dma("sp", P.dr["nsg"][si - 1, e, d].rearrange("h p v -> p h v"), St[:, :, :])


def mixer_ab_impl(P, e):
    cfg = P.cfg
    D = cfg.D_MODEL
    groups = token_groups(cfg)
    HA, DKA, DVA, HB, DKB, DVB = cfg.H_A, cfg.DK_A, cfg.DV_A, cfg.H_B, cfg.DK_B, cfg.DV_B
    import os
    stop = int(os.environ.get("AB_STOP", "99"))
    steps = [
        lambda: proj_fm(P, P.dr["w_in_ab"][e], D, cfg.PROJ_AB, P.dr["hT"], P.dr["ab_pT"], F32, groups),
        lambda: ab_rope(P),
        lambda: fm_to_tm(P, P.dr["ab_rqk"], HA * DKA, HA * DKA, P.dr["ab_rk_tok"], 0, BF16, BF16),
        lambda: fm_to_tm(P, P.dr["ab_pT"], 2 * HA * DKA, HA * DVA, P.dr["ab_rv_tok"], 0, F32, BF16),
        lambda: fm_to_tm(P, P.dr["ab_pT"], 2 * HA * DKA + HA * DVA, HA * DVA, P.dr["ab_rg_tok"], 0, F32, BF16, func=AF.Silu),
        lambda: ab_gdn_conv(P, e),
        lambda: fm_to_tm(P, P.dr["ab_gT"], HB * DKB, 2 * HB * DKB, P.dr["ab_gtok"], 0, BF16, BF16),
        lambda: fm_to_tm(P, P.dr["ab_pT"], 2 * HA * DKA + 2 * HA * DVA + 3 * HB * DKB, HB * DVB, P.dr["ab_gz_tok"], 0, F32, BF16, func=AF.Silu),
        lambda: ab_gates(P, e),
        lambda: ab_retention(P, e),
        lambda: ab_gdn(P, e),
        lambda: proj_fm(P, P.dr["w_out_ab"][e], cfg.MIX_AB_OUT, D, P.dr["ab_catT"], P.dr["yT"], F32, groups),
    ]
    names = ["inproj", "rope", "rk_tok", "rv_tok", "rg_tok", "gconv", "g_tok", "gz_tok", "gates", "retention", "gdn", "outproj"]
    for i, f in enumerate(steps):
        if i >= stop:
            break
        f()
        P.mark(f"ab{e}.{names[i]}")
```

```python
import math
from contextlib import ExitStack

import ml_dtypes
import numpy as np

import concourse.bass as bass
import concourse.mybir as mybir
from concourse.bass_utils import run_bass_kernel_spmd

F32 = mybir.dt.float32
BF16 = mybir.dt.bfloat16
AF = mybir.ActivationFunctionType
ALU = mybir.AluOpType
NPBF = ml_dtypes.bfloat16


class Cfg:
    D_MODEL = 2048
    BATCH = 16
    SEQ = 256
    DEPTH = 4
    DEC_BATCH = 8
    DEC_SEQ = 4096
    GRID_W = 64
    H_A = 4
    DK_A = 256
    DV_A = 512
    ROPE_BASE = 10000.0
    H_B = 8
    DK_B = 128
    DV_B = 128
    CONV_B = 5
    HY_CONV = 3
    HY_BANDS = 16
    HY_FFN = 64
    HY_FAST_DECAY = 0.3
    HY_SLOW_DECAY = 1.5
    HY_TARGET = 1e-2
    EPS = 1e-6
    N_CORES = 8

    def __init__(self, **kw):
        for k_, v in kw.items():
            setattr(self, k_, v)
        self.D_FF = ((8 * self.D_MODEL // 3 + 255) // 256) * 256
        self.HY_EMB = 1 + 2 * self.HY_BANDS
        self.N_EVEN = (self.DEPTH + 1) // 2
        self.N_ODD = self.DEPTH // 2
        self.SIZES_AB = (self.H_A * self.DK_A, self.H_A * self.DK_A, self.H_A * self.DV_A, self.H_A * self.DV_A,
                         2 * self.H_B * self.DK_B + self.H_B * self.DV_B, self.H_B * self.DV_B,
                         2 * self.H_B, 2 * self.H_B)
        self.PROJ_AB = sum(self.SIZES_AB)
        self.MIX_AB_OUT = self.H_A * self.DV_A + self.H_B * self.DV_B
        self.KC = self.D_MODEL // 128
        self.NPROMPT = self.BATCH // self.N_CORES
        self.T = self.DEC_SEQ + self.NPROMPT * self.SEQ
        self.SEGS = [(0, self.DEC_SEQ, 0)] + [(self.DEC_SEQ + i * self.SEQ, self.SEQ, 1) for i in range(self.NPROMPT)]


class Buf:
    __slots__ = ("t", "w", "r", "x")

    def __init__(self, t, excl=False):
        self.t = t
        self.x = excl
        self.w = []
        self.r = []

    def __getitem__(self, idx):
        return V(self, self.t[idx])


class V:
    __slots__ = ("buf", "ap")

    def __init__(self, buf, ap):
        self.buf = buf
        self.ap = ap

    def __getitem__(self, idx):
        return V(self.buf, self.ap[idx])

    def rearrange(self, *a, **kw):
        return V(self.buf, self.ap.rearrange(*a, **kw))

    def unsqueeze(self, *a):
        return V(self.buf, self.ap.unsqueeze(*a))

    def to_broadcast(self, *a):
        return V(self.buf, self.ap.to_broadcast(*a))

    def bitcast(self, *a):
        return V(self.buf, self.ap.bitcast(*a))


def _ap(x):
    return x.ap if isinstance(x, V) else x


def _bufs(xs):
    return [x.buf for x in xs if isinstance(x, V)]


class K:
    ENG = ("pe", "act", "dve", "pool", "sp")

    def __init__(self, nc):
        self.nc = nc
        self.es = ExitStack()
        self.E = {"pe": nc.tensor, "act": nc.scalar, "dve": nc.vector, "pool": nc.gpsimd, "sp": nc.sync}
        self.sem = {}
        self.cnt = {}
        self.semid = {}
        self.allsems = []
        for e in self.ENG:
            s = self.es.enter_context(nc.semaphore("s_" + e))
            self.sem[e] = s
            self.cnt[e] = 0
            self.allsems.append(s)
        self.dsem = {}
        self.dval = {}
        self.dnext = {}
        for q, n in (("sp", 16), ("pool", 16), ("act", 8)):
            self.dsem[q] = [self.es.enter_context(nc.semaphore(f"d_{q}{i}")) for i in range(n)]
            self.dval[q] = [0] * n
            self.dnext[q] = 0
            self.allsems += self.dsem[q]
        self.val = {id(s): 0 for s in self.allsems}
        self.seen = {e: {} for e in self.ENG}
        self.nuniq = 0
        self.ninst = 0

    def _wait(self, en, ev):
        sem, val = ev
        sid = id(sem)
        sd = self.seen[en]
        if sd.get(sid, 0) >= val:
            return
        sd[sid] = val
        self.E[en].wait_ge(sem, val)

    def _deps(self, en, reads, writes):
        own = id(self.sem[en])
        for b in reads:
            for ev in b.w:
                if en == "pe" and id(ev[0]) == own:
                    continue
                self._wait(en, ev)
            if b.x:
                for ev in b.r:
                    if id(ev[0]) != own:
                        self._wait(en, ev)
        for b in writes:
            for ev in b.w:
                if id(ev[0]) == own:
                    continue
                self._wait(en, ev)
            for ev in b.r:
                if id(ev[0]) == own:
                    continue
                self._wait(en, ev)

    def _mark(self, ev, reads, writes):
        for b in reads:
            sid = id(ev[0])
            b.r = [e for e in b.r if id(e[0]) != sid]
            b.r.append(ev)
        for b in writes:
            b.w = [ev]
            b.r = []

    def do(self, en, fn, reads=(), writes=()):
        rb, wb = _bufs(reads), _bufs(writes)
        self._deps(en, rb, wb)
        ins = fn(self.E[en])
        self.cnt[en] += 1
        ins.then_inc(self.sem[en], 1)
        ev = (self.sem[en], self.cnt[en])
        self.val[id(self.sem[en])] = self.cnt[en]
        self._mark(ev, rb, wb)
        self.ninst += 1
        return ev

    def dma(self, q, out, in_):
        rb, wb = _bufs([in_]), _bufs([out])
        j = self.dnext[q]
        self.dnext[q] = (j + 1) % len(self.dsem[q])
        sem = self.dsem[q][j]
        self._wait(q, (sem, self.dval[q][j]))
        self._deps(q, rb, wb)
        self.E[q].dma_start(out=_ap(out), in_=_ap(in_)).then_inc(sem, 16)
        self.dval[q][j] += 16
        ev = (sem, self.dval[q][j])
        self.val[id(sem)] = self.dval[q][j]
        self._mark(ev, rb, wb)
        self.ninst += 1
        return ev

    def barrier(self, engines=None):
        for en in (engines or self.ENG):
            for s in self.allsems:
                if s is self.sem[en]:
                    continue
                v = self.val[id(s)]
                if v:
                    self._wait(en, (s, v))

    class Scope:
        def __init__(self, k):
            self.k = k
            self.es = ExitStack()

        def sb(self, shape, dt, name=None):
            self.k.nuniq += 1
            t = self.es.enter_context(self.k.nc.sbuf_tensor(name or f"t{self.k.nuniq}", list(shape), dt))
            return Buf(t)

        def ps(self, shape, dt=F32, name=None):
            self.k.nuniq += 1
            t = self.es.enter_context(self.k.nc.psum_tensor(name or f"p{self.k.nuniq}", list(shape), dt))
            return Buf(t, excl=True)

        def __enter__(self):
            return self

        def __exit__(self, *a):
            if a[0] is None:
                self.k.barrier()
            self.es.close()
            return False

    def scope(self):
        return K.Scope(self)

    def mm(self, out, lhsT, rhs, start=True, stop=True):
        return self.do("pe", lambda e: e.matmul(_ap(out), lhsT=_ap(lhsT), rhs=_ap(rhs), start=start, stop=stop),
                       reads=(lhsT, rhs), writes=(out,))

    def tr(self, out, in_, ident):
        return self.do("pe", lambda e: e.transpose(_ap(out), _ap(in_), _ap(ident)), reads=(in_, ident), writes=(out,))

    def act(self, out, in_, func, bias=None, scale=None, en="act"):
        kw = {}
        rd = [in_]
        if bias is not None:
            kw["bias"] = _ap(bias)
            rd.append(bias)
        if scale is not None:
            kw["scale"] = _ap(scale)
            rd.append(scale)
        return self.do("act", lambda e: e.activation(out=_ap(out), in_=_ap(in_), func=func, **kw),
                       reads=rd, writes=(out,))

    def tt(self, out, in0, in1, op, en="dve"):
        return self.do(en, lambda e: e.tensor_tensor(out=_ap(out), in0=_ap(in0), in1=_ap(in1), op=op),
                       reads=(in0, in1), writes=(out,))

    def ts(self, out, in0, s1, s2=None, op0=ALU.mult, op1=None, en="dve"):
        kw = {}
        if op1 is not None:
            kw["op1"] = op1
        return self.do(en, lambda e: e.tensor_scalar(out=_ap(out), in0=_ap(in0), scalar1=_ap(s1), scalar2=_ap(s2),
                                                     op0=op0, **kw),
                       reads=(in0, s1, s2), writes=(out,))

    def stt(self, out, in0, scalar, in1, op0, op1):
        return self.do("dve", lambda e: e.scalar_tensor_tensor(out=_ap(out), in0=_ap(in0), scalar=_ap(scalar),
                                                               in1=_ap(in1), op0=op0, op1=op1),
                       reads=(in0, scalar, in1), writes=(out,))

    def copy(self, out, in_, en="dve"):
        if en == "act":
            return self.do("act", lambda e: e.copy(out=_ap(out), in_=_ap(in_)), reads=(in_,), writes=(out,))
        return self.do(en, lambda e: e.tensor_copy(out=_ap(out), in_=_ap(in_)), reads=(in_,), writes=(out,))

    def recip(self, out, in_):
        return self.do("dve", lambda e: e.reciprocal(out=_ap(out), in_=_ap(in_)), reads=(in_,), writes=(out,))

    def memset(self, out, val, en="dve"):
        return self.do(en, lambda e: e.memset(_ap(out), val), writes=(out,))


class Prog:
    def __init__(self, cfg):
        self.cfg = cfg
        self.nc = bass.Bass("TRN2", target_bir_lowering=False)
        self.k = K(self.nc)
        self.dr = {}

    def mark(self, label):
        if not hasattr(self, "marks"):
            self.marks = []
        self.marks.append((label, dict(self.k.cnt)))

    def din(self, name, shape, dt=F32):
        self.dr[name] = self.nc.dram_tensor(name, list(shape), dt, kind="ExternalInput").ap()
        return self.dr[name]

    def dout(self, name, shape, dt=F32):
        self.dr[name] = self.nc.dram_tensor(name, list(shape), dt, kind="ExternalOutput").ap()
        return self.dr[name]

    def dtmp(self, name, shape, dt=F32):
        kind = "ExternalOutput" if name in getattr(self.cfg, "DEBUG_OUT", ()) else "Internal"
        self.dr[name] = self.nc.dram_tensor(name, list(shape), dt, kind=kind).ap()
        return self.dr[name]

    def load_fm_vec(self, s, dst, src, n):
        k = self.k
        if not hasattr(s, "_fm_tmp"):
            s._fm_tmp = s.sb([128, 128], F32)
            s._fm_ps = s.ps([128, 128], F32)
        tmp, ps = s._fm_tmp, s._fm_ps
        k.dma("sp", tmp[0:n, :], src.rearrange("(c p) -> c p", p=128))
        k.tr(ps[:, 0:n], tmp[0:n, :], self.ident[0:n, 0:n])
        k.copy(dst, ps[:, 0:n])

    def rstd_from_ss(self, s, r, ps, n_feat, width):
        k = self.k
        k.ts(r, ps, 1.0 / n_feat, self.cfg.EPS, op0=ALU.mult, op1=ALU.add)
        k.act(r, r, AF.Sqrt)
        k.recip(r, r)

    def phase_consts(self, S):
        k, cfg = self.k, self.cfg
        self.ident_b = S.sb([128, 128], F32)
        k.dma("sp", self.ident_b[:, :], self.dr["c_ident"])
        self.ident = self.ident_b[:, :]
        self.identh_b = S.sb([128, 128], BF16)
        k.dma("pool", self.identh_b[:, :], self.dr["c_ident"])
        self.identh = self.identh_b[:, :]
        self.ones_b = S.sb([128, 128], BF16)
        k.memset(self.ones_b[:, :], 1.0)
        self.ones = self.ones_b[:, :]

    def phase_mod(self, S):
        k, cfg = self.k, self.cfg
        KC, D = cfg.KC, cfg.D_MODEL
        NL = cfg.DEPTH
        self.modv = S.sb([128, NL * 6 * KC * 2], F32)
        mv = self.modv

        def mvs(l, j, kc, c0=0, c1=2):
            base = ((l * 6 + j) * KC + kc) * 2
            return mv[:, base + c0:base + c1]
        self.mvs = mvs
        with k.scope() as s:
            cond = s.sb([2, D], F32)
            k.dma("sp", cond[:, :], self.dr["cond"])
            k.act(cond[:, :], cond[:, :], AF.Silu)
            scT = s.sb([128, KC, 2], BF16)
            pst = s.ps([128, 512], F32)
            for kc in range(KC):
                k.tr(pst[:, 2 * kc:2 * kc + 2], cond[:, kc * 128:(kc + 1) * 128], self.ident[0:2, 0:2])
            k.copy(scT[:, :, :], pst[:, 0:2 * KC].rearrange("p (c t) -> p c t", t=2))
            nw = s.sb([128, 4, KC], F32)
            raw = s.sb([128, 6 * KC, 2], F32)
            bm = s.sb([128, 6 * KC], F32)
            CB = 512 if 6 * D >= 512 else 6 * D
            wbufs = [s.sb([128, KC, CB], BF16) for _ in range(2)]
            pss = [s.ps([128, 512], F32) for _ in range(2)]
            for l in range(NL):
                for i, nm in enumerate(("norm_mix_pre", "norm_mix_post", "norm_ffn_pre", "norm_ffn_post")):
                    self.load_fm_vec(s, nw[:, i, :], self.dr[nm][l], KC)
                self.load_fm_vec(s, bm[:, :], self.dr["b_mod"][l], 6 * KC)
                wsrc = self.dr["w_mod"][l].rearrange("(c p) n -> p c n", p=128)
                for cb in range(6 * D // CB):
                    wb = wbufs[cb % 2]
                    k.dma("pool", wb[:, :, :], wsrc[:, :, cb * CB:(cb + 1) * CB])
                    ps = pss[cb % 2]
                    nch = CB // 128
                    for m in range(nch):
                        for kc in range(KC):
                            k.mm(ps[:, 2 * m:2 * m + 2], wb[:, kc, m * 128:(m + 1) * 128], scT[:, kc, :],
                                 start=(kc == 0), stop=(kc == KC - 1))
                    ch0 = cb * nch
                    k.tt(raw[:, ch0:ch0 + nch, :], ps[:, 0:2 * nch].rearrange("p (c t) -> p c t", t=2),
                         bm[:, ch0:ch0 + nch].unsqueeze(2).to_broadcast([128, nch, 2]), ALU.add)
                for kc in range(KC):
                    for half, (npre, npost) in enumerate(((0, 1), (2, 3))):
                        sh = raw[:, (3 * half + 0) * KC + kc, :]
                        sc = raw[:, (3 * half + 1) * KC + kc, :]
                        g = raw[:, (3 * half + 2) * KC + kc, :]
                        k.ts(mvs(l, 3 * half + 0, kc), sc, 1.0, nw[:, npre, kc:kc + 1], op0=ALU.add, op1=ALU.mult)
                        k.copy(mvs(l, 3 * half + 1, kc), sh)
                        k.ts(mvs(l, 3 * half + 2, kc), g, nw[:, npost, kc:kc + 1], None, op0=ALU.mult)

    def phase_in(self):
        k, cfg = self.k, self.cfg
        KC, D, T = cfg.KC, cfg.D_MODEL, cfg.T
        xT = self.dr["xT"].rearrange("(c p) t -> p c t", p=128)
        with k.scope() as s:
            xin = [s.sb([128, D], F32) for _ in range(2)]
            xo = [s.sb([128, KC, 512], F32) for _ in range(2)]
            pss = [s.ps([128, 512], F32) for _ in range(4)]
            n = 0
            for t0 in range(0, T, 512):
                ob = xo[(t0 // 512) % 2]
                for tt in range(4):
                    ib = xin[n % 2]
                    k.dma("sp", ib[:, :], self.dr["x_in"][t0 + tt * 128:t0 + (tt + 1) * 128, :])
                    for g in range(KC // 4 if KC >= 4 else 1):
                        ps = pss[n % 4]
                        nn = min(4, KC)
                        for i in range(nn):
                            kc = g * 4 + i
                            k.tr(ps[:, i * 128:(i + 1) * 128], ib[:, kc * 128:(kc + 1) * 128], self.ident)
                        k.copy(ob[:, g * 4:g * 4 + nn, tt * 128:(tt + 1) * 128],
                               ps[:, 0:nn * 128].rearrange("p (c t) -> p c t", t=128),
                               en=("act" if n % 2 else "dve"))
                        n += 1
                k.dma("sp", xT[:, :, t0:t0 + 512], ob[:, :, :])

    def phase_out(self):
        k, cfg = self.k, self.cfg
        KC, D, T = cfg.KC, cfg.D_MODEL, cfg.T
        xT = self.dr["xT"].rearrange("(c p) t -> p c t", p=128)
        with k.scope() as s:
            xi = [s.sb([128, KC, 512], F32) for _ in range(2)]
            yo = [s.sb([128, D], F32) for _ in range(2)]
            pss = [s.ps([128, 512], F32) for _ in range(4)]
            n = 0
            m = 0
            for t0 in range(0, T, 512):
                ib = xi[(t0 // 512) % 2]
                k.dma("sp", ib[:, :, :], xT[:, :, t0:t0 + 512])
                for tt in range(4):
                    ob = yo[m % 2]
                    m += 1
                    for g in range(KC // 4 if KC >= 4 else 1):
                        ps = pss[n % 4]
                        nn = min(4, KC)
                        for i in range(nn):
                            kc = g * 4 + i
                            k.tr(ps[:, i * 128:(i + 1) * 128], ib[:, kc, tt * 128:(tt + 1) * 128], self.ident)
                        k.copy(ob[:, g * 512:g * 512 + nn * 128], ps[:, 0:nn * 128], en=("act" if n % 2 else "dve"))
                        n += 1
                    k.dma("sp", self.dr["y"][t0 + tt * 128:t0 + (tt + 1) * 128, :], ob[:, :])

    def phase_norm(self, l, half):
        k, cfg = self.k, self.cfg
        KC, D, T = cfg.KC, cfg.D_MODEL, cfg.T
        xT = self.dr["xT"].rearrange("(c p) t -> p c t", p=128)
        hT = self.dr["hT"].rearrange("(c p) t -> p c t", p=128)
        with k.scope() as s:
            xb = [s.sb([128, KC, 512], F32) for _ in range(2)]
            sq = [s.sb([128, KC, 512], BF16) for _ in range(2)]
            hb = [s.sb([128, KC, 512], BF16) for _ in range(2)]
            rr = [s.sb([128, 512], F32) for _ in range(2)]
            tm = [s.sb([128, 512], F32) for _ in range(3)]
            pss = [s.ps([128, 512], F32) for _ in range(2)]
            items = [(t0, min(512, st + L - t0), ci) for (st, L, ci) in cfg.SEGS for t0 in range(st, st + L, 512)]

            def load(i, it):
                t0, w, ci = it
                k.dma("sp", xb[i % 2][:, :, 0:w], xT[:, :, t0:t0 + w])

            def body(i, it):
                t0, w, ci = it
                i2 = i % 2
                x_, q_, h_, r_, ps = xb[i2], sq[i2], hb[i2], rr[i2], pss[i2]
                k.act(q_[:, :, 0:w], x_[:, :, 0:w], AF.Square)
                for kc in range(KC):
                    k.mm(ps[:, 0:w], self.ones, q_[:, kc, 0:w], start=(kc == 0), stop=(kc == KC - 1))
                self.rstd_from_ss(s, r_[:, 0:w], ps[:, 0:w], D, w)
                for kc in range(KC):
                    t_ = tm[kc % 3]
                    k.stt(t_[:, 0:w], x_[:, kc, 0:w], self.mvs(l, 3 * half + 0, kc, ci, ci + 1), r_[:, 0:w],
                          ALU.mult, ALU.mult)
                    k.act(h_[:, kc, 0:w], t_[:, 0:w], AF.Identity, bias=self.mvs(l, 3 * half + 1, kc, ci, ci + 1))
                k.dma("sp", hT[:, :, t0:t0 + w], h_[:, :, 0:w])
            pipeline(items, load, body)

    def phase_post(self, l, half):
        k, cfg = self.k, self.cfg
        KC, D, T = cfg.KC, cfg.D_MODEL, cfg.T
        xT = self.dr["xT"].rearrange("(c p) t -> p c t", p=128)
        yT = self.dr["yT"].rearrange("(c p) t -> p c t", p=128)
        with k.scope() as s:
            xb = [s.sb([128, KC, 512], F32) for _ in range(2)]
            yb = [s.sb([128, KC, 512], F32) for _ in range(2)]
            sq = [s.sb([128, KC, 512], BF16) for _ in range(2)]
            rr = [s.sb([128, 512], F32) for _ in range(2)]
            tm = [s.sb([128, 512], F32) for _ in range(3)]
            pss = [s.ps([128, 512], F32) for _ in range(2)]
            items = [(t0, min(512, st + L - t0), ci) for (st, L, ci) in cfg.SEGS for t0 in range(st, st + L, 512)]

            def load(i, it):
                t0, w, ci = it
                k.dma("sp", yb[i % 2][:, :, 0:w], yT[:, :, t0:t0 + w])
                k.dma("sp", xb[i % 2][:, :, 0:w], xT[:, :, t0:t0 + w])

            def body(i, it):
                t0, w, ci = it
                i2 = i % 2
                x_, y_, q_, r_, ps = xb[i2], yb[i2], sq[i2], rr[i2], pss[i2]
                k.act(q_[:, :, 0:w], y_[:, :, 0:w], AF.Square)
                for kc in range(KC):
                    k.mm(ps[:, 0:w], self.ones, q_[:, kc, 0:w], start=(kc == 0), stop=(kc == KC - 1))
                self.rstd_from_ss(s, r_[:, 0:w], ps[:, 0:w], D, w)
                for kc in range(KC):
                    t_ = tm[kc % 3]
                    k.stt(t_[:, 0:w], y_[:, kc, 0:w], self.mvs(l, 3 * half + 2, kc, ci, ci + 1), r_[:, 0:w],
                          ALU.mult, ALU.mult)
                    k.tt(x_[:, kc, 0:w], x_[:, kc, 0:w], t_[:, 0:w], ALU.add, en="pool")
                k.dma("sp", xT[:, :, t0:t0 + w], x_[:, :, 0:w])
            pipeline(items, load, body)

    def phase_ffn(self, l):
        k, cfg = self.k, self.cfg
        KC, D, T, FF = cfg.KC, cfg.D_MODEL, cfg.T, cfg.D_FF
        JC = FF // 128
        hT = self.dr["hT"].rearrange("(c p) t -> p c t", p=128)
        yT = self.dr["yT"].rearrange("(c p) t -> p c t", p=128)
        wg_src = self.dr["w_ffn_gate"][l].rearrange("(c p) n -> p c n", p=128)
        wu_src = self.dr["w_ffn_up"][l].rearrange("(c p) n -> p c n", p=128)
        wd_src = self.dr["w_ffn_down"][l].rearrange("(c p) n -> p c n", p=128)
        NG = 1024
        CB = 256
        groups = []
        for (st, L, ci) in cfg.SEGS:
            for t0 in range(st, st + L, NG):
                groups.append((t0, min(NG, st + L - t0)))
        merged = []
        for g in groups:
            if merged and merged[-1][1] + g[1] <= NG and merged[-1][0] + merged[-1][1] == g[0]:
                merged[-1] = (merged[-1][0], merged[-1][1] + g[1])
            else:
                merged.append(g)
        for (t0, w) in merged:
            nb = (w + 511) // 512
            with k.scope() as s:
                aT = s.sb([128, JC, w], BF16)
                with k.scope() as s1:
                    h_ = s1.sb([128, KC, w], BF16)
                    k.dma("sp", h_[:, :, :], hT[:, :, t0:t0 + w])
                    wgb = [s1.sb([128, KC, CB], BF16) for _ in range(2)]
                    wub = [s1.sb([128, KC, CB], BF16) for _ in range(2)]
                    sg = [s1.sb([128, 512], F32) for _ in range(2)]
                    pg = [s1.ps([128, 512], F32) for _ in range(2)]
                    pu = [s1.ps([128, 512], F32) for _ in range(2)]
                    n = 0
                    for jb in range(FF // CB):
                        wg, wu = wgb[jb % 2], wub[jb % 2]
                        k.dma("pool", wg[:, :, :], wg_src[:, :, jb * CB:(jb + 1) * CB])
                        k.dma("pool", wu[:, :, :], wu_src[:, :, jb * CB:(jb + 1) * CB])
                        for jj in range(CB // 128):
                            j = jb * (CB // 128) + jj
                            for b in range(nb):
                                c0, c1 = b * 512, min(w, (b + 1) * 512)
                                cw = c1 - c0
                                g_, u_, s_ = pg[n % 2], pu[n % 2], sg[n % 2]
                                n += 1
                                for kc in range(KC):
                                    k.mm(g_[:, 0:cw], wg[:, kc, jj * 128:(jj + 1) * 128], h_[:, kc, c0:c1],
                                         start=(kc == 0), stop=(kc == KC - 1))
                                for kc in range(KC):
                                    k.mm(u_[:, 0:cw], wu[:, kc, jj * 128:(jj + 1) * 128], h_[:, kc, c0:c1],
                                         start=(kc == 0), stop=(kc == KC - 1))
                                k.act(s_[:, 0:cw], g_[:, 0:cw], AF.Silu)
                                k.tt(aT[:, j, c0:c1], s_[:, 0:cw], u_[:, 0:cw], ALU.mult)
                with k.scope() as s2:
                    MB = 2
                    JB = 11 if JC % 11 == 0 else (JC if JC <= 12 else 6)
                    assert JC % JB == 0
                    wdb = [s2.sb([128, JB, MB * 128], BF16) for _ in range(3)]
                    yo = [s2.sb([128, MB, w], F32) for _ in range(2)]
                    pss = [s2.ps([128, 512], F32) for _ in range(8)]
                    nw_ = 0
                    for mg in range(KC // MB):
                        pb = (mg % 2) * 4
                        for jb in range(JC // JB):
                            wd = wdb[nw_ % 3]
                            nw_ += 1
                            k.dma("pool", wd[:, :, :], wd_src[:, jb * JB:(jb + 1) * JB, mg * MB * 128:(mg + 1) * MB * 128])
                            for jj in range(JB):
                                j = jb * JB + jj
                                for mm_ in range(MB):
                                    for b in range(nb):
                                        c0, c1 = b * 512, min(w, (b + 1) * 512)
                                        k.mm(pss[pb + mm_ * 2 + b][:, 0:c1 - c0], wd[:, jj, mm_ * 128:(mm_ + 1) * 128],
                                             aT[:, j, c0:c1], start=(j == 0), stop=(j == JC - 1))
                        yb = yo[mg % 2]
                        for mm_ in range(MB):
                            for b in range(nb):
                                c0, c1 = b * 512, min(w, (b + 1) * 512)
                                k.copy(yb[:, mm_, c0:c1], pss[pb + mm_ * 2 + b][:, 0:c1 - c0],
                                       en=("act" if (mm_ + b) % 2 else "dve"))
                        k.dma("sp", yT[:, mg * MB:(mg + 1) * MB, t0:t0 + w], yb[:, :, :])

    def build(self):
        cfg, k = self.cfg, self.k
        D, T, NL, FF = cfg.D_MODEL, cfg.T, cfg.DEPTH, cfg.D_FF
        self.din("x_in", [T, D])
        self.din("cond", [2, D])
        self.din("c_ident", [128, 128])
        self.din("w_mod", [NL, D, 6 * D])
        self.din("b_mod", [NL, 6 * D])
        for nm in ("norm_mix_pre", "norm_mix_post", "norm_ffn_pre", "norm_ffn_post"):
            self.din(nm, [NL, D])
        self.din("w_ffn_gate", [NL, D, FF])
        self.din("w_ffn_up", [NL, D, FF])
        self.din("w_ffn_down", [NL, FF, D])
        self.declare_mixer_io()
        self.dout("y", [T, D])
        self.dtmp("xT", [D, T])
        self.dtmp("hT", [D, T], BF16)
        self.dtmp("yT", [D, T])
        with k.scope() as S:
            self.phase_consts(S)
            self.phase_mod(S)
            self.mark("mod")
            self.phase_in()
            self.mark("in")
            for l in range(NL):
                if getattr(cfg, "MIXERS", True):
                    self.phase_norm(l, 0)
                    self.mark(f"L{l}.norm1")
                    if l % 2 == 0:
                        if getattr(cfg, "SKIP_AB", False):
                            continue_ = True
                        else:
                            self.mixer_ab(l // 2)
                            self.phase_post(l, 0)
                            self.mark(f"L{l}.post1")
                    else:
                        self.mixer_c(l // 2)
                        self.phase_post(l, 0)
                        self.mark(f"L{l}.post1")
                self.phase_norm(l, 1)
                self.mark(f"L{l}.norm2")
                self.phase_ffn(l)
                self.mark(f"L{l}.ffn")
                self.phase_post(l, 1)
                self.mark(f"L{l}.post2")
            self.phase_out()
            self.mark("out")
        k.es.close()
        return self.nc

    def declare_mixer_io(self):
        declare_mixer_c(self)
        declare_mixer_ab(self)

    def mixer_ab(self, e):
        mixer_ab_impl(self, e)

    def mixer_c(self, o):
        mixer_c_impl(self, o)


def host_consts(cfg):
    out = {"c_ident": np.eye(128, dtype=np.float32)}
    out.update(hy_host_consts(cfg))
    out.update(ab_host_consts(cfg))
    return out


def make_in_maps(cfg, inputs):
    consts = host_consts(cfg)
    maps = []
    for core in range(cfg.N_CORES):
        m = dict(consts)
        xs = inputs["x_sample"][core]
        xp = inputs["x_prompt"][core * cfg.NPROMPT:(core + 1) * cfg.NPROMPT].reshape(-1, cfg.D_MODEL)
        m["x_in"] = np.ascontiguousarray(np.concatenate([xs, xp], axis=0))
        m["cond"] = np.ascontiguousarray(np.stack([inputs["c"][core], inputs["c_ctx"]], axis=0))
        for nm in ("w_mod", "b_mod", "norm_mix_pre", "norm_mix_post", "norm_ffn_pre", "norm_ffn_post",
                   "w_ffn_gate", "w_ffn_up", "w_ffn_down") + MIXC_WEIGHTS + MIXAB_WEIGHTS:
            m[nm] = inputs[nm]
        ab_core_inputs(cfg, m, inputs, core)
        maps.append(m)
    return maps


def run(cfg, inputs, trace=False):
    prog = Prog(cfg)
    nc = prog.build()
    maps = make_in_maps(cfg, inputs)
    res = run_bass_kernel_spmd(nc, maps, core_ids=list(range(cfg.N_CORES)))
    ys = np.stack([r["y"][:cfg.DEC_SEQ] for r in res.results], axis=0)
    yp = np.concatenate([r["y"][cfg.DEC_SEQ:].reshape(cfg.NPROMPT, cfg.SEQ, cfg.D_MODEL) for r in res.results], axis=0)
    nsr = np.concatenate([r["nsr"] for r in res.results], axis=0)
    nsg = np.concatenate([r["nsg"] for r in res.results], axis=0)
    return (ys, yp, nsr, nsg), res, prog


_CACHE = {}


def kernel(**inputs):
    cfg = Cfg()
    inputs = {k_: np.asarray(v) for k_, v in inputs.items()}
    outs, res, prog = run(cfg, inputs)
    ys, yp, nsr, nsg = outs
    return (np.ascontiguousarray(yp, dtype=np.float32), np.ascontiguousarray(ys, dtype=np.float32),
            np.ascontiguousarray(nsr, dtype=np.float32), np.ascontiguousarray(nsg, dtype=np.float32))


def pipeline(items, load, body):
    if not items:
        return
    load(0, items[0])
    for i, it in enumerate(items):
        if i + 1 < len(items):
            load(i + 1, items[i + 1])
        body(i, it)


def token_groups(cfg, NG=1024):
    groups = []
    for (st, L, ci) in cfg.SEGS:
        for t0 in range(st, st + L, NG):
            groups.append((t0, min(NG, st + L - t0)))
    merged = []
    for g in groups:
        if merged and merged[-1][1] + g[1] <= NG and merged[-1][0] + merged[-1][1] == g[0]:
            merged[-1] = (merged[-1][0], merged[-1][1] + g[1])
        else:
            merged.append(g)
    return merged


def proj_fm(P, w_dram, n_in, ncols, in_dram, out_dram, out_dt, groups, col0=0, out_row0=0, CB=256):
    k = P.k
    KCi = n_in // 128
    wsrc = w_dram.rearrange("(c p) n -> p c n", p=128)
    inT = in_dram.rearrange("(c p) t -> p c t", p=128)
    blocks = []
    c = 0
    while c < ncols:
        bw = min(CB, ncols - c)
        blocks.append((c, bw))
        c += bw
    for (t0, w) in groups:
        nb = (w + 511) // 512
        with k.scope() as s:
            h_ = s.sb([128, KCi, w], BF16)
            k.dma("sp", h_[:, :, :], inT[:, :, t0:t0 + w])
            wbs = [s.sb([128, KCi, CB], BF16) for _ in range(2)]
            obs = [s.sb([128, CB // 128, w], out_dt) for _ in range(2)]
            pss = [s.ps([128, 512], F32) for _ in range(4)]
            n = 0
            for bi, (c0, bw) in enumerate(blocks):
                wb, ob = wbs[bi % 2], obs[bi % 2]
                k.dma("pool", wb[:, :, 0:bw], wsrc[:, :, col0 + c0:col0 + c0 + bw])
                nch = (bw + 127) // 128
                for jj in range(nch):
                    m = min(128, bw - jj * 128)
                    for b in range(nb):
                        a0, a1 = b * 512, min(w, (b + 1) * 512)
                        ps = pss[n % 4]
                        for kc in range(KCi):
                            k.mm(ps[0:m, 0:a1 - a0], wb[:, kc, jj * 128:jj * 128 + m], h_[:, kc, a0:a1],
                                 start=(kc == 0), stop=(kc == KCi - 1))
                        k.copy(ob[0:m, jj, a0:a1], ps[0:m, 0:a1 - a0], en=("act" if n % 2 else "dve"))
                        n += 1
                r0 = out_row0 + c0
                if bw % 128 == 0:
                    k.dma("sp", out_dram[r0:r0 + bw, t0:t0 + w].rearrange("(c p) t -> p c t", p=128), ob[:, 0:nch, :])
                else:
                    assert bw < 128
                    k.dma("sp", out_dram[r0:r0 + bw, t0:t0 + w], ob[0:bw, 0, :])


def hy_sizes(L):
    N = 2 * L
    NK = L // 128
    NFC = (L + 1 + 127) // 128
    return N, NK, NFC


def hy_host_consts(cfg):
    out = {}
    for L in sorted({cfg.DEC_SEQ, cfg.SEQ}):
        N, NK, NFC = hy_sizes(L)
        FP = NFC * 128
        n = np.arange(L, dtype=np.float64)
        f = np.arange(FP, dtype=np.float64)
        ang = 2.0 * np.pi * ((n[:, None] * f[None, :]) % N) / N
        valid = (f <= L)[None, :]
        C = np.where(valid, np.cos(ang), 0.0)
        S = np.where(valid, -np.sin(ang), 0.0)
        fw = np.stack([C, S]).reshape(2, NK, 128, NFC, 128).transpose(0, 3, 2, 1, 4)
        out[f"hy_dft_{L}"] = np.ascontiguousarray(fw).astype(NPBF)
        wf = np.where((f == 0) | (f == L), 1.0, 2.0) * (f <= L) / N
        Ci = (wf[:, None] * np.cos(ang.T))
        Si = (-wf[:, None] * np.sin(ang.T))
        nbw = min(512, L)
        iv = np.stack([Ci, Si]).reshape(2, NFC, 128, L // nbw, nbw).transpose(3, 2, 0, 1, 4)
        out[f"hy_idft_{L}"] = np.ascontiguousarray(iv).astype(NPBF)
        t = np.linspace(0.0, 1.0, L, dtype=np.float32).astype(np.float64)
        w_ang = 2.0 * math.pi * np.arange(L, dtype=np.float32).astype(np.float64) / L
        bands = np.linspace(1e-4, cfg.HY_BANDS - 1, cfg.HY_BANDS, dtype=np.float32).astype(np.float64)
        z = np.concatenate([t[:, None], np.cos(bands[None] * w_ang[:, None]), -np.sin(bands[None] * w_ang[:, None])], -1)
        out[f"hy_zT_{L}"] = np.ascontiguousarray(z.T).astype(np.float32)
        out[f"hy_negt_{L}"] = np.ascontiguousarray((-t).reshape(NK, 128).T).astype(np.float32)
    D = cfg.D_MODEL
    deltas = np.abs(np.linspace(math.log(cfg.HY_TARGET) / cfg.HY_SLOW_DECAY, math.log(cfg.HY_TARGET) / cfg.HY_FAST_DECAY,
                                D, dtype=np.float32))
    out["hy_deltas"] = np.ascontiguousarray(np.broadcast_to(deltas[None, :], (128, D))).astype(np.float32)
    return out


def _range_reduce(k, y, m):
    PI = math.pi
    for _ in range(2):
        k.ts(m, y, -PI, 2.0 * PI, op0=ALU.is_lt, op1=ALU.mult)
        k.tt(y, y, m, ALU.add)
        k.ts(m, y, PI, -2.0 * PI, op0=ALU.is_gt, op1=ALU.mult)
        k.tt(y, y, m, ALU.add)


def hy_filters(P, o, L):
    k, cfg = P.k, P.cfg
    D, HF, EMB = cfg.D_MODEL, cfg.HY_FFN, cfg.HY_EMB
    N, NK, NFC = hy_sizes(L)
    BW = min(512, L)
    with k.scope() as s:
        w1 = s.sb([EMB, HF], F32)
        k.dma("sp", w1[:, :], P.dr["hy_w1"][o])
        w2 = s.sb([HF, HF], F32)
        k.dma("sp", w2[:, :], P.dr["hy_w2"][o])
        w3 = s.sb([HF, 2 * D], F32)
        k.dma("sp", w3[:, :], P.dr["hy_w3"][o])
        zT = s.sb([EMB, L], F32)
        k.dma("sp", zT[:, :], P.dr[f"hy_zT_{L}"])
        vec = s.sb([HF, 4], F32)
        k.dma("sp", vec[:, 0:1], P.dr["hy_b1"][o].rearrange("(p o) -> p o", o=1))
        k.dma("sp", vec[:, 1:2], P.dr["hy_freq"][o].rearrange("(p o) -> p o", o=1))
        k.dma("sp", vec[:, 2:3], P.dr["hy_b2"][o].rearrange("(p o) -> p o", o=1))
        fb = s.sb([HF, 2], F32)
        k.tt(fb[:, 0:1], vec[:, 0:1], vec[:, 1:2], ALU.mult)
        k.tt(fb[:, 1:2], vec[:, 2:3], vec[:, 1:2], ALU.mult)
        negt = s.sb([128, NK], F32)
        k.dma("sp", negt[:, :], P.dr[f"hy_negt_{L}"])
        dl = s.sb([128, D], F32)
        k.dma("sp", dl[:, :], P.dr["hy_deltas"])
        bias = s.sb([1, D], F32)
        k.dma("sp", bias[:, :], P.dr["hy_bias"][o].rearrange("(o n) -> o n", o=1))
        h1 = s.sb([HF, L], F32)
        h2 = s.sb([HF, L], F32)
        mk = s.sb([HF, BW], F32)
        ps = s.ps([128, 512], F32)
        for (src, wgt, kdim, dst, fbi) in ((zT, w1, EMB, h1, 0), (h1, w2, HF, h2, 1)):
            for b0 in range(0, L, BW):
                k.mm(ps[0:HF, 0:BW], wgt[0:kdim, :], src[0:kdim, b0:b0 + BW])
                y = dst[:, b0:b0 + BW]
                k.ts(y, ps[0:HF, 0:BW], vec[:, 1:2], fb[:, fbi:fbi + 1], op0=ALU.mult, op1=ALU.add)
                _range_reduce(k, y, mk[:, :])
                k.act(y, y, AF.Sin)
        win = s.sb([128, D], F32)
        hf = s.sb([128, D], F32)
        hb = s.sb([128, D], F32)
        At = [s.sb([128, D], BF16) for _ in range(2)]
        Bt = [s.sb([128, D], BF16) for _ in range(2)]
        pss = [s.ps([128, 512], F32) for _ in range(2)]
        CW = min(512, D)
        n = 0
        for nk in range(NK):
            k.act(win[:, :], dl[:, :], AF.Exp, scale=negt[:, nk:nk + 1])
            for half, dst in ((0, hf), (1, hb)):
                for c0 in range(0, D, CW):
                    p_ = pss[n % 2]
                    n += 1
                    k.mm(p_[:, 0:CW], h2[0:HF, nk * 128:(nk + 1) * 128], w3[0:HF, half * D + c0:half * D + c0 + CW])
                    k.tt(dst[:, c0:c0 + CW], p_[:, 0:CW], win[:, c0:c0 + CW], ALU.mult)
            if nk == 0:
                k.memset(hb[0:1, :], 0.0)
                k.tt(hf[0:1, :], hf[0:1, :], bias[0:1, :], ALU.add)
            a_, b_ = At[nk % 2], Bt[nk % 2]
            k.tt(a_[:, :], hf[:, :], hb[:, :], ALU.add, en="pool")
            k.tt(b_[:, :], hf[:, :], hb[:, :], ALU.subtract, en="pool")
            k.dma("sp", P.dr["hy_A"][nk * 128:(nk + 1) * 128, :], a_[:, :])
            k.dma("sp", P.dr["hy_B"][nk * 128:(nk + 1) * 128, :], b_[:, :])


def hy_fwd(P, L, srcA, srcB, emit, NH=1):
    k, cfg = P.k, P.cfg
    D = cfg.D_MODEL
    N, NK, NFC = hy_sizes(L)
    HW = min(512, D)
    CW = min(NH * HW, D)
    NH = CW // HW
    dft = P.dr[f"hy_dft_{L}"]
    for c0 in range(0, D, CW):
        with k.scope() as s:
            A_ = s.sb([128, NK, CW], BF16)
            k.dma("sp", A_[:, :, :], srcA[:, c0:c0 + CW].rearrange("(k p) c -> p k c", p=128))
            if srcB is srcA:
                B_ = A_
            else:
                B_ = s.sb([128, NK, CW], BF16)
                k.dma("sp", B_[:, :, :], srcB[:, c0:c0 + CW].rearrange("(k p) c -> p k c", p=128))
            dcs = [s.sb([128, NK, 128], BF16) for _ in range(2)]
            dss = [s.sb([128, NK, 128], BF16) for _ in range(2)]
            pre = [[s.ps([128, 512], F32) for _ in range(NH)] for _ in range(2)]
            pim = [[s.ps([128, 512], F32) for _ in range(NH)] for _ in range(2)]

            def load(i, fc):
                k.dma("sp", dcs[i % 2][:, :, :], dft[0, fc])
                k.dma("sp", dss[i % 2][:, :, :], dft[1, fc])

            def body(i, fc):
                dc, ds = dcs[i % 2], dss[i % 2]
                for hf in range(NH):
                    p_re, p_im = pre[i % 2][hf], pim[i % 2][hf]
                    for nk in range(NK):
                        k.mm(p_re[:, 0:HW], dc[:, nk, :], A_[:, nk, hf * HW:(hf + 1) * HW], start=(nk == 0), stop=(nk == NK - 1))
                    for nk in range(NK):
                        k.mm(p_im[:, 0:HW], ds[:, nk, :], B_[:, nk, hf * HW:(hf + 1) * HW], start=(nk == 0), stop=(nk == NK - 1))
                    emit(s, fc, c0 + hf * HW, HW, p_re, p_im)
            pipeline(list(range(NFC)), load, body)


def hy_filter_spectrum(P, L):
    k = P.k
    st = {"n": 0}

    def emit(s, fc, c0, CW, p_re, p_im):
        if "s" not in st or st["s"] is not s:
            st["s"] = s
            st["o"] = [s.sb([128, 2, CW], F32) for _ in range(2)]
        ob = st["o"][st["n"] % 2]
        st["n"] += 1
        k.copy(ob[:, 0, :], p_re[:, 0:CW], en="dve")
        k.copy(ob[:, 1, :], p_im[:, 0:CW], en="act")
        k.dma("sp", P.dr["hy_H"][:, fc * 128:(fc + 1) * 128, c0:c0 + CW].rearrange("s p c -> p s c"), ob[:, :, :])
    hy_fwd(P, L, P.dr["hy_A"][0:L, :], P.dr["hy_B"][0:L, :], emit)


def hy_data_spectrum(P, L, tok0):
    k = P.k
    st = {"n": 0}

    def emit(s, fc, c0, CW, p_re, p_im):
        if "s" not in st or st["s"] is not s:
            st["s"] = s
            st["h"] = [s.sb([128, 2, CW], F32) for _ in range(2)]
            st["t"] = [s.sb([128, 4, CW], F32) for _ in range(2)]
            st["p"] = [s.sb([128, 2, CW], BF16) for _ in range(2)]
        i2 = st["n"] % 2
        st["n"] += 1
        hb, tb, pb = st["h"][i2], st["t"][i2], st["p"][i2]
        k.dma("sp", hb[:, :, :], P.dr["hy_H"][:, fc * 128:(fc + 1) * 128, c0:c0 + CW].rearrange("s p c -> p s c"))
        k.tt(tb[:, 0, :], p_re[:, 0:CW], hb[:, 0, :], ALU.mult)
        k.tt(tb[:, 1, :], p_im[:, 0:CW], hb[:, 1, :], ALU.mult)
        k.tt(tb[:, 2, :], p_re[:, 0:CW], hb[:, 1, :], ALU.mult)
        k.tt(tb[:, 3, :], p_im[:, 0:CW], hb[:, 0, :], ALU.mult)
        k.tt(pb[:, 0, :], tb[:, 0, :], tb[:, 1, :], ALU.subtract, en="pool")
        k.tt(pb[:, 1, :], tb[:, 2, :], tb[:, 3, :], ALU.add, en="pool")
        k.dma("sp", P.dr["hy_P"][:, fc * 128:(fc + 1) * 128, c0:c0 + CW].rearrange("s p c -> p s c"), pb[:, :, :])
    src = P.dr["hy_wtok"][tok0:tok0 + L, :]
    hy_fwd(P, L, src, src, emit, NH=2)


def hy_inverse(P, L, tok0):
    k, cfg = P.k, P.cfg
    D = cfg.D_MODEL
    N, NK, NFC = hy_sizes(L)
    FK = 2 * NFC
    PZ = 11 if FK % 11 == 0 else FK
    assert FK % PZ == 0 and PZ <= 12
    BWn = min(512, L)
    CW = min(512, D)
    NCH = CW // 128
    idft = P.dr[f"hy_idft_{L}"]
    for c0 in range(0, D, CW):
        with k.scope() as s:
            P_ = s.sb([128, 2, NFC, CW], BF16)
            for cs in range(2):
                k.dma("sp", P_[:, cs, :, :], P.dr["hy_P"][cs, 0:NFC * 128, c0:c0 + CW].rearrange("(c p) n -> p c n", p=128))
            idb = [s.sb([128, PZ, BWn], BF16) for _ in range(3)]
            x0b = [s.sb([128, NCH, BWn], BF16) for _ in range(2)]
            gb = [s.sb([128, NCH, BWn], BF16) for _ in range(2)]
            pss = [s.ps([128, 512], F32) for _ in range(8)]
            ni = 0
            for nb in range(L // BWn):
                pset = pss[(nb % 2) * 4:(nb % 2) * 4 + 4]
                xb, g_ = x0b[nb % 2], gb[nb % 2]
                t0 = tok0 + nb * BWn
                k.dma("sp", xb[:, :, :], P.dr["hy_x0"][c0:c0 + CW, t0:t0 + BWn].rearrange("(c p) t -> p c t", p=128))
                for pz in range(FK // PZ):
                    ib = idb[ni % 3]
                    ni += 1
                    k.dma("sp", ib[:, :, :], idft[nb].rearrange("p s c n -> p (s c) n")[:, pz * PZ:(pz + 1) * PZ, :])
                    for i in range(PZ):
                        fk = pz * PZ + i
                        cs, fc = fk // NFC, fk % NFC
                        for c in range(NCH):
                            k.mm(pset[c][:, 0:BWn], P_[:, cs, fc, c * 128:(c + 1) * 128], ib[:, i, :],
                                 start=(fk == 0), stop=(fk == FK - 1))
                for c in range(NCH):
                    k.tt(g_[:, c, :], pset[c][:, 0:BWn], xb[:, c, :], ALU.mult)
                k.dma("sp", P.dr["hy_gT"][c0:c0 + CW, t0:t0 + BWn].rearrange("(c p) t -> p c t", p=128), g_[:, :, :])


def hy_conv(P, o):
    k, cfg = P.k, P.cfg
    D = cfg.D_MODEL
    CW = min(256, D)
    NCH = CW // 128
    NC3 = 3 * D // 128
    TB = min(1024, max(L for (_, L, _) in cfg.SEGS))
    NJ = TB // 128
    with k.scope() as s:
        cw = s.sb([128, 3, NC3], F32)
        for j in range(3):
            P.load_fm_vec(s, cw[:, j, :], P.dr["hy_conv_w"][o, j], NC3)
        us = [[s.sb([128, NCH, TB + 2], F32) for _ in range(3)] for _ in range(2)]
        cs2 = [[s.sb([128, NCH, TB], F32) for _ in range(3)] for _ in range(2)]
        x0o = [s.sb([128, NCH, TB], BF16) for _ in range(2)]
        wo = [s.sb([128, NCH, TB], BF16) for _ in range(2)]
        wt = [s.sb([128, NJ, CW], BF16) for _ in range(2)]
        JP = max(1, 512 // CW)
        psb = [s.ps([128, 512], BF16) for _ in range(3)]
        items = []
        for (st, L, ci) in cfg.SEGS:
            for t0 in range(st, st + L, TB):
                tw = min(TB, st + L - t0)
                for c0 in range(0, D, CW):
                    items.append((st, L, t0, tw, c0))
        nt = [0]

        def load(i, it):
            st, L, t0, tw, c0 = it
            lo = max(st, t0 - 1)
            hi = min(st + L, t0 + tw + 1)
            for part in range(3):
                ub = us[i % 2][part]
                if lo == t0:
                    k.memset(ub[:, :, 0:1], 0.0, en="pool")
                if hi == t0 + tw:
                    k.memset(ub[:, :, tw + 1:tw + 2], 0.0, en="pool")
                r0 = part * D + c0
                k.dma("sp", ub[:, :, lo - (t0 - 1):hi - (t0 - 1)],
                      P.dr["hy_uT"][r0:r0 + CW, lo:hi].rearrange("(c p) t -> p c t", p=128))

        def body(i, it):
            st, L, t0, tw, c0 = it
            u3, cs_ = us[i % 2], cs2[i % 2]
            for part in range(3):
                ub = u3[part]
                cb = cs_[part]
                for c in range(NCH):
                    ch = (part * D + c0) // 128 + c
                    k.act(cb[:, c, 0:tw], ub[:, c, 1:tw + 1], AF.Identity, scale=cw[:, 1, ch:ch + 1])
                    k.stt(cb[:, c, 0:tw], ub[:, c, 0:tw], cw[:, 0, ch:ch + 1], cb[:, c, 0:tw], ALU.mult, ALU.add)
                    k.stt(cb[:, c, 0:tw], ub[:, c, 2:tw + 2], cw[:, 2, ch:ch + 1], cb[:, c, 0:tw], ALU.mult, ALU.add)
            xo, w_, wt_ = x0o[i % 2], wo[i % 2], wt[i % 2]
            k.copy(xo[:, :, 0:tw], cs_[0][:, :, 0:tw], en="pool")
            k.tt(w_[:, :, 0:tw], cs_[2][:, :, 0:tw], cs_[1][:, :, 0:tw], ALU.mult, en="pool")
            k.dma("sp", P.dr["hy_x0"][c0:c0 + CW, t0:t0 + tw].rearrange("(c p) t -> p c t", p=128), xo[:, :, 0:tw])
            nj = tw // 128
            for j0 in range(0, nj, JP):
                n_ = nt[0]
                nt[0] += 1
                pb = psb[n_ % 3]
                jn = min(JP, nj - j0)
                for jj in range(jn):
                    for c in range(NCH):
                        k.tr(pb[:, jj * CW + c * 128:jj * CW + (c + 1) * 128], w_[:, c, (j0 + jj) * 128:(j0 + jj + 1) * 128], P.identh)
                k.copy(wt_[:, j0:j0 + jn, :], pb[:, 0:jn * CW].rearrange("p (j c) -> p j c", c=CW), en=("act" if n_ % 2 else "dve"))
            k.dma("sp", P.dr["hy_wtok"][t0:t0 + tw, c0:c0 + CW].rearrange("(j p) c -> p j c", p=128), wt_[:, 0:nj, :])
        pipeline(items, load, body)


def mixer_c_impl(P, o):
    k, cfg = P.k, P.cfg
    D = cfg.D_MODEL
    groups = token_groups(cfg)
    proj_fm(P, P.dr["w_in_c"][o], D, 3 * D, P.dr["hT"], P.dr["hy_uT"], F32, groups)
    P.mark(f"c{o}.inproj")
    hy_conv(P, o)
    P.mark(f"c{o}.conv")
    done_L = None
    for (st, L, ci) in cfg.SEGS:
        if L != done_L:
            hy_filters(P, o, L)
            P.mark(f"c{o}.filters{L}")
            hy_filter_spectrum(P, L)
            P.mark(f"c{o}.fspec{L}")
            done_L = L
        hy_data_spectrum(P, L, st)
        P.mark(f"c{o}.dspec{st}")
        hy_inverse(P, L, st)
        P.mark(f"c{o}.inv{st}")
    proj_fm(P, P.dr["w_out_c"][o], D, D, P.dr["hy_gT"], P.dr["yT"], F32, groups)
    P.mark(f"c{o}.outproj")


def declare_mixer_c(P):
    cfg = P.cfg
    D, T, NO = cfg.D_MODEL, cfg.T, cfg.N_ODD
    P.din("w_in_c", [NO, D, 3 * D])
    P.din("hy_conv_w", [NO, 3, 3 * D])
    P.din("w_out_c", [NO, D, D])
    P.din("hy_w1", [NO, cfg.HY_EMB, cfg.HY_FFN])
    P.din("hy_b1", [NO, cfg.HY_FFN])
    P.din("hy_freq", [NO, cfg.HY_FFN])
    P.din("hy_w2", [NO, cfg.HY_FFN, cfg.HY_FFN])
    P.din("hy_b2", [NO, cfg.HY_FFN])
    P.din("hy_w3", [NO, cfg.HY_FFN, 2 * D])
    P.din("hy_bias", [NO, D])
    P.din("hy_deltas", [128, D])
    Lmax = max(cfg.DEC_SEQ, cfg.SEQ)
    for L in sorted({cfg.DEC_SEQ, cfg.SEQ}):
        N, NK, NFC = hy_sizes(L)
        P.din(f"hy_dft_{L}", [2, NFC, 128, NK, 128], BF16)
        P.din(f"hy_idft_{L}", [L // min(512, L), 128, 2, NFC, min(512, L)], BF16)
        P.din(f"hy_zT_{L}", [cfg.HY_EMB, L])
        P.din(f"hy_negt_{L}", [128, NK])
    FPmax = hy_sizes(Lmax)[2] * 128
    P.dtmp("hy_uT", [3 * D, T])
    P.dtmp("hy_x0", [D, T], BF16)
    P.dtmp("hy_wtok", [T, D], BF16)
    P.dtmp("hy_gT", [D, T], BF16)
    P.dtmp("hy_A", [Lmax, D], BF16)
    P.dtmp("hy_B", [Lmax, D], BF16)
    P.dtmp("hy_H", [2, FPmax, D])
    P.dtmp("hy_P", [2, FPmax, D], BF16)


MIXC_WEIGHTS = ("w_in_c", "hy_conv_w", "w_out_c", "hy_w1", "hy_b1", "hy_freq", "hy_w2", "hy_b2", "hy_w3", "hy_bias")


MIXAB_WEIGHTS = ("w_in_ab", "w_out_ab", "ret_decay", "ret_gn_w", "gdn_conv_w", "gdn_a_log", "gdn_dt_bias", "gdn_norm_w")
CH = 128
NEG = -30000.0


def ab_host_consts(cfg):
    out = {}
    i = np.arange(CH)
    s_, t_ = i[:, None], i[None, :]
    relf = np.where(t_ >= s_, (t_ - s_).astype(np.float32), 1e9)
    relb = np.where(s_ >= t_, (s_ - t_).astype(np.float32), 1e9)
    out["ab_rel"] = np.stack([relf, relb]).astype(np.float32)
    qd = np.stack([np.broadcast_to((i + 1.0)[None, :], (CH, CH)), np.broadcast_to((CH - i * 1.0)[None, :], (CH, CH))])
    out["ab_qexp"] = np.ascontiguousarray(qd).astype(np.float32)
    out["ab_kexp"] = np.stack([CH - 1.0 - i, i * 1.0], axis=1).astype(np.float32)
    Uf = (s_ <= t_).astype(np.float32)
    Ub = (s_ >= t_).astype(np.float32)
    out["ab_U"] = np.stack([Uf, Ub]).astype(np.float32)
    out["ab_negm"] = np.stack([np.where(s_ <= t_, 0.0, NEG), np.where(s_ >= t_, 0.0, NEG)]).astype(np.float32)
    out["ab_strict"] = np.stack([(s_ < t_), (s_ > t_)]).astype(np.float32)
    lm = np.zeros((2, 7, CH, CH), np.float32)
    for lv in range(7):
        m = 1 << lv
        tt_, ss_ = i[:, None], i[None, :]
        pat = ((tt_ // (2 * m)) == (ss_ // (2 * m))) & ((tt_ % (2 * m)) >= m) & ((ss_ % (2 * m)) < m)
        lm[0, lv] = -pat.astype(np.float32)
        lm[1, lv] = -pat.T.astype(np.float32)
    out["ab_lm"] = lm
    out["ab_lmT"] = np.ascontiguousarray(lm.transpose(0, 1, 3, 2))
    L = cfg.DEC_SEQ
    rows = L // cfg.GRID_W
    r = np.repeat(np.arange(rows, dtype=np.float32), cfg.GRID_W)
    col = (np.arange(rows * cfg.GRID_W) % cfg.GRID_W).astype(np.float32)
    n_freq = cfg.DK_A // 4
    inv = (np.float32(cfg.ROPE_BASE) ** (-np.arange(n_freq, dtype=np.float32) / n_freq)).astype(np.float32)
    ang = np.concatenate([r[:, None] * inv, col[:, None] * inv], axis=-1).astype(np.float32)
    out["ab_rope"] = np.ascontiguousarray(np.stack([np.cos(ang).T, np.sin(ang).T])).astype(np.float32)
    return out


def ab_core_inputs(cfg, m, inputs, core):
    m["state_ret"] = np.ascontiguousarray(inputs["state_ret"][core])
    m["state_gdn"] = np.ascontiguousarray(inputs["state_gdn"][core])


def declare_mixer_ab(P):
    cfg = P.cfg
    D, T, NE = cfg.D_MODEL, cfg.T, cfg.N_EVEN
    HA, DKA, DVA, HB, DKB, DVB = cfg.H_A, cfg.DK_A, cfg.DV_A, cfg.H_B, cfg.DK_B, cfg.DV_B
    P.din("w_in_ab", [NE, D, cfg.PROJ_AB])
    P.din("w_out_ab", [NE, cfg.MIX_AB_OUT, D])
    P.din("ret_decay", [NE, 2, HA])
    P.din("ret_gn_w", [NE, HA * DVA])
    P.din("gdn_conv_w", [NE, cfg.CONV_B, 3 * HB * DKB])
    P.din("gdn_a_log", [NE, 2, HB])
    P.din("gdn_dt_bias", [NE, 2, HB])
    P.din("gdn_norm_w", [NE, DVB])
    P.din("state_ret", [NE, 2, HA, DKA, DVA])
    P.din("state_gdn", [NE, 2, HB, DKB, DVB])
    P.din("ab_rel", [2, CH, CH])
    P.din("ab_qexp", [2, CH, CH])
    P.din("ab_kexp", [CH, 2])
    P.din("ab_U", [2, CH, CH])
    P.din("ab_negm", [2, CH, CH])
    P.din("ab_strict", [2, CH, CH])
    P.din("ab_lm", [2, 7, CH, CH])
    P.din("ab_lmT", [2, 7, CH, CH])
    P.din("ab_rope", [2, 128, cfg.DEC_SEQ])
    P.dout("nsr", [cfg.NPROMPT, NE, 2, HA, DKA, DVA])
    P.dout("nsg", [cfg.NPROMPT, NE, 2, HB, DKB, DVB])
    P.dtmp("ab_pT", [cfg.PROJ_AB, T])
    P.dtmp("ab_rqk", [2 * HA * DKA, T], BF16)
    P.dtmp("ab_rk_tok", [T, HA * DKA], BF16)
    P.dtmp("ab_rv_tok", [T, HA * DVA], BF16)
    P.dtmp("ab_rg_tok", [T, HA * DVA], BF16)
    P.dtmp("ab_gT", [3 * HB * DKB, T], BF16)
    P.dtmp("ab_gtok", [T, 2 * HB * DKB], BF16)
    P.dtmp("ab_gz_tok", [T, HB * DVB], BF16)
    P.dtmp("ab_gbT", [32, T])
    P.dtmp("ab_gb", [T, 32])
    P.dtmp("ab_o1r", [T, HA * DVA])
    P.dtmp("ab_o1g", [T, HB * DVB])
    P.dtmp("ab_catT", [cfg.MIX_AB_OUT, T], BF16)


def fm_to_tm(P, src, row0, nrows, dst, col0, src_dt, dst_dt, func=None):
    k, cfg = P.k, P.cfg
    T = cfg.T
    G = min(4, nrows // 128)
    ident = P.ident if src_dt == F32 else P.identh
    with k.scope() as s:
        ib = [s.sb([128, G, 512], src_dt) for _ in range(2)]
        ob = [s.sb([128, G * 128], dst_dt) for _ in range(3)]
        pss = [s.ps([128, 512], src_dt) for _ in range(3)]
        items = [(t0, min(512, T - t0), r0) for t0 in range(0, T, 512) for r0 in range(0, nrows, G * 128)]
        cnt = [0]

        def load(i, it):
            t0, tw, r0 = it
            k.dma("sp", ib[i % 2][:, :, 0:tw], src[row0 + r0:row0 + r0 + G * 128, t0:t0 + tw].rearrange("(c p) t -> p c t", p=128))

        def body(i, it):
            t0, tw, r0 = it
            i_ = ib[i % 2]
            for tj in range(tw // 128):
                m = cnt[0]
                cnt[0] += 1
                ps, o_ = pss[m % 3], ob[m % 3]
                for g in range(G):
                    k.tr(ps[:, g * 128:(g + 1) * 128], i_[:, g, tj * 128:(tj + 1) * 128], ident)
                if func is not None:
                    k.act(o_[:, :], ps[:, 0:G * 128], func)
                else:
                    k.copy(o_[:, :], ps[:, 0:G * 128], en=("act" if m % 2 else "dve"))
                k.dma("sp", dst[t0 + tj * 128:t0 + (tj + 1) * 128, col0 + r0:col0 + r0 + G * 128], o_[:, :])
        pipeline(items, load, body)


def ab_rope(P):
    k, cfg = P.k, P.cfg
    HA, DKA = cfg.H_A, cfg.DK_A
    qs = DKA ** -0.5
    with k.scope() as s:
        xs = [[s.sb([128, 512], F32) for _ in range(2)] for _ in range(2)]
        cs = [[s.sb([128, 512], F32) for _ in range(2)] for _ in range(2)]
        tm = [s.sb([128, 512], F32) for _ in range(4)]
        ob = [s.sb([128, 2, 512], BF16) for _ in range(2)]
        items = []
        for si, (st, L, ci) in enumerate(cfg.SEGS):
            for bi, t0 in enumerate(range(st, st + L, 512)):
                tw = min(512, st + L - t0)
                for qk in range(2):
                    for h in range(HA):
                        items.append((si, st, bi, t0, tw, qk, h))

        def load(i, it):
            si, st, bi, t0, tw, qk, h = it
            if si == 0 and qk == 0 and h == 0:
                c_, s_ = cs[bi % 2]
                k.dma("sp", c_[:, 0:tw], P.dr["ab_rope"][0, :, t0 - st:t0 - st + tw])
                k.dma("sp", s_[:, 0:tw], P.dr["ab_rope"][1, :, t0 - st:t0 - st + tw])
            x1, x2 = xs[i % 2]
            r0 = qk * HA * DKA + h * DKA
            k.dma("sp", x1[:, 0:tw], P.dr["ab_pT"][r0:r0 + 128, t0:t0 + tw])
            k.dma("sp", x2[:, 0:tw], P.dr["ab_pT"][r0 + 128:r0 + 256, t0:t0 + tw])

        def body(i, it):
            si, st, bi, t0, tw, qk, h = it
            c_, s_ = cs[bi % 2]
            x1, x2 = xs[i % 2]
            o_ = ob[i % 2]
            sc = qs if qk == 0 else 1.0
            r0 = qk * HA * DKA + h * DKA
            if si == 0:
                k.tt(tm[0][:, 0:tw], x1[:, 0:tw], c_[:, 0:tw], ALU.mult)
                k.tt(tm[1][:, 0:tw], x2[:, 0:tw], s_[:, 0:tw], ALU.mult, en="pool")
                k.tt(tm[2][:, 0:tw], x1[:, 0:tw], s_[:, 0:tw], ALU.mult)
                k.tt(tm[3][:, 0:tw], x2[:, 0:tw], c_[:, 0:tw], ALU.mult, en="pool")
                k.tt(tm[0][:, 0:tw], tm[0][:, 0:tw], tm[1][:, 0:tw], ALU.subtract)
                k.act(o_[:, 0, 0:tw], tm[0][:, 0:tw], AF.Identity, scale=sc)
                k.tt(tm[2][:, 0:tw], tm[2][:, 0:tw], tm[3][:, 0:tw], ALU.add)
                k.act(o_[:, 1, 0:tw], tm[2][:, 0:tw], AF.Identity, scale=sc)
            else:
                k.act(o_[:, 0, 0:tw], x1[:, 0:tw], AF.Identity, scale=sc)
                k.act(o_[:, 1, 0:tw], x2[:, 0:tw], AF.Identity, scale=sc)
            k.dma("sp", P.dr["ab_rqk"][r0:r0 + 256, t0:t0 + tw].rearrange("(c p) t -> p c t", p=128), o_[:, :, 0:tw])
        pipeline(items, load, body)


def ab_gdn_conv(P, e):
    k, cfg = P.k, P.cfg
    HB, DKB = cfg.H_B, cfg.DK_B
    NCH = 3 * HB * DKB // 128
    KT = cfg.CONV_B
    PD = KT // 2
    TB = min(2048, max(L for (_, L, _) in cfg.SEGS))
    row_base = 2 * cfg.H_A * cfg.DK_A + 2 * cfg.H_A * cfg.DV_A
    qs = DKB ** -0.5
    with k.scope() as s:
        cw = s.sb([128, KT, NCH], F32)
        for j in range(KT):
            P.load_fm_vec(s, cw[:, j, :], P.dr["gdn_conv_w"][e, j], NCH)
        ub = [s.sb([128, TB + 2 * PD], F32) for _ in range(2)]
        cb = [s.sb([128, TB], F32) for _ in range(2)]
        sq = [s.sb([128, TB], BF16) for _ in range(2)]
        rr = [s.sb([128, TB], F32) for _ in range(2)]
        ob = [s.sb([128, TB], BF16) for _ in range(2)]
        pss = [s.ps([128, 512], F32) for _ in range(4)]
        items = []
        for (st, L, ci) in cfg.SEGS:
            for t0 in range(st, st + L, TB):
                tw = min(TB, st + L - t0)
                for ch in range(NCH):
                    items.append((st, L, t0, tw, ch))
        pcnt = [0]

        def load(i, it):
            st, L, t0, tw, ch = it
            u_ = ub[i % 2]
            lo = max(st, t0 - PD)
            hi = min(st + L, t0 + tw + PD)
            if lo > t0 - PD:
                k.memset(u_[:, 0:PD], 0.0, en="pool")
            if hi < t0 + tw + PD:
                k.memset(u_[:, tw + PD:tw + 2 * PD], 0.0, en="pool")
            r0 = row_base + ch * 128
            k.dma("sp", u_[:, lo - (t0 - PD):hi - (t0 - PD)], P.dr["ab_pT"][r0:r0 + 128, lo:hi])

        def body(i, it):
            st, L, t0, tw, ch = it
            u_, c_, q_, r_, o_ = ub[i % 2], cb[i % 2], sq[i % 2], rr[i % 2], ob[i % 2]
            k.act(c_[:, 0:tw], u_[:, 0:tw], AF.Identity, scale=cw[:, 0, ch:ch + 1])
            for j in range(1, KT):
                k.stt(c_[:, 0:tw], u_[:, j:j + tw], cw[:, j, ch:ch + 1], c_[:, 0:tw], ALU.mult, ALU.add)
            k.act(c_[:, 0:tw], c_[:, 0:tw], AF.Silu)
            part = ch // (HB * DKB // 128)
            if part < 2:
                k.act(q_[:, 0:tw], c_[:, 0:tw], AF.Square)
                for b0 in range(0, tw, 512):
                    bw = min(512, tw - b0)
                    ps = pss[pcnt[0] % 4]
                    pcnt[0] += 1
                    k.mm(ps[:, 0:bw], P.ones, q_[:, b0:b0 + bw])
                    k.ts(r_[:, b0:b0 + bw], ps[:, 0:bw], cfg.EPS, None, op0=ALU.add)
                k.act(r_[:, 0:tw], r_[:, 0:tw], AF.Sqrt)
                k.recip(r_[:, 0:tw], r_[:, 0:tw])
                if part == 0:
                    k.stt(o_[:, 0:tw], c_[:, 0:tw], qs, r_[:, 0:tw], ALU.mult, ALU.mult)
                else:
                    k.tt(o_[:, 0:tw], c_[:, 0:tw], r_[:, 0:tw], ALU.mult, en="pool")
            else:
                k.copy(o_[:, 0:tw], c_[:, 0:tw], en="pool")
            k.dma("sp", P.dr["ab_gT"][ch * 128:(ch + 1) * 128, t0:t0 + tw], o_[:, 0:tw])
        pipeline(items, load, body)


def ab_gates(P, e):
    k, cfg = P.k, P.cfg
    T = cfg.T
    r0 = cfg.PROJ_AB - 32
    with k.scope() as s:
        par = s.sb([16, 2], F32)
        k.dma("sp", par[:, 0:1], P.dr["gdn_dt_bias"][e].rearrange("d (h o) -> (d h) o", o=1))
        k.dma("sp", par[:, 1:2], P.dr["gdn_a_log"][e].rearrange("d (h o) -> (d h) o", o=1))
        k.act(par[:, 1:2], par[:, 1:2], AF.Exp)
        k.ts(par[:, 1:2], par[:, 1:2], -1.0, None, op0=ALU.mult)
        xa = [s.sb([16, 512], F32) for _ in range(2)]
        xb = [s.sb([16, 512], F32) for _ in range(2)]
        t1 = [s.sb([16, 512], F32) for _ in range(2)]
        t2 = [s.sb([16, 512], F32) for _ in range(2)]
        for i, t0 in enumerate(range(0, T, 512)):
            tw = min(512, T - t0)
            a_, b_, u_, v_ = xa[i % 2], xb[i % 2], t1[i % 2], t2[i % 2]
            k.dma("sp", a_[:, 0:tw], P.dr["ab_pT"][r0:r0 + 16, t0:t0 + tw])
            k.dma("sp", b_[:, 0:tw], P.dr["ab_pT"][r0 + 16:r0 + 32, t0:t0 + tw])
            k.ts(a_[:, 0:tw], a_[:, 0:tw], par[:, 0:1], None, op0=ALU.add)
            k.ts(u_[:, 0:tw], a_[:, 0:tw], -1.0, None, op0=ALU.mult)
            k.tt(u_[:, 0:tw], u_[:, 0:tw], a_[:, 0:tw], ALU.max)
            k.act(u_[:, 0:tw], u_[:, 0:tw], AF.Exp, scale=-1.0)
            k.act(u_[:, 0:tw], u_[:, 0:tw], AF.Ln, bias=1.0)
            k.ts(v_[:, 0:tw], a_[:, 0:tw], 0.0, None, op0=ALU.max)
            k.tt(v_[:, 0:tw], v_[:, 0:tw], u_[:, 0:tw], ALU.add)
            k.ts(v_[:, 0:tw], v_[:, 0:tw], par[:, 1:2], None, op0=ALU.mult)
            k.act(b_[:, 0:tw], b_[:, 0:tw], AF.Sigmoid)
            k.dma("sp", P.dr["ab_gbT"][0:16, t0:t0 + tw], v_[:, 0:tw])
            k.dma("sp", P.dr["ab_gbT"][16:32, t0:t0 + tw], b_[:, 0:tw])
    with k.scope() as s:
        ib = [s.sb([32, 512], F32) for _ in range(2)]
        ob = [s.sb([128, 32], F32) for _ in range(2)]
        pss = [s.ps([128, 32], F32) for _ in range(2)]
        m = 0
        for i, t0 in enumerate(range(0, T, 512)):
            tw = min(512, T - t0)
            i_ = ib[i % 2]
            k.dma("sp", i_[:, 0:tw], P.dr["ab_gbT"][:, t0:t0 + tw])
            for tj in range(tw // 128):
                ps, o_ = pss[m % 2], ob[m % 2]
                m += 1
                k.tr(ps[:, :], i_[:, tj * 128:(tj + 1) * 128], P.ident[0:32, 0:32])
                k.copy(o_[:, :], ps[:, :])
                k.dma("sp", P.dr["ab_gb"][t0 + tj * 128:t0 + (tj + 1) * 128, :], o_[:, :])


def ab_retention(P, e):
    k, cfg = P.k, P.cfg
    HA, DKA, DVA = cfg.H_A, cfg.DK_A, cfg.DV_A
    NQ = HA * DKA // 128
    DC = DKA // 128
    EV = HA * DVA
    with k.scope() as S:
        lg = S.sb([128, 2 * HA], F32)
        k.dma("sp", lg[:, :], P.dr["ret_decay"][e].rearrange("d h -> (d h)").partition_broadcast(128))
        k.act(lg[:, :], lg[:, :], AF.Exp, scale=math.log(2.0))
        k.ts(lg[:, :], lg[:, :], -1.0, 1.0, op0=ALU.mult, op1=ALU.add)
        k.act(lg[:, :], lg[:, :], AF.Ln)
        rel = S.sb([128, 2, CH], F32)
        k.dma("sp", rel[:, :, :], P.dr["ab_rel"].rearrange("d s t -> s d t"))
        qex = S.sb([128, 2, CH], F32)
        k.dma("sp", qex[:, :, :], P.dr["ab_qexp"].rearrange("d s t -> s d t"))
        kex = S.sb([128, 2], F32)
        k.dma("sp", kex[:, :], P.dr["ab_kexp"])
        mask = S.sb([128, 2 * HA, CH], F32)
        qdec = S.sb([128, 2 * HA, CH], F32)
        kdec = S.sb([128, 2 * HA], F32)
        cdec = S.sb([128, 2 * HA], F32)
        for d in range(2):
            for h in range(HA):
                i = d * HA + h
                k.act(mask[:, i, :], rel[:, d, :], AF.Exp, scale=lg[:, i:i + 1])
                k.act(qdec[:, i, :], qex[:, d, :], AF.Exp, scale=lg[:, i:i + 1])
                k.act(kdec[:, i:i + 1], kex[:, d:d + 1], AF.Exp, scale=lg[:, i:i + 1])
        k.act(cdec[:, :], lg[:, :], AF.Exp, scale=float(CH))
        gnw = S.sb([128, EV], F32)
        k.dma("sp", gnw[:, :], P.dr["ret_gn_w"][e].partition_broadcast(128))
        St = S.sb([128, HA * DC, DVA], F32)
        Sb = S.sb([128, HA * DC, DVA], BF16)
        qT = [S.sb([128, NQ, CH], BF16) for _ in range(2)]
        kT = [S.sb([128, NQ, CH], BF16) for _ in range(2)]
        kt = [S.sb([128, HA * DKA], BF16) for _ in range(2)]
        vt = [S.sb([128, EV], BF16) for _ in range(2)]
        sD = [S.sb([128, HA, CH], BF16) for _ in range(2)]
        qd = [S.sb([128, NQ, CH], BF16) for _ in range(2)]
        kd = [S.sb([128, HA * DKA], BF16) for _ in range(2)]
        ot = [S.sb([128, EV], F32) for _ in range(2)]
        o1 = [S.sb([128, EV], F32) for _ in range(2)]
        sg = [S.sb([128, EV], BF16) for _ in range(2)]
        st6 = S.sb([128, HA, 6], F32)
        mv = S.sb([128, HA, 2], F32)
        ya = [S.sb([128, EV], BF16) for _ in range(2)]
        yT = [S.sb([128, 4, CH], BF16) for _ in range(2)]
        p_sc = [S.ps([128, 512], F32) for _ in range(1)]
        p_o = [S.ps([128, 512], F32) for _ in range(2)]
        p_s = [S.ps([128, 512], F32) for _ in range(3)]
        p_t = [S.ps([128, 512], BF16) for _ in range(2)]
        cnt = {"o": 0, "s": 0, "t": 0}
        for si, (st, L, ci) in enumerate(cfg.SEGS):
            NCk = L // CH
            for d in range(2):
                if si == 0:
                    k.dma("sp", St[:, :, :], P.dr["state_ret"][e, d].rearrange("h (c p) v -> p (h c) v", p=128))
                    k.copy(Sb[:, :, :], St[:, :, :], en="pool")
                else:
                    k.memset(St[:, :, :], 0.0, en="pool")
                    k.memset(Sb[:, :, :], 0.0, en="pool")
                order = list(range(NCk)) if d == 0 else list(range(NCk - 1, -1, -1))

                def load(i, cn, d=d, st=st):
                    t0 = st + cn * CH
                    i2 = i % 2
                    k.dma("sp", qT[i2][:, :, :], P.dr["ab_rqk"][0:NQ * 128, t0:t0 + CH].rearrange("(c p) t -> p c t", p=128))
                    k.dma("sp", kT[i2][:, :, :], P.dr["ab_rqk"][NQ * 128:2 * NQ * 128, t0:t0 + CH].rearrange("(c p) t -> p c t", p=128))
                    k.dma("sp", kt[i2][:, :], P.dr["ab_rk_tok"][t0:t0 + CH, :])
                    k.dma("sp", vt[i2][:, :], P.dr["ab_rv_tok"][t0:t0 + CH, :])
                    if d == 1:
                        k.dma("sp", o1[i2][:, :], P.dr["ab_o1r"][t0:t0 + CH, :])
                        k.dma("sp", sg[i2][:, :], P.dr["ab_rg_tok"][t0:t0 + CH, :])

                def body(i, cn, d=d, st=st):
                    t0 = st + cn * CH
                    i2 = i % 2
                    q_, k_, kt_, v_, o_ = qT[i2], kT[i2], kt[i2], vt[i2], ot[i2]
                    s_, qd_, kd_ = sD[i2], qd[i2], kd[i2]
                    psc = p_sc[0]
                    hs = slice(d * HA, (d + 1) * HA)
                    for h in range(HA):
                        for dc in range(DC):
                            k.mm(psc[:, h * CH:(h + 1) * CH], k_[:, h * DC + dc, :], q_[:, h * DC + dc, :],
                                 start=(dc == 0), stop=(dc == DC - 1))
                    k.tt(s_[:, :, :], psc[:, 0:HA * CH].rearrange("p (h t) -> p h t", t=CH), mask[:, hs, :], ALU.mult)
                    k.tt(qd_[:, :, :].rearrange("p (h c) t -> p h c t", c=DC), q_[:, :, :].rearrange("p (h c) t -> p h c t", c=DC),
                         qdec[:, hs, :].unsqueeze(2).to_broadcast([128, HA, DC, CH]), ALU.mult, en="pool")
                    k.tt(kd_[:, :].rearrange("p (h x) -> p h x", x=DKA), kt_[:, :].rearrange("p (h x) -> p h x", x=DKA),
                         kdec[:, hs].unsqueeze(2).to_broadcast([128, HA, DKA]), ALU.mult, en="pool")
                    for h in range(HA):
                        ii = d * HA + h
                        po = p_o[cnt["o"] % 2]
                        cnt["o"] += 1
                        k.mm(po[:, 0:DVA], s_[:, h, :], v_[:, h * DVA:(h + 1) * DVA], start=True, stop=False)
                        for dc in range(DC):
                            k.mm(po[:, 0:DVA], qd_[:, h * DC + dc, :], Sb[:, h * DC + dc, :], start=False, stop=(dc == DC - 1))
                        k.copy(o_[:, h * DVA:(h + 1) * DVA], po[:, 0:DVA], en="act")
                    for h in range(HA):
                        ii = d * HA + h
                        for dc in range(DC):
                            pst = p_s[cnt["s"] % 3]
                            cnt["s"] += 1
                            k.mm(pst[:, 0:DVA], kd_[:, h * DKA + dc * 128:h * DKA + (dc + 1) * 128], v_[:, h * DVA:(h + 1) * DVA])
                            k.stt(St[:, h * DC + dc, :], St[:, h * DC + dc, :], cdec[:, ii:ii + 1], pst[:, 0:DVA], ALU.mult, ALU.add)
                            k.copy(Sb[:, h * DC + dc, :], St[:, h * DC + dc, :], en="act")
                    if d == 0:
                        k.dma("sp", P.dr["ab_o1r"][t0:t0 + CH, :], o_[:, :])
                    else:
                        o1_, sg_, ya_ = o1[i2], sg[i2], ya[i2]
                        k.tt(o_[:, :], o_[:, :], o1_[:, :], ALU.add, en="pool")
                        for h in range(HA):
                            k.do("dve", lambda en_, h=h: en_.bn_stats(out=st6[:, h, :].ap, in_=o_[:, h * DVA:(h + 1) * DVA].ap),
                                 reads=(o_[:, :],), writes=(st6[:, :, :],))
                            k.do("dve", lambda en_, h=h: en_.bn_aggr(out=mv[:, h, :].ap, in_=st6[:, h, :].ap),
                                 reads=(st6[:, :, :],), writes=(mv[:, :, :],))
                        k.ts(mv[:, :, 1], mv[:, :, 1], cfg.EPS, None, op0=ALU.add)
                        k.act(mv[:, :, 1], mv[:, :, 1], AF.Sqrt)
                        k.recip(mv[:, :, 1], mv[:, :, 1])
                        for h in range(HA):
                            sl = slice(h * DVA, (h + 1) * DVA)
                            k.ts(o_[:, sl], o_[:, sl], mv[:, h, 0:1], mv[:, h, 1:2], op0=ALU.subtract, op1=ALU.mult)
                        k.tt(o_[:, :], o_[:, :], gnw[:, :], ALU.mult, en="pool")
                        k.tt(ya_[:, :], o_[:, :], sg_[:, :], ALU.mult)
                        for g in range(EV // 512):
                            pt, y_ = p_t[cnt["t"] % 2], yT[cnt["t"] % 2]
                            cnt["t"] += 1
                            for c in range(4):
                                k.tr(pt[:, c * 128:(c + 1) * 128], ya_[:, g * 512 + c * 128:g * 512 + (c + 1) * 128], P.identh)
                            k.copy(y_[:, :, :], pt[:, 0:512].rearrange("p (c t) -> p c t", t=128), en=("act" if cnt["t"] % 2 else "dve"))
                            k.dma("sp", P.dr["ab_catT"][g * 512:(g + 1) * 512, t0:t0 + CH].rearrange("(c p) t -> p c t", p=128), y_[:, :, :])
                pipeline(order, load, body)
                if si > 0:
                    k.dma("sp", P.dr["nsr"][si - 1, e, d].rearrange("h (c p) v -> p (h c) v", p=128), St[:, :, :])


def ab_gdn(P, e):
    k, cfg = P.k, P.cfg
    HB, DKB, DVB = cfg.H_B, cfg.DK_B, cfg.DV_B
    EV = HB * DVB
    NLEV = 7
    G = 4
    NG_ = HB // G
    GW = G * CH
    with k.scope() as S:
        U = S.sb([128, 2, CH], F32)
        k.dma("sp", U[:, :, :], P.dr["ab_U"].rearrange("d s t -> s d t"))
        negm = S.sb([128, 2, CH], F32)
        k.dma("sp", negm[:, :, :], P.dr["ab_negm"].rearrange("d s t -> s d t"))
        strict = S.sb([128, 2, CH], F32)
        k.dma("sp", strict[:, :, :], P.dr["ab_strict"].rearrange("d s t -> s d t"))
        gnw = S.sb([128, DVB], F32)
        k.dma("sp", gnw[:, :], P.dr["gdn_norm_w"][e].partition_broadcast(128))
        lm = S.sb([128, 2, 7, CH], F32)
        lmT = S.sb([128, 2, 7, CH], F32)
        for d_ in range(2):
            k.dma("sp", lm[:, d_, :, :], P.dr["ab_lm"][d_].rearrange("l t s -> t l s"))
            k.dma("sp", lmT[:, d_, :, :], P.dr["ab_lmT"][d_].rearrange("l t s -> t l s"))
        St = S.sb([128, HB, DVB], F32)
        Sb = S.sb([128, HB, DVB], BF16)
        qT = [S.sb([128, HB, CH], BF16) for _ in range(2)]
        kT = [S.sb([128, HB, CH], BF16) for _ in range(2)]
        kv = [S.sb([128, 2 * EV], BF16) for _ in range(2)]
        gb = [S.sb([128, 32], F32) for _ in range(2)]
        ot = [S.sb([128, EV], F32) for _ in range(2)]
        o1 = [S.sb([128, EV], F32) for _ in range(2)]
        sz = [S.sb([128, EV], BF16) for _ in range(2)]
        ybf = [S.sb([128, EV], F32) for _ in range(2)]
        yT = [S.sb([128, 4, CH], BF16) for _ in range(2)]
        ss = S.sb([128, HB], F32)
        sqt = S.sb([128, EV], F32)

        def grp(shape, dt):
            return [S.sb(shape, dt) for _ in range(NG_)]
        W3 = [128, G, CH]
        gbc, bbc, gsm = grp(W3, F32), grp(W3, F32), grp(W3, F32)
        sc = grp([128, 6, G], F32)
        dec, eGr, t1, Qf, uf = grp(W3, F32), grp(W3, F32), grp(W3, F32), grp(W3, F32), grp(W3, F32)
        Wb, Zb, Lb, LTb, M1, M2 = (grp(W3, BF16) for _ in range(6))
        rv, rk, wT, ub, qk, qd, kd = (grp(W3, BF16) for _ in range(7))
        tS = grp(W3, F32)
        bank = [[S.ps([128, 512], F32) for _ in range(4)] for _ in range(NG_)]
        identf = P.ident

        def bc_h(v):
            return v.unsqueeze(1).to_broadcast([128, G, CH])

        def bc_t(v):
            return v.unsqueeze(2).to_broadcast([128, G, CH])

        def v3(b):
            return b[:, 0:GW].rearrange("p (g t) -> p g t", t=CH)
        it = 0
        nt = 0
        for si, (st, L, ci) in enumerate(cfg.SEGS):
            NCk = L // CH
            for d in range(2):
                if si == 0:
                    k.dma("sp", St[:, :, :], P.dr["state_gdn"][e, d].rearrange("h p v -> p h v"))
                    k.copy(Sb[:, :, :], St[:, :, :], en="pool")
                else:
                    k.memset(St[:, :, :], 0.0, en="pool")
                    k.memset(Sb[:, :, :], 0.0, en="pool")
                last = CH - 1 if d == 0 else 0
                order = list(range(NCk)) if d == 0 else list(range(NCk - 1, -1, -1))

                def load(i, cn, d=d, st=st):
                    t0 = st + cn * CH
                    i2 = i % 2
                    k.dma("sp", qT[i2][:, :, :], P.dr["ab_gT"][0:EV, t0:t0 + CH].rearrange("(c p) t -> p c t", p=128))
                    k.dma("sp", kT[i2][:, :, :], P.dr["ab_gT"][EV:2 * EV, t0:t0 + CH].rearrange("(c p) t -> p c t", p=128))
                    k.dma("sp", kv[i2][:, :], P.dr["ab_gtok"][t0:t0 + CH, :])
                    k.dma("sp", gb[i2][:, :], P.dr["ab_gb"][t0:t0 + CH, :])
                    if d == 1:
                        k.dma("sp", o1[i2][:, :], P.dr["ab_o1g"][t0:t0 + CH, :])
                        k.dma("sp", sz[i2][:, :], P.dr["ab_gz_tok"][t0:t0 + CH, :])

                def body(i, cn, d=d, st=st, last=last):
                    nonlocal nt
                    t0 = st + cn * CH
                    i2 = i % 2
                    q_, k_, kv_, gb_, o_ = qT[i2], kT[i2], kv[i2], gb[i2], ot[i2]
                    stages = []
                    for g in range(NG_):
                        h0 = g * G
                        B0, B1, B2, B3 = bank[g]
                        gcols = gb_[:, d * HB + h0:d * HB + h0 + G]
                        bcols = gb_[:, 16 + d * HB + h0:16 + d * HB + h0 + G]
                        ktok3 = kv_[:, h0 * DKB:(h0 + G) * DKB].rearrange("p (g t) -> p g t", t=DKB)
                        vtok3 = kv_[:, EV + h0 * DVB:EV + (h0 + G) * DVB].rearrange("p (g t) -> p g t", t=DVB)
                        sc_ = sc[g]
                        sg = []

                        def s1(g=g, h0=h0, B0=B0, B1=B1, B3=B3, gcols=gcols, bcols=bcols, sc_=sc_):
                            k.copy(gbc[g][:, :, :], bc_t(gcols), en="pool")
                            k.tt(gsm[g][:, :, :], gbc[g][:, :, :], bc_h(strict[:, 1 - d, :]), ALU.mult, en="pool")
                            k.copy(bbc[g][:, :, :], bc_t(bcols), en="pool")
                            for h in range(G):
                                k.mm(B0[:, h * CH:(h + 1) * CH], gbc[g][:, h, :], U[:, d, :])
                            for h in range(G):
                                k.mm(B1[:, h * CH:(h + 1) * CH], gsm[g][:, h, :], U[:, d, :])
                            for h in range(G):
                                k.mm(B3[:, h:h + 1], U[:, d, :], gcols[:, h:h + 1])
                        sg.append(s1)

                        def s2(g=g, B0=B0, B1=B1, B3=B3, sc_=sc_, bcols=bcols):
                            k.copy(sc_[:, 0, :], B3[:, 0:G])
                            k.act(sc_[:, 1, :], sc_[:, 0, :], AF.Exp)
                            k.act(sc_[:, 2, :], v3(B0)[:, :, last], AF.Exp)
                            k.act(sc_[:, 3, :], v3(B1)[:, :, last], AF.Exp)
                            k.tt(sc_[:, 4, :], sc_[:, 1, :], bcols, ALU.mult)
                            k.tt(dec[g][:, :, :], v3(B1), bc_h(negm[:, d, :]), ALU.add)
                            k.act(dec[g][:, :, :], dec[g][:, :, :], AF.Exp)
                            k.act(eGr[g][:, :, :], v3(B0), AF.Exp)
                        sg.append(s2)

                        def s3(g=g, h0=h0, B0=B0, B2=B2, B3=B3):
                            for h in range(G):
                                k.mm(B2[:, h * CH:(h + 1) * CH], bbc[g][:, h, :], identf)
                            for h in range(G):
                                k.mm(B3[:, h * CH:(h + 1) * CH], k_[:, h0 + h, :], k_[:, h0 + h, :])
                            for h in range(G):
                                k.mm(B0[:, h * CH:(h + 1) * CH], k_[:, h0 + h, :], q_[:, h0 + h, :])
                            k.tt(t1[g][:, :, :], dec[g][:, :, :], bc_h(strict[:, d, :]), ALU.mult, en="pool")
                        sg.append(s3)

                        def s4(g=g, B0=B0, B1=B1, B2=B2, B3=B3):
                            k.tt(t1[g][:, :, :], v3(B2), t1[g][:, :, :], ALU.mult)
                            k.stt(Qf[g][:, :, :], v3(B3), -1.0, t1[g][:, :, :], ALU.mult, ALU.mult)
                            k.tt(qk[g][:, :, :], v3(B0), dec[g][:, :, :], ALU.mult)
                            for h in range(G):
                                k.tr(B1[:, h * CH:(h + 1) * CH], Qf[g][:, h, :], identf)
                            k.copy(uf[g][:, :, :], v3(B1), en="act")
                            k.copy(Wb[g][:, :, :], bc_h(identf), en="pool")
                            k.copy(Zb[g][:, :, :], bc_h(identf), en="pool")
                        sg.append(s4)
                        for lev in range(NLEV):
                            lastlev = lev == NLEV - 1

                            def sl(g=g, lev=lev, lastlev=lastlev, B0=B0, B1=B1, B2=B2, B3=B3):
                                k.tt(Lb[g][:, :, :], uf[g][:, :, :], bc_h(lm[:, d, lev, :]), ALU.mult, en="pool")
                                for h in range(G):
                                    k.mm(B2[:, h * CH:(h + 1) * CH], Lb[g][:, h, :], Wb[g][:, h, :])
                                k.act(M1[g][:, :, :], v3(B2), AF.Identity, scale=-1.0)
                                for h in range(G):
                                    k.mm(B0[:, h * CH:(h + 1) * CH], Zb[g][:, h, :], M1[g][:, h, :])
                                if not lastlev:
                                    k.tt(LTb[g][:, :, :], Qf[g][:, :, :], bc_h(lmT[:, d, lev, :]), ALU.mult, en="pool")
                                    for h in range(G):
                                        k.mm(B3[:, h * CH:(h + 1) * CH], LTb[g][:, h, :], Zb[g][:, h, :])
                                    k.ts(M2[g][:, :, :], v3(B3), -1.0, None, op0=ALU.mult)
                                    for h in range(G):
                                        k.mm(B1[:, h * CH:(h + 1) * CH], Wb[g][:, h, :], M2[g][:, h, :])
                                k.tt(Wb[g][:, :, :], v3(B0), Wb[g][:, :, :], ALU.add)
                                if not lastlev:
                                    k.tt(Zb[g][:, :, :], v3(B1), Zb[g][:, :, :], ALU.add)
                            sg.append(sl)

                        def s5(g=g, h0=h0, B0=B0, B1=B1, B2=B2, B3=B3, sc_=sc_, bcols=bcols, ktok3=ktok3, vtok3=vtok3):
                            k.tt(rv[g][:, :, :], vtok3, bc_t(bcols), ALU.mult, en="pool")
                            k.tt(rk[g][:, :, :], ktok3, bc_t(sc_[:, 4, :]), ALU.mult, en="pool")
                            k.tt(kd[g][:, :, :], ktok3, bc_t(sc_[:, 3, :]), ALU.mult, en="pool")
                            k.tt(qd[g][:, :, :], q_[:, h0:h0 + G, :], eGr[g][:, :, :], ALU.mult, en="pool")
                            for h in range(G):
                                k.mm(B2[:, h * CH:(h + 1) * CH], Wb[g][:, h, :], rv[g][:, h, :])
                            for h in range(G):
                                k.mm(B3[:, h * CH:(h + 1) * CH], rk[g][:, h, :], Wb[g][:, h, :])
                            k.copy(wT[g][:, :, :], v3(B3), en="act")
                            k.copy(tS[g][:, :, :], v3(B2))
                            for h in range(G):
                                k.mm(B0[:, h * CH:(h + 1) * CH], wT[g][:, h, :], Sb[:, h0 + h, :])
                            k.tt(ub[g][:, :, :], tS[g][:, :, :], v3(B0), ALU.subtract)
                        sg.append(s5)

                        def s6(g=g, h0=h0, B1=B1, B2=B2, sc_=sc_):
                            for h in range(G):
                                k.mm(B1[:, h * CH:(h + 1) * CH], qd[g][:, h, :], Sb[:, h0 + h, :], start=True, stop=False)
                                k.mm(B1[:, h * CH:(h + 1) * CH], qk[g][:, h, :], ub[g][:, h, :], start=False, stop=True)
                            k.copy(o_[:, h0 * DVB:(h0 + G) * DVB], B1[:, 0:GW], en="act")
                            for h in range(G):
                                k.mm(B2[:, h * CH:(h + 1) * CH], kd[g][:, h, :], ub[g][:, h, :])
                            k.tt(tS[g][:, :, :], St[:, h0:h0 + G, :], bc_t(sc_[:, 2, :]), ALU.mult, en="pool")
                            k.tt(St[:, h0:h0 + G, :], v3(B2), tS[g][:, :, :], ALU.add)
                            k.copy(Sb[:, h0:h0 + G, :], St[:, h0:h0 + G, :], en="act")
                        sg.append(s6)
                        stages.append(sg)
                    for sidx in range(len(stages[0])):
                        for g in range(NG_):
                            stages[g][sidx]()
                    if d == 0:
                        k.dma("sp", P.dr["ab_o1g"][t0:t0 + CH, :], o_[:, :])
                    else:
                        o1_, sz_, yb_ = o1[i2], sz[i2], ybf[i2]
                        k.tt(o_[:, :], o_[:, :], o1_[:, :], ALU.add, en="pool")
                        k.act(sqt[:, :], o_[:, :], AF.Square)
                        k.do("dve", lambda en_: en_.tensor_reduce(out=ss[:, :].ap, in_=sqt[:, :].rearrange("p (h v) -> p h v", v=DVB).ap,
                                                                    axis=mybir.AxisListType.X, op=ALU.add),
                             reads=(sqt[:, :],), writes=(ss[:, :],))
                        k.ts(ss[:, :], ss[:, :], 1.0 / DVB, cfg.EPS, op0=ALU.mult, op1=ALU.add)
                        k.act(ss[:, :], ss[:, :], AF.Sqrt)
                        k.recip(ss[:, :], ss[:, :])
                        o3 = o_[:, :].rearrange("p (h v) -> p h v", v=DVB)
                        k.tt(o3, o3, ss[:, :].unsqueeze(2).to_broadcast([128, HB, DVB]), ALU.mult)
                        k.tt(o3, o3, gnw[:, :].unsqueeze(1).to_broadcast([128, HB, DVB]), ALU.mult, en="pool")
                        k.tt(yb_[:, :], o_[:, :], sz_[:, :], ALU.mult)
                        for gg in range(EV // 512):
                            pt, y_ = bank[gg % NG_][0], yT[nt % 2]
                            nt += 1
                            for c in range(4):
                                k.tr(pt[:, c * 128:(c + 1) * 128], yb_[:, gg * 512 + c * 128:gg * 512 + (c + 1) * 128], identf)
                            k.copy(y_[:, :, :], pt[:, 0:512].rearrange("p (c t) -> p c t", t=128), en=("act" if nt % 2 else "dve"))
                            r0 = cfg.H_A * cfg.DV_A + gg * 512
                            k.dma("sp", P.dr["ab_catT"][r0:r0 + 512, t0:t0 + CH].rearrange("(c p) t -> p c t", p=128), y_[:, :, :])
                pipeline(order, load, body)
                if si > 0:
                    k.dma("sp", P.dr["nsg"][si - 1, e, d].rearrange("h p v -> p h v"), St[:, :, :])


def mixer_ab_impl(P, e):
    cfg = P.cfg
    D = cfg.D_MODEL
    groups = token_groups(cfg)
    HA, DKA, DVA, HB, DKB, DVB = cfg.H_A, cfg.DK_A, cfg.DV_A, cfg.H_B, cfg.DK_B, cfg.DV_B
    import os
    stop = int(os.environ.get("AB_STOP", "99"))
    steps = [
        lambda: proj_fm(P, P.dr["w_in_ab"][e], D, cfg.PROJ_AB, P.dr["hT"], P.dr["ab_pT"], F32, groups),
        lambda: ab_rope(P),
        lambda: fm_to_tm(P, P.dr["ab_rqk"], HA * DKA, HA * DKA, P.dr["ab_rk_tok"], 0, BF16, BF16),
        lambda: fm_to_tm(P, P.dr["ab_pT"], 2 * HA * DKA, HA * DVA, P.dr["ab_rv_tok"], 0, F32, BF16),
        lambda: fm_to_tm(P, P.dr["ab_pT"], 2 * HA * DKA + HA * DVA, HA * DVA, P.dr["ab_rg_tok"], 0, F32, BF16, func=AF.Silu),
        lambda: ab_gdn_conv(P, e),
        lambda: fm_to_tm(P, P.dr["ab_gT"], HB * DKB, 2 * HB * DKB, P.dr["ab_gtok"], 0, BF16, BF16),
        lambda: fm_to_tm(P, P.dr["ab_pT"], 2 * HA * DKA + 2 * HA * DVA + 3 * HB * DKB, HB * DVB, P.dr["ab_gz_tok"], 0, F32, BF16, func=AF.Silu),
        lambda: ab_gates(P, e),
        lambda: ab_retention(P, e),
        lambda: ab_gdn(P, e),
        lambda: proj_fm(P, P.dr["w_out_ab"][e], cfg.MIX_AB_OUT, D, P.dr["ab_catT"], P.dr["yT"], F32, groups),
    ]
    names = ["inproj", "rope", "rk_tok", "rv_tok", "rg_tok", "gconv", "g_tok", "gz_tok", "gates", "retention", "gdn", "outproj"]
    for i, f in enumerate(steps):
        if i >= stop:
            break
        f()
        P.mark(f"ab{e}.{names[i]}")
```
